# Optimizing a Trainium2 kernel written in Bass

```python
import math
import numpy as np
import jax
import jax.numpy as jnp
from jax import lax


D_MODEL = 1024
BATCH = 8
SEQ = 4096
DEPTH = 4

GRID_W = 64
CTX_LEN = 256
ROPE_THETA = 10000.0
Q_BLOCK = 128
EPS = 1e-6
ADA_SCALE = 0.2

MIX_WIDTH = D_MODEL
GROUP_WIDTH = MIX_WIDTH // 4

RET_HEADS = 4
RET_DK = GROUP_WIDTH // RET_HEADS
RET_DV = GROUP_WIDTH // RET_HEADS
RET_CHUNK = 128
DIFF_HEADS = 4
DIFF_DV = GROUP_WIDTH // DIFF_HEADS
DIFF_D = DIFF_DV // 2
GQA_HEADS = 4
GQA_KV_HEADS = 2
GQA_D = GROUP_WIDTH // GQA_HEADS
MLA_HEADS = 4
MLA_Q_RANK = 192
MLA_KV_RANK = 128
MLA_NOPE = 64
MLA_ROPE = 32
MLA_DV = GROUP_WIDTH // MLA_HEADS

IN_SPLITS = (
    RET_HEADS * RET_DK, RET_HEADS * RET_DK, RET_HEADS * RET_DV, RET_HEADS * RET_DV,
    DIFF_HEADS * 2 * DIFF_D, DIFF_HEADS * 2 * DIFF_D, DIFF_HEADS * DIFF_DV,
    GQA_HEADS * GQA_D, GQA_KV_HEADS * GQA_D, GQA_KV_HEADS * GQA_D,
    MLA_Q_RANK, MLA_KV_RANK, MLA_ROPE,
)
IN_WIDTH = sum(IN_SPLITS)

N_EXPERTS = 32
TOP_K = 4
D_FF = D_MODEL
SWIGLU_LIMIT = 7.0
SWIGLU_ALPHA = 1.702
MOE_BLOCK = 256

kernel_name = 'hybrid_parallel_heads_retention_diff_gqa_mla_moe'


def rms_norm(x, g):
    xf = x.astype(jnp.float32)
    y = xf * lax.rsqrt(jnp.mean(xf * xf, axis=-1, keepdims=True) + EPS)
    return y.astype(x.dtype) * g


def modulate(x, g, shift, scale):
    return rms_norm(x, g) * (1 + scale) + shift


def grid_rope(rows, rot_dim):
    row = jnp.repeat(jnp.arange(rows), GRID_W)
    col = jnp.tile(jnp.arange(GRID_W), rows)
    n_freq = rot_dim // 4
    inv = ROPE_THETA ** (-jnp.arange(n_freq, dtype=jnp.float32) / n_freq)
    ang = jnp.concatenate([row[:, None] * inv, col[:, None] * inv], axis=-1)
    return jnp.cos(ang), jnp.sin(ang)


def apply_rope(x, rope):
    cos, sin = rope
    half = x.shape[-1] // 2
    x1 = x[..., :half].astype(jnp.float32)
    x2 = x[..., half:].astype(jnp.float32)
    return jnp.concatenate([x1 * cos - x2 * sin, x1 * sin + x2 * cos], axis=-1).astype(x.dtype)


def split_heads(a, n_heads):
    b, s, w = a.shape
    return a.reshape(b, s, n_heads, w // n_heads).transpose(0, 2, 1, 3)


def merge_heads(o):
    b, h, s, d = o.shape
    return o.transpose(0, 2, 1, 3).reshape(b, s, h * d)


def softmax_f32(s):
    return jax.nn.softmax(s.astype(jnp.float32), axis=-1)


def sweep_query_blocks(block_fn, *qs):
    n = qs[0].shape[-2]
    nb = n // Q_BLOCK

    def to_blocks(a):
        return jnp.moveaxis(a.reshape(a.shape[:-2] + (nb, Q_BLOCK, a.shape[-1])), -3, 0)

    out = lax.map(lambda qb: block_fn(*qb), tuple(to_blocks(a) for a in qs))
    out = jnp.moveaxis(out, 0, -3)
    return out.reshape(out.shape[:-3] + (n, out.shape[-1]))


def retention_chunks(q, k, v, log_g, s0):
    b, h, s, _ = q.shape
    nc = s // RET_CHUNK
    pos = jnp.arange(RET_CHUNK, dtype=jnp.float32)
    dist = pos[:, None] - pos[None, :]
    decay = jnp.where(dist >= 0, jnp.exp(log_g[:, None, None] * jnp.maximum(dist, 0.0)), 0.0)
    xi = jnp.exp(log_g[:, None] * (pos + 1.0))[:, :, None]
    zeta = jnp.exp(log_g[:, None] * (RET_CHUNK - 1.0 - pos))[:, :, None]
    g_chunk = jnp.exp(log_g * RET_CHUNK)[:, None, None]

    def to_chunks(a):
        return jnp.moveaxis(a.reshape(b, h, nc, RET_CHUNK, a.shape[-1]), 2, 0)

    def step(state, qkv):
        qc, kc, vc = qkv
        inner = jnp.einsum('bhnd,bhmd->bhnm', qc, kc) * decay
        o = jnp.einsum('bhnm,bhme->bhne', inner, vc) + jnp.einsum('bhnd,bhde->bhne', qc, state) * xi
        state = g_chunk * state + jnp.einsum('bhmd,bhme->bhde', kc * zeta, vc)
        return state, o

    state, o = lax.scan(step, s0, (to_chunks(q), to_chunks(k), to_chunks(v)))
    o = jnp.moveaxis(o, 0, 2).reshape(b, h, s, v.shape[-1])
    return o, state


def head_group_norm(o, w, bias):
    of = o.astype(jnp.float32)
    mu = jnp.mean(of, axis=-1, keepdims=True)
    var = jnp.mean(jnp.square(of - mu), axis=-1, keepdims=True)
    return merge_heads((of - mu) * lax.rsqrt(var + EPS)) * w + bias


def retention_group(q_l, k_l, v_l, g_l, q_c, k_c, v_c, g_c, log_decay, gn_w, gn_b, rope, ctx_out):
    scale = RET_DK ** -0.5
    ql = apply_rope(split_heads(q_l, RET_HEADS), rope)
    kl = apply_rope(split_heads(k_l, RET_HEADS), rope) * scale
    vl = split_heads(v_l, RET_HEADS)
    qc = split_heads(q_c, RET_HEADS)
    kc = split_heads(k_c, RET_HEADS) * scale
    vc = split_heads(v_c, RET_HEADS)
    log_g = -jnp.exp(log_decay.astype(jnp.float32))
    zero = jnp.zeros((qc.shape[0], RET_HEADS, RET_DK, RET_DV), jnp.float32)
    flip = lambda a: jnp.flip(a, axis=2)
    oc_f, sc_f = retention_chunks(qc, kc, vc, log_g[0], zero)
    oc_b, sc_b = retention_chunks(flip(qc), flip(kc), flip(vc), log_g[1], zero)
    ol_f, _ = retention_chunks(ql, kl, vl, log_g[0], sc_f)
    ol_b, _ = retention_chunks(flip(ql), flip(kl), flip(vl), log_g[1], sc_b)

    def readout(o, g):
        return (head_group_norm(o, gn_w, gn_b) * jax.nn.silu(g)).astype(g.dtype)

    y_l = readout(ol_f + flip(ol_b), g_l)
    y_c = readout(oc_f + flip(oc_b), g_c) if ctx_out else None
    return y_l, y_c


def diff_group(q_l, k_l, v_l, q_c, k_c, v_c, lam_params, subln_w, lam_init, rope, ctx_out):
    def prep(q, k, v):
        q = split_heads(q, DIFF_HEADS)
        k = split_heads(k, DIFF_HEADS)
        return q[..., :DIFF_D], q[..., DIFF_D:], k[..., :DIFF_D], k[..., DIFF_D:], split_heads(v, DIFF_HEADS)

    q1l, q2l, k1l, k2l, vl = prep(q_l, k_l, v_l)
    q1l, q2l, k1l, k2l = (apply_rope(a, rope) for a in (q1l, q2l, k1l, k2l))
    q1c, q2c, k1c, k2c, vc = prep(q_c, k_c, v_c)
    lp = lam_params.astype(jnp.float32)
    lam = jnp.exp(jnp.sum(lp[0] * lp[1])) - jnp.exp(jnp.sum(lp[2] * lp[3])) + lam_init
    scale = DIFF_D ** -0.5

    def attend(q1, q2, k1, k2, v):
        p1 = softmax_f32(jnp.einsum('bhqd,bhkd->bhqk', q1, k1) * scale)
        p2 = softmax_f32(jnp.einsum('bhqd,bhkd->bhqk', q2, k2) * scale)
        return jnp.einsum('bhqk,bhkd->bhqd', (p1 - lam * p2).astype(v.dtype), v)

    k1a = jnp.concatenate([k1c, k1l], axis=2)
    k2a = jnp.concatenate([k2c, k2l], axis=2)
    va = jnp.concatenate([vc, vl], axis=2)

    def readout(o):
        return merge_heads(rms_norm(o, subln_w) * (1 - lam_init))

    y_l = readout(sweep_query_blocks(lambda a, b: attend(a, b, k1a, k2a, va), q1l, q2l))
    y_c = readout(attend(q1c, q2c, k1c, k2c, vc)) if ctx_out else None
    return y_l, y_c


def gqa_group(q_l, k_l, v_l, q_c, k_c, v_c, qk_norm, rope, ctx_out):
    n_rep = GQA_HEADS // GQA_KV_HEADS

    def prep(q, k, v):
        b, s, _ = q.shape
        q = q.reshape(b, s, GQA_KV_HEADS, n_rep, GQA_D).transpose(0, 2, 3, 1, 4)
        k = split_heads(k, GQA_KV_HEADS)
        return rms_norm(q, qk_norm[0]), rms_norm(k, qk_norm[1]), split_heads(v, GQA_KV_HEADS)

    ql, kl, vl = prep(q_l, k_l, v_l)
    ql, kl = apply_rope(ql, rope), apply_rope(kl, rope)
    qc, kc, vc = prep(q_c, k_c, v_c)
    scale = GQA_D ** -0.5

    def attend(q, k, v):
        p = softmax_f32(jnp.einsum('bngqd,bnkd->bngqk', q, k) * scale)
        return jnp.einsum('bngqk,bnkd->bngqd', p.astype(v.dtype), v)

    ka = jnp.concatenate([kc, kl], axis=2)
    va = jnp.concatenate([vc, vl], axis=2)

    def readout(o):
        b, _, _, s, _ = o.shape
        return o.transpose(0, 3, 1, 2, 4).reshape(b, s, GROUP_WIDTH)

    y_l = readout(sweep_query_blocks(lambda q: attend(q, ka, va), ql))
    y_c = readout(attend(qc, kc, vc)) if ctx_out else None
    return y_l, y_c


def mla_group(cq_l, ckv_l, kr_l, cq_c, ckv_c, kr_c, q_norm, kv_norm, w_uq, w_ukv, rope, ctx_out):
    def prep(cq, ckv, kr):
        q = split_heads(rms_norm(cq, q_norm) @ w_uq, MLA_HEADS)
        kv = split_heads(rms_norm(ckv, kv_norm) @ w_ukv, MLA_HEADS)
        return q[..., :MLA_NOPE], q[..., MLA_NOPE:], kv[..., :MLA_NOPE], kv[..., MLA_NOPE:], kr

    qnl, qrl, knl, vl, krl = prep(cq_l, ckv_l, kr_l)
    qrl, krl = apply_rope(qrl, rope), apply_rope(krl, rope)
    qnc, qrc, knc, vc, krc = prep(cq_c, ckv_c, kr_c)
    scale = (MLA_NOPE + MLA_ROPE) ** -0.5

    def attend(qn, qr, kn, kr, v):
        s = jnp.einsum('bhqd,bhkd->bhqk', qn, kn) + jnp.einsum('bhqr,bkr->bhqk', qr, kr)
        p = softmax_f32(s * scale)
        return jnp.einsum('bhqk,bhkd->bhqd', p.astype(v.dtype), v)

    kna = jnp.concatenate([knc, knl], axis=2)
    kra = jnp.concatenate([krc, krl], axis=1)
    va = jnp.concatenate([vc, vl], axis=2)
    y_l = merge_heads(sweep_query_blocks(lambda a, b: attend(a, b, kna, kra, va), qnl, qrl))
    y_c = merge_heads(attend(qnc, qrc, knc, krc, vc)) if ctx_out else None
    return y_l, y_c


def mixing_sublayer(h_lat, h_ctx, w_in, w_out, ret_log_decay, ret_gn_w, ret_gn_b, diff_lambda,
                    diff_subln, gqa_qk_norm, mla_q_norm, mla_kv_norm, mla_w_uq, mla_w_ukv,
                    rope64, rope32, lam_init, ctx_out):
    points = [int(p) for p in np.cumsum(IN_SPLITS)[:-1]]
    pl = jnp.split(h_lat @ w_in, points, axis=-1)
    pc = jnp.split(h_ctx @ w_in, points, axis=-1)
    ret_l, ret_c = retention_group(*pl[0:4], *pc[0:4], ret_log_decay, ret_gn_w, ret_gn_b, rope64, ctx_out)
    dif_l, dif_c = diff_group(*pl[4:7], *pc[4:7], diff_lambda, diff_subln, lam_init, rope32, ctx_out)
    gqa_l, gqa_c = gqa_group(*pl[7:10], *pc[7:10], gqa_qk_norm, rope64, ctx_out)
    mla_l, mla_c = mla_group(*pl[10:13], *pc[10:13], mla_q_norm, mla_kv_norm, mla_w_uq, mla_w_ukv,
                             rope32, ctx_out)
    y_l = jnp.concatenate([ret_l, dif_l, gqa_l, mla_l], axis=-1) @ w_out
    y_c = jnp.concatenate([ret_c, dif_c, gqa_c, mla_c], axis=-1) @ w_out if ctx_out else None
    return y_l, y_c


def moe_ffn(h, w_r, b_r, w1, b1, w2, b2):
    n_tok, d = h.shape
    logits = (h @ w_r + b_r).astype(jnp.float32)
    top_logit, top_idx = lax.top_k(logits, TOP_K)
    gates = jax.nn.softmax(top_logit, axis=-1)
    n_assign = n_tok * TOP_K
    n_blocks = -(-n_assign // MOE_BLOCK) + N_EXPERTS
    e_flat = top_idx.reshape(-1)
    order = jnp.argsort(e_flat)
    e_sorted = e_flat[order]
    counts = jnp.bincount(e_flat, length=N_EXPERTS)
    starts = jnp.cumsum(counts) - counts
    padded = (counts + MOE_BLOCK - 1) // MOE_BLOCK * MOE_BLOCK
    pad_ends = jnp.cumsum(padded)
    dest = pad_ends[e_sorted] - padded[e_sorted] + jnp.arange(n_assign) - starts[e_sorted]
    src_tok = jnp.full((n_blocks * MOE_BLOCK,), n_tok, jnp.int32).at[dest].set((order // TOP_K).astype(jnp.int32))
    row_gate = jnp.zeros((n_blocks * MOE_BLOCK,), jnp.float32).at[dest].set(gates.reshape(-1)[order])
    block_expert = jnp.minimum(
        jnp.searchsorted(pad_ends, jnp.arange(n_blocks) * MOE_BLOCK, side='right'), N_EXPERTS - 1)
    x_rows = jnp.take(h, src_tok, axis=0, mode='fill', fill_value=0).reshape(n_blocks, MOE_BLOCK, d)

    def expert_block(args):
        xb, e = args
        z = xb @ w1[e] + b1[e]
        glu = jnp.minimum(z[:, ::2], SWIGLU_LIMIT)
        lin = jnp.clip(z[:, 1::2], -SWIGLU_LIMIT, SWIGLU_LIMIT)
        return (glu * jax.nn.sigmoid(SWIGLU_ALPHA * glu) * (lin + 1)) @ w2[e] + b2[e]

    y_rows = lax.map(expert_block, (x_rows, block_expert)).reshape(n_blocks * MOE_BLOCK, d)
    out = jnp.zeros((n_tok, d), jnp.float32).at[src_tok].add(y_rows * row_gate[:, None], mode='drop')
    return out.astype(h.dtype)


def setup_inputs(seed: int = 0) -> dict:
    key = jax.random.key(seed)
    ks = jax.random.split(key, 25)
    f32 = jnp.float32

    def normal(k, shape, scale):
        return jax.random.normal(k, shape, f32) * scale

    def gain(k, shape):
        return 1.0 + 0.02 * jax.random.normal(k, shape, f32)

    base_decay = jnp.log(-jnp.log(1.0 - 2.0 ** (-5.0 - jnp.arange(RET_HEADS, dtype=f32))))
    return {
        'x': normal(ks[0], (BATCH, SEQ, D_MODEL), 1.0),
        'c': normal(ks[1], (BATCH, D_MODEL), 1.0),
        'ctx': normal(ks[2], (BATCH, CTX_LEN, D_MODEL), 1.0),
        'c_ctx': normal(ks[3], (D_MODEL,), 1.0),
        'ada_w': normal(ks[4], (DEPTH, D_MODEL, 6 * D_MODEL), ADA_SCALE * D_MODEL ** -0.5),
        'ada_b': normal(ks[5], (DEPTH, 6 * D_MODEL), 0.01),
        'norm_g': gain(ks[6], (DEPTH, 4, D_MODEL)),
        'w_in': normal(ks[7], (DEPTH, D_MODEL, IN_WIDTH), D_MODEL ** -0.5),
        'w_out': normal(ks[8], (DEPTH, MIX_WIDTH, D_MODEL), MIX_WIDTH ** -0.5),
        'ret_log_decay': base_decay + normal(ks[9], (DEPTH, 2, RET_HEADS), 0.05),
        'ret_gn_w': gain(ks[10], (DEPTH, GROUP_WIDTH)),
        'ret_gn_b': normal(ks[11], (DEPTH, GROUP_WIDTH), 0.01),
        'diff_lambda': normal(ks[12], (DEPTH, 4, DIFF_D), 0.1),
        'diff_subln': gain(ks[13], (DEPTH, DIFF_DV)),
        'gqa_qk_norm': gain(ks[14], (DEPTH, 2, GQA_D)),
        'mla_q_norm': gain(ks[15], (DEPTH, MLA_Q_RANK)),
        'mla_kv_norm': gain(ks[16], (DEPTH, MLA_KV_RANK)),
        'mla_w_uq': normal(ks[17], (DEPTH, MLA_Q_RANK, MLA_HEADS * (MLA_NOPE + MLA_ROPE)), MLA_Q_RANK ** -0.5),
        'mla_w_ukv': normal(ks[18], (DEPTH, MLA_KV_RANK, MLA_HEADS * (MLA_NOPE + MLA_DV)), MLA_KV_RANK ** -0.5),
        'router_w': normal(ks[19], (DEPTH, D_MODEL, N_EXPERTS), D_MODEL ** -0.5),
        'router_b': normal(ks[20], (DEPTH, N_EXPERTS), 0.01),
        'exp_w1': normal(ks[21], (DEPTH, N_EXPERTS, D_MODEL, 2 * D_FF), D_MODEL ** -0.5),
        'exp_b1': normal(ks[22], (DEPTH, N_EXPERTS, 2 * D_FF), 0.01),
        'exp_w2': normal(ks[23], (DEPTH, N_EXPERTS, D_FF, D_MODEL), D_FF ** -0.5),
        'exp_b2': normal(ks[24], (DEPTH, N_EXPERTS, D_MODEL), 0.01),
    }


def reference(x, c, ctx, c_ctx, ada_w, ada_b, norm_g, w_in, w_out, ret_log_decay, ret_gn_w, ret_gn_b,
              diff_lambda, diff_subln, gqa_qk_norm, mla_q_norm, mla_kv_norm, mla_w_uq, mla_w_ukv,
              router_w, router_b, exp_w1, exp_b1, exp_w2, exp_b2):
    b, s, d = x.shape
    n_ctx = ctx.shape[1]
    ROWS = s // GRID_W
    rope64 = grid_rope(ROWS, RET_DK)
    rope32 = grid_rope(ROWS, DIFF_D)
    silu_c = jax.nn.silu(c)
    silu_cc = jax.nn.silu(c_ctx)
    for l in range(DEPTH):
        last = l == DEPTH - 1
        sh_a, sc_a, g_a, sh_f, sc_f, g_f = jnp.split((silu_c @ ada_w[l] + ada_b[l])[:, None, :], 6, axis=-1)
        csh_a, csc_a, cg_a, csh_f, csc_f, cg_f = jnp.split(silu_cc @ ada_w[l] + ada_b[l], 6, axis=-1)
        lam_init = 0.8 - 0.6 * math.exp(-0.3 * l)
        h_lat = modulate(x, norm_g[l, 0], sh_a, sc_a)
        h_ctx = modulate(ctx, norm_g[l, 0], csh_a, csc_a)
        y_lat, y_ctx = mixing_sublayer(h_lat, h_ctx, w_in[l], w_out[l], ret_log_decay[l], ret_gn_w[l],
                                       ret_gn_b[l], diff_lambda[l], diff_subln[l], gqa_qk_norm[l],
                                       mla_q_norm[l], mla_kv_norm[l], mla_w_uq[l], mla_w_ukv[l],
                                       rope64, rope32, lam_init, not last)
        x = x + g_a * rms_norm(y_lat, norm_g[l, 1])
        moe_params = (router_w[l], router_b[l], exp_w1[l], exp_b1[l], exp_w2[l], exp_b2[l])
        if last:
            h = modulate(x, norm_g[l, 2], sh_f, sc_f)
            y = moe_ffn(h.reshape(b * s, d), *moe_params).reshape(b, s, d)
            x = x + g_f * rms_norm(y, norm_g[l, 3])
        else:
            ctx = ctx + cg_a * rms_norm(y_ctx, norm_g[l, 1])
            h_lat = modulate(x, norm_g[l, 2], sh_f, sc_f).reshape(b * s, d)
            h_ctx = modulate(ctx, norm_g[l, 2], csh_f, csc_f).reshape(b * n_ctx, d)
            y = moe_ffn(jnp.concatenate([h_lat, h_ctx], axis=0), *moe_params)
            x = x + g_f * rms_norm(y[:b * s].reshape(b, s, d), norm_g[l, 3])
            ctx = ctx + cg_f * rms_norm(y[b * s:].reshape(b, n_ctx, d), norm_g[l, 3])
    return x
```

```python
import os
import math
import numpy as np
from contextlib import ExitStack
import concourse.bass as bass
import concourse.mybir as mybir
from concourse.bass_utils import run_bass_kernel_spmd

F32 = mybir.dt.float32
BF16 = mybir.dt.bfloat16
I32 = mybir.dt.int32
U32 = mybir.dt.uint32
AF = mybir.ActivationFunctionType
ALU = mybir.AluOpType
AX = mybir.AxisListType

D = 1024
NCTX = 256
NLAT = 4096
T = NCTX + NLAT
NT = T // 128
DEPTH = 4
GRID_W = 64
EPS = 1e-6
NEXP = 32
CAP = 1152
NSLOT = NEXP * CAP
TB = [(i * 512, 512) for i in range(8)] + [(4096, 256)]

ENGS = ("pe", "act", "dve", "pool", "sp")
N_DMA_SEMS = 8
SAME_ENGINE_SYNC = True


class Res:
    __slots__ = ("name", "w", "r")

    def __init__(self, name=""):
        self.name = name
        self.w = None
        self.r = []


class Sched:
    def __init__(self, nc, stack):
        self.nc = nc
        self.sems = {}
        self.cnt = {}
        for e in ENGS:
            self.sems[e] = stack.enter_context(nc.semaphore("s_" + e))
            self.cnt[e] = 0
        self.dma_next = {}
        for q in ("sp", "pool", "act"):
            for i in range(N_DMA_SEMS):
                k = ("dma", q, i)
                self.sems[k] = stack.enter_context(nc.semaphore("d_%s_%d" % (q, i)))
                self.cnt[k] = 0
            self.dma_next[q] = 0
        self.known = {e: {} for e in ENGS}
        self.lists = {e: [] for e in ENGS}
        self.n_ops = 0

    def _need(self, eng, deps):
        out = {}
        for d in deps:
            if d is None:
                continue
            k, v = d
            if k == eng and not (SAME_ENGINE_SYNC and eng != "pe"):
                continue
            if self.known[eng].get(k, 0) >= v:
                continue
            if out.get(k, 0) < v:
                out[k] = v
        for k, v in out.items():
            self.known[eng][k] = v
        return list(out.items())

    @staticmethod
    def _deps(reads, writes):
        deps = []
        for r in reads:
            deps.append(r.w)
        for w in writes:
            deps.append(w.w)
            deps.extend(w.r)
        return deps

    @staticmethod
    def _mark(tag, reads, writes):
        for r in reads:
            r.r.append(tag)
            if len(r.r) > 64:
                best = {}
                for k, v in r.r:
                    if best.get(k, 0) < v:
                        best[k] = v
                r.r = list(best.items())
        for w in writes:
            w.w = tag
            w.r = []

    def op(self, eng, fn, reads=(), writes=()):
        waits = self._need(eng, self._deps(reads, writes))
        self.cnt[eng] += 1
        v = self.cnt[eng]
        sem = self.sems[eng]
        sems = self.sems

        def emit(e, fn=fn, waits=waits, sem=sem):
            for k, val in waits:
                e.wait_ge(sems[k], val)
            fn(e).then_inc(sem, 1)
        self.lists[eng].append(emit)
        self._mark((eng, v), reads, writes)
        self.n_ops += 1

    def dma(self, q, out, in_, reads=(), writes=(), indirect=None, **kw):
        i = self.dma_next[q]
        self.dma_next[q] = (i + 1) % N_DMA_SEMS
        k = ("dma", q, i)
        deps = self._deps(reads, writes)
        if self.cnt[k] > 0:
            deps.append((k, self.cnt[k]))
        waits = self._need(q, deps)
        self.cnt[k] += 16
        v = self.cnt[k]
        sem = self.sems[k]
        sems = self.sems

        def emit(e, waits=waits, sem=sem, out=out, in_=in_, kw=kw, indirect=indirect):
            for kk, val in waits:
                e.wait_ge(sems[kk], val)
            if indirect is None:
                e.dma_start(out=out, in_=in_, **kw).then_inc(sem, 16)
            else:
                e.indirect_dma_start(out=out, in_=in_, **indirect).then_inc(sem, 16)
        self.lists[q].append(emit)
        self._mark((k, v), reads, writes)
        self.n_ops += 1

    def wait_all(self, eng, resources):
        deps = []
        for r in resources:
            deps.append(r.w)
            deps.extend(r.r)
        waits = self._need(eng, deps)
        sems = self.sems

        def emit(e, waits=waits):
            for kk, val in waits:
                e.wait_ge(sems[kk], val)
        self.lists[eng].append(emit)

    def drain(self):
        deps = [(k, v) for k, v in self.cnt.items() if isinstance(k, tuple) and v > 0]
        waits = self._need("sp", deps)
        sems = self.sems

        def emit(e, waits=waits):
            for kk, val in waits:
                e.wait_ge(sems[kk], val)
        self.lists["sp"].append(emit)
        self.flush()
        for e in ENGS:
            for k, v in self.cnt.items():
                self.known[e][k] = v

    def flush(self):
        nc = self.nc
        lists = self.lists
        self.lists = {e: [] for e in ENGS}
        if not any(lists.values()):
            return
        with nc.Block() as block:
            @block.tensor
            def _(e):
                for f in lists["pe"]:
                    f(e)

            @block.scalar
            def _(e):
                for f in lists["act"]:
                    f(e)

            @block.vector
            def _(e):
                for f in lists["dve"]:
                    f(e)

            @block.gpsimd
            def _(e):
                for f in lists["pool"]:
                    f(e)

            @block.sync
            def _(e):
                for f in lists["sp"]:
                    f(e)


def _col(v):
    v = np.asarray(v, np.float32)
    return np.ascontiguousarray(v.reshape(-1, 128).T)


def _swap_blocks(cols, blk):
    cols = np.asarray(cols)
    out = cols.reshape(-1, 2, blk // 2)[:, ::-1, :].reshape(-1)
    return out


def build_wext_index():
    r = np.arange
    units = []
    units.append(r(0, 128)); units.append(r(128, 256))
    units.append(r(256, 384)); units.append(r(384, 512))
    units.append(r(1024, 1152)); units.append(r(1152, 1280))
    units.append(r(1280, 1408)); units.append(r(1408, 1536))
    gq = 1792
    units.append(np.concatenate([r(gq, gq + 64), r(gq + 128, gq + 192)]))
    units.append(np.concatenate([r(gq + 64, gq + 128), r(gq + 192, gq + 256)]))
    units.append(r(2048, 2176))
    units.append(np.tile(r(2624, 2656), 4))
    blks = [64, 64, 64, 64, 32, 32, 32, 32, 64, 64, 64, 32]
    sw = [_swap_blocks(u, b) for u, b in zip(units, blks)]
    feat = units + sw
    feat.append(r(2304, 2432))
    feat.append(r(2432, 2496))
    feat.append(r(2496, 2624))
    cols = []
    for u in feat:
        if len(u) < 128:
            u = np.concatenate([u, np.zeros(128 - len(u), np.int64)])
        cols.append(u)
    tokc = np.concatenate([r(512, 1024), r(1536, 1792), r(2176, 2304)])
    return np.concatenate(cols + [tokc]).astype(np.int64)


WEXT_IDX = build_wext_index()
NFEAT_UNITS = 27
NWEXT = WEXT_IDX.shape[0]
TOK_OFF = NFEAT_UNITS * 128
U_RQ, U_RK, U_DQ, U_DK, U_GQ, U_GK, U_MKR = 0, 2, 4, 6, 8, 10, 11
U_SW = 12
U_CQ0, U_CQ1, U_CKV = 24, 25, 26

_r = np.arange
WUQ_IDX = np.concatenate([
    _r(0, 64), _r(96, 160), _r(192, 256), _r(288, 352),
    _r(64, 96), _r(160, 192), _r(256, 288), _r(352, 384),
    _swap_blocks(np.concatenate([_r(64, 96), _r(160, 192), _r(256, 288), _r(352, 384)]), 32)])
WUKV_IDX = np.concatenate([
    _r(0, 64), _r(128, 192), _r(256, 320), _r(384, 448),
    _r(64, 128), _r(192, 256), _r(320, 384), _r(448, 512)])

RV_G1, RV_G3, RV_GNW, RV_GNB, RV_SUBLN, RV_ADAB, RV_RB, RV_DECAY, RV_LAM, RV_LAMINIT = (
    0, 1024, 2048, 2304, 2560, 2624, 8768, 8800, 8808, 8936)
RV_G0, RV_G2 = 8944, 9968
NRV = 10992
CV_GQG, CV_GQGS, CV_GKG, CV_GKGS, CV_QN0, CV_QN1, CV_KVG, CV_G0, CV_G2 = 0, 1, 2, 3, 4, 5, 6, 8, 16
NCV = 24

CM_ID, CM_DPOS, CM_DNEG, CM_MF, CM_MB, CM_LTRI, CM_ONES, CM_NP1, CM_NREV, CM_BLK64, CM_IOTA, CM_PCOL, CM_PREV, CM_SEL = (
    0, 128, 256, 384, 512, 640, 768, 896, 1024, 1152, 1280, 1312, 1313, 1314)
NCM = 1314 + 256


def build_consts():
    cm = np.zeros((128, NCM), np.float32)
    m = np.arange(128)[:, None].astype(np.float32)
    n = np.arange(128)[None, :].astype(np.float32)
    cm[:, CM_ID:CM_ID + 128] = np.eye(128)
    cm[:, CM_DPOS:CM_DPOS + 128] = np.maximum(n - m, 0)
    cm[:, CM_DNEG:CM_DNEG + 128] = np.maximum(m - n, 0)
    cm[:, CM_MF:CM_MF + 128] = (n >= m)
    cm[:, CM_MB:CM_MB + 128] = (m >= n)
    cm[:, CM_LTRI:CM_LTRI + 128] = (m < n)
    cm[:, CM_ONES:CM_ONES + 128] = 1.0
    cm[:, CM_NP1:CM_NP1 + 128] = n + 1.0
    cm[:, CM_NREV:CM_NREV + 128] = 128.0 - n
    cm[:, CM_BLK64:CM_BLK64 + 128] = ((np.arange(128)[:, None] // 64) == (np.arange(128)[None, :] // 64)) / 64.0
    cm[:, CM_IOTA:CM_IOTA + 32] = np.arange(32)[None, :]
    cm[:, CM_PCOL] = np.arange(128)
    cm[:, CM_PREV] = 127.0 - np.arange(128)
    cm[0, CM_SEL:CM_SEL + 128] = 1.0
    cm[1, CM_SEL + 128:CM_SEL + 256] = 1.0
    rows = NLAT // GRID_W
    row = np.repeat(np.arange(rows), GRID_W).astype(np.float32)
    colp = np.tile(np.arange(GRID_W), rows).astype(np.float32)
    tabs = np.zeros((4, 128, T), np.float32)
    for ti, rot in ((0, 64), (2, 32)):
        nf = rot // 4
        half = rot // 2
        inv = (10000.0 ** (-np.arange(nf, dtype=np.float32) / nf)).astype(np.float32)
        ang = np.concatenate([row[:, None] * inv, colp[:, None] * inv], axis=-1).astype(np.float32)
        cos = np.cos(ang).astype(np.float32)
        sin = np.sin(ang).astype(np.float32)
        f = np.arange(128)
        j = f % half
        sign = np.where((f % rot) < half, -1.0, 1.0).astype(np.float32)
        tabs[ti, :, :NCTX] = 1.0
        tabs[ti, :, NCTX:] = cos[:, j].T
        tabs[ti + 1, :, NCTX:] = (sin[:, j] * sign[None, :]).T
    return cm, tabs


def prep_layer_arrays(inp, ls):
    f = lambda k: np.asarray(inp[k], np.float32)
    w_in = f("w_in")[ls]
    L = len(ls)
    out = {}
    out["wext"] = np.ascontiguousarray(w_in[:, :, WEXT_IDX])
    out["wuq"] = np.ascontiguousarray(f("mla_w_uq")[ls][:, :, WUQ_IDX])
    out["wukv"] = np.ascontiguousarray(f("mla_w_ukv")[ls][:, :, WUKV_IDX])
    out["adaw"] = np.ascontiguousarray(f("ada_w")[ls])
    out["wout"] = np.ascontiguousarray(f("w_out")[ls])
    out["wr"] = np.ascontiguousarray(f("router_w")[ls])
    out["w1"] = np.ascontiguousarray(f("exp_w1")[ls])
    out["w2"] = np.ascontiguousarray(f("exp_w2")[ls])
    out["b2"] = np.ascontiguousarray(f("exp_b2")[ls])
    b1 = f("exp_b1")[ls]
    b1 = b1.reshape(L, NEXP, 8, 128, 2).transpose(0, 1, 3, 2, 4).reshape(L, NEXP, 128, 16)
    out["b1c"] = np.ascontiguousarray(b1)
    rv = np.zeros((L, NRV), np.float32)
    cv = np.zeros((L, 128, NCV), np.float32)
    ng = f("norm_g")[ls]
    for i, l in enumerate(ls):
        rv[i, RV_G1:RV_G1 + 1024] = ng[i, 1]
        rv[i, RV_G3:RV_G3 + 1024] = ng[i, 3]
        rv[i, RV_G0:RV_G0 + 1024] = ng[i, 0]
        rv[i, RV_G2:RV_G2 + 1024] = ng[i, 2]
        rv[i, RV_GNW:RV_GNW + 256] = f("ret_gn_w")[l]
        rv[i, RV_GNB:RV_GNB + 256] = f("ret_gn_b")[l]
        rv[i, RV_SUBLN:RV_SUBLN + 64] = f("diff_subln")[l]
        rv[i, RV_ADAB:RV_ADAB + 6144] = f("ada_b")[l]
        rv[i, RV_RB:RV_RB + 32] = f("router_b")[l]
        rv[i, RV_DECAY:RV_DECAY + 8] = f("ret_log_decay")[l].reshape(-1)
        rv[i, RV_LAM:RV_LAM + 128] = f("diff_lambda")[l].reshape(-1)
        rv[i, RV_LAMINIT] = 0.8 - 0.6 * math.exp(-0.3 * l)
        qk = f("gqa_qk_norm")[l]
        sw = _swap_blocks(np.arange(64), 64)
        cv[i, :, CV_GQG] = np.tile(qk[0], 2)
        cv[i, :, CV_GQGS] = np.tile(qk[0][sw], 2)
        cv[i, :, CV_GKG] = np.tile(qk[1], 2)
        cv[i, :, CV_GKGS] = np.tile(qk[1][sw], 2)
        qn = f("mla_q_norm")[l]
        cv[i, :, CV_QN0] = qn[:128]
        cv[i, :64, CV_QN1] = qn[128:]
        cv[i, :, CV_KVG] = f("mla_kv_norm")[l]
        cv[i, :, CV_G0:CV_G0 + 8] = _col(ng[i, 0])
        cv[i, :, CV_G2:CV_G2 + 8] = _col(ng[i, 2])
    out["rowv"] = rv
    out["colv"] = cv
    return out


class Prog:
    def __init__(self, L, debug=(), as_input=()):
        self.L = L
        self.debug = set(debug)
        self.as_input = set(as_input)
        nc = self.nc = bass.Bass("TRN2", target_bir_lowering=False)
        di = lambda n, s, d=F32: nc.dram_tensor(n, list(s), d, kind="ExternalInput").ap()
        self.xin = di("xin", [T, D])
        self.cvec = di("cvec", [128, 16])
        self.cmat = di("cmat", [128, NCM])
        self.ropet = di("ropet", [4, 128, T])
        self.adaw = di("adaw", [L, D, 6 * D])
        self.rowv = di("rowv", [L, NRV])
        self.colv = di("colv", [L, 128, NCV])
        self.wext = di("wext", [L, D, NWEXT])
        self.wuq = di("wuq", [L, 192, 512])
        self.wukv = di("wukv", [L, 128, 512])
        self.wout = di("wout", [L, D, D])
        self.wr = di("wr", [L, D, NEXP])
        self.w1 = di("w1", [L, NEXP, D, 2 * D])
        self.b1c = di("b1c", [L, NEXP, 128, 16])
        self.w2 = di("w2", [L, NEXP, D, D])
        self.b2 = di("b2", [L, NEXP, D])
        self.yout = nc.dram_tensor("yout", [T, D], F32, kind="ExternalOutput").ap()
        self.dbg = {}
        sc = lambda n, s, d: self._scratch(n, s, d)
        self.xres = sc("xres", [T, D], F32)
        self.fm = sc("fm", [17, 128, T], BF16)
        self.tokv = sc("tokv", [T, 1152], BF16)
        self.mixd = sc("mixd", [T, D], BF16)
        self.xg = sc("xg", [NSLOT + 128, D], BF16)
        self.yg = sc("yg", [NSLOT + 128, D], F32)
        self.modd = sc("modd", [L, 2, 6 * D], F32)

    def _scratch(self, n, s, d):
        if n in getattr(self, "as_input", ()):
            return self.nc.dram_tensor(n, list(s), d, kind="ExternalInput").ap()
        kind = "ExternalOutput" if n in self.debug else "Internal"
        t = self.nc.dram_tensor(n, list(s), d, kind=kind).ap()
        if n in self.debug:
            self.dbg[n] = t
        return t


FM = dict(rq0=0, rq1=1, rk0=2, rk1=3, dq0=4, dq1=5, dk0=6, dk1=7, gq0=8, gq1=9, gk=10,
          mqn0=11, mqn1=12, mqr=13, mkn0=14, mkn1=15, mkr=16)
TV_RV, TV_RG, TV_DV, TV_GV, TV_MV = 0, 256, 512, 768, 896


ATT_GROUPS = ("diff", "gqa", "mla")
RET_STOP = int(os.environ.get("RET_STOP", "0"))
ATT_MAXMAPS = None


def build_program(L, debug=(), stop_after=None, as_input=(), start_at=None):
    P = Prog(L, debug, as_input)
    nc = P.nc
    with ExitStack() as top:
        S = Sched(nc, top)
        uid = [0]

        def sbt(st, n, s, d):
            uid[0] += 1
            return st.enter_context(nc.sbuf_tensor("%s_s%d" % (n, uid[0]), list(s), d))

        def pst(st, n, s, d):
            uid[0] += 1
            return st.enter_context(nc.psum_tensor("%s_p%d" % (n, uid[0]), list(s), d))
        cm = sbt(top, "cm", [128, NCM], F32)
        cmb = sbt(top, "cmb", [128, NCM], BF16)
        cvec = sbt(top, "cvec", [128, 16], F32)
        R_cm, R_cmb, R_cvec, R_hT = Res("cm"), Res("cmb"), Res("cvec"), Res("hT")
        S.dma("sp", cm[:], P.cmat[:, :], writes=[R_cm])
        S.dma("pool", cmb[:], P.cmat[:, :], writes=[R_cmb])
        S.dma("sp", cvec[:], P.cvec[:, :], writes=[R_cvec])
        R_xres = [Res("xres%d" % i) for i in range(NT)]
        for i in range(NT):
            S.dma("sp" if i % 2 else "act", P.xres[i * 128:(i + 1) * 128, :], P.xin[i * 128:(i + 1) * 128, :],
                  writes=[R_xres[i]])
        with ExitStack() as zst:
            zt = sbt(zst, "zt", [128, 8 * D], BF16)
            R_z = Res()
            S.op("pool", lambda e: e.memset(zt[:], 0.0), writes=[R_z])
            for j in range(NSLOT // 1024):
                S.dma("sp" if j % 2 else "act", P.xg[j * 1024:(j + 1) * 1024, :].rearrange("(p a) d -> p (a d)", a=8), zt[:], reads=[R_z], writes=[Res()])
            ztf = sbt(zst, "ztf", [128, D], F32)
            S.op("pool", lambda e: e.memset(ztf[:], 0.0), writes=[R_z])
            S.dma("sp", P.yg[NSLOT:NSLOT + 128, :], ztf[:], reads=[R_z], writes=[Res()])
            S.drain()
        S.op("act", lambda e: e.activation(out=cvec[:], in_=cvec[:], func=AF.Silu), reads=[R_cvec], writes=[R_cvec])
        S.drain()
        ctx = dict(P=P, S=S, sbt=sbt, pst=pst, cm=cm, cmb=cmb, cvec=cvec, hT=None, R_cm=R_cm, R_cmb=R_cmb,
                   R_cvec=R_cvec, R_hT=R_hT, R_xres=R_xres, nc=nc)
        ctx["start_at"] = start_at
        for l in range(L):
            layer(ctx, l, first=(l == 0), stop_after=stop_after)
            if stop_after:
                break
        if not stop_after:
            R_out = Res("yout")
            for i in range(NT):
                S.dma("sp" if i % 2 else "act", P.yout[i * 128:(i + 1) * 128, :], P.xres[i * 128:(i + 1) * 128, :],
                      reads=[R_xres[i]], writes=[R_out] if i == 0 else [Res("o%d" % i)])
        S.drain()
    return P


def bc(ap, parts):
    return ap.to_broadcast([parts, ap.shape[-1]])


def layer(c, l, first, stop_after=None):
    P, S, nc = c["P"], c["S"], c["nc"]
    sa = c.get("start_at")
    phase_adaln(c, l)
    S.drain()
    if stop_after == "A":
        return
    if sa is None:
        with ExitStack() as hst:
            c["hT"] = c["sbt"](hst, "hT", [128, 8, T], BF16)
            phase_modulate(c, l, which="a")
            S.drain()
            phase_proj(c, l)
            S.drain()
        c["hT"] = None
    if stop_after == "C":
        return
    if sa in (None, "R"):
        phase_retention(c, l)
        S.drain()
    if stop_after == "R":
        return
    if sa in (None, "R", "AT"):
        phase_attention(c, l)
    S.drain()
    if stop_after == "AT":
        return
    phase_outproj(c, l)
    S.drain()
    if stop_after == "E":
        return
    with ExitStack() as st:
        rt = phase_modulate(c, l, which="f", keep=st)
        S.drain()
        phase_experts(c, l)
        S.drain()
        if stop_after == "F":
            return
        phase_combine(c, l, rt)
        S.drain()


def phase_adaln(c, l):
    P, S, nc, sbt, pst = c["P"], c["S"], c["nc"], c["sbt"], c["pst"]
    cvec, R_cvec = c["cvec"], c["R_cvec"]
    with ExitStack() as st:
        aw = [sbt(st, "aw%d" % i, [128, 8, 512], F32) for i in range(2)]
        R_aw = [Res("aw0"), Res("aw1")]
        msb = sbt(st, "msb", [2, 6 * D], F32)
        adab = sbt(st, "adab", [2, 6 * D], F32)
        pm = [pst(st, "pm%d" % i, [2, 512], F32) for i in range(2)]
        R_pm = [Res("pm0"), Res("pm1")]
        R_msb, R_adab = Res("msb"), Res("adab")
        S.dma("act", adab[:], bc(P.rowv[l:l + 1, RV_ADAB:RV_ADAB + 6 * D], 2), writes=[R_adab])
        src = P.adaw[l].rearrange("(c p) n -> p c n", p=128)
        for j in range(12):
            b = j % 2
            S.dma("sp", aw[b][:], src[:, :, j * 512:(j + 1) * 512], writes=[R_aw[b]])
            for h0 in (0, 256):
                for k in range(8):
                    S.op("pe", lambda e, k=k, b=b, h0=h0: e.matmul(pm[b][:, h0:h0 + 256], lhsT=cvec[:, k::8], rhs=aw[b][:, k, h0:h0 + 256],
                                                                 start=(k == 0), stop=(k == 7)),
                         reads=[R_cvec, R_aw[b]], writes=[R_pm[b]])
            S.op("dve", lambda e, j=j, b=b: e.tensor_tensor(out=msb[:, j * 512:(j + 1) * 512], in0=pm[b][:],
                                                           in1=adab[:, j * 512:(j + 1) * 512], op=ALU.add),
                 reads=[R_pm[b], R_adab], writes=[R_msb])
        R_modd = c.setdefault("R_modd", {})
        R_modd[l] = Res("modd%d" % l)
        S.dma("sp", P.modd[l], msb[:], reads=[R_msb], writes=[R_modd[l]])
        S.drain()


def load_mod_bcast(c, l, st, part_sc, part_sh, rv_g, name):
    P, S, sbt = c["P"], c["S"], c["sbt"]
    gsc = sbt(st, "gsc" + name, [128, 2, D], F32)
    sh = sbt(st, "sh" + name, [128, 2, D], F32)
    gb = sbt(st, "gb" + name, [128, D], F32)
    R1, R2, R3 = Res("gsc"), Res("sh"), Res("gb")
    S.dma("sp", gb[:], bc(P.rowv[l:l + 1, rv_g:rv_g + D], 128), writes=[R3])
    for r in range(2):
        S.dma("sp", gsc[:, r, :], bc(P.modd[l, r:r + 1, part_sc * D:(part_sc + 1) * D], 128),
              reads=[c["R_modd"][l]], writes=[R1])
        S.dma("act", sh[:, r, :], bc(P.modd[l, r:r + 1, part_sh * D:(part_sh + 1) * D], 128),
              reads=[c["R_modd"][l]], writes=[R2])
        S.op("dve", lambda e, r=r: e.scalar_tensor_tensor(out=gsc[:, r, :], in0=gsc[:, r, :], scalar=1.0, in1=gb[:],
                                                         op0=ALU.add, op1=ALU.mult),
             reads=[R1, R3], writes=[R1])
    return gsc, sh, R1, R2


def load_gate_bcast(c, l, st, part_g, rv_g, name):
    P, S, sbt = c["P"], c["S"], c["sbt"]
    G = sbt(st, "G" + name, [128, 2, D], F32)
    gb = sbt(st, "Gb" + name, [128, D], F32)
    R1, R3 = Res("G"), Res("Gb")
    S.dma("sp", gb[:], bc(P.rowv[l:l + 1, rv_g:rv_g + D], 128), writes=[R3])
    for r in range(2):
        S.dma("act", G[:, r, :], bc(P.modd[l, r:r + 1, part_g * D:(part_g + 1) * D], 128),
              reads=[c["R_modd"][l]], writes=[R1])
        S.op("dve", lambda e, r=r: e.tensor_tensor(out=G[:, r, :], in0=G[:, r, :], in1=gb[:], op=ALU.mult),
             reads=[R1, R3], writes=[R1])
    return G, R1


def rms_rstd(S, e_reads, x_ap, n, junk, R_junk, ss, R_ss, rstd, R_rstd):
    S.op("act", lambda e: e.activation(out=junk, in_=x_ap, func=AF.Square, accum_out=ss),
         reads=e_reads, writes=[R_junk, R_ss])
    S.op("dve", lambda e: e.tensor_scalar(out=rstd, in0=ss, scalar1=1.0 / n, scalar2=EPS, op0=ALU.mult, op1=ALU.add),
         reads=[R_ss], writes=[R_rstd])
    S.op("act", lambda e: e.activation(out=rstd, in_=rstd, func=AF.Sqrt), reads=[R_rstd], writes=[R_rstd])
    S.op("dve", lambda e: e.reciprocal(out=rstd, in_=rstd), reads=[R_rstd], writes=[R_rstd])


def phase_modulate(c, l, which, keep=None):
    P, S, nc, sbt, pst = c["P"], c["S"], c["nc"], c["sbt"], c["pst"]
    cm, R_cm, hT, R_hT, R_xres = c["cm"], c["R_cm"], c["hT"], c["R_hT"], c["R_xres"]
    route = which == "f"
    rt = None
    if route:
        rt = dict()
        rt["gates"] = sbt(keep, "gatesall", [128, NT, 4], F32)
        rt["slots"] = sbt(keep, "slotsall", [128, NT, 4], U32)
        rt["R_gates"] = Res(); rt["R_slots"] = Res()
    with ExitStack() as st:
        if which == "a":
            gsc, sh, R_gsc, R_sh = load_mod_bcast(c, l, st, 1, 0, RV_G0, "a")
        else:
            gsc, sh, R_gsc, R_sh = load_mod_bcast(c, l, st, 4, 3, RV_G2, "f")
        NB = 2
        xt = [sbt(st, "xt%d" % i, [128, D], F32) for i in range(NB)]
        hx = [sbt(st, "hx%d" % i, [128, D], F32) for i in range(NB)]
        junk = sbt(st, "junk", [128, D], F32)
        ss = [sbt(st, "ss%d" % i, [128, 1], F32) for i in range(NB)]
        rstd = [sbt(st, "rstd%d" % i, [128, 1], F32) for i in range(NB)]
        ptr = [pst(st, "ptr%d" % i, [128, 8, 128], F32) for i in range(NB)]
        R_xt = [Res() for _ in range(NB)]; R_hx = [Res() for _ in range(NB)]; R_junk = Res()
        R_ss = [Res() for _ in range(NB)]; R_rstd = [Res() for _ in range(NB)]; R_ptr = [Res() for _ in range(NB)]
        if route:
            h32 = [sbt(st, "h32_%d" % i, [128, 8, 128], F32) for i in range(NB)]
            hb = [sbt(st, "hb%d" % i, [128, D], BF16) for i in range(NB)]
            R_h32 = [Res() for _ in range(NB)]; R_hb = [Res() for _ in range(NB)]
            wr = sbt(st, "wr", [128, 8, NEXP], F32)
            rb = sbt(st, "rb", [1, NEXP], F32)
            R_wr, R_rb = Res(), Res()
            S.dma("sp", wr[:], P.wr[l].rearrange("(c p) n -> p c n", p=128), writes=[R_wr])
            S.dma("sp", rb[:], P.rowv[l:l + 1, RV_RB:RV_RB + NEXP], writes=[R_rb])
            plg = [pst(st, "plg%d" % i, [128, NEXP], F32) for i in range(NB)]
            ppos = [pst(st, "ppos%d" % i, [128, NEXP], F32) for i in range(NB)]
            R_plg = [Res() for _ in range(NB)]; R_ppos = [Res() for _ in range(NB)]
            lg = sbt(st, "lg", [128, NEXP], F32); m8 = sbt(st, "m8", [128, 8], F32); i8 = sbt(st, "i8", [128, 8], U32)
            idxf = sbt(st, "idxf", [128, 4], F32); negm = sbt(st, "negm", [128, 1], F32)
            e4 = sbt(st, "e4", [128, 4], F32); se = sbt(st, "se", [128, 1], F32)
            oh = sbt(st, "oh", [128, 4, NEXP], F32); mask = sbt(st, "mask", [128, NEXP], F32)
            cum = sbt(st, "cum", [128, NEXP], F32); posk = sbt(st, "posk", [128, 4], F32)
            j32 = sbt(st, "j32", [128, NEXP], F32); slf = sbt(st, "slf", [128, 4], F32); valid = sbt(st, "valid", [128, 4], F32)
            R_r = Res("route_tmp"); R_cum = Res("cum"); R_mask = Res("mask")
            S.op("pool", lambda e: e.memset(cum[:], 0.0), writes=[R_cum])
            R_xg = c.setdefault("R_xg", Res("xg"))
            rt["R_scatter"] = []
        for i in range(NT):
            b = i % NB
            r = 1 if i < 2 else 0
            S.dma("sp" if i % 2 else "act", xt[b][:], P.xres[i * 128:(i + 1) * 128, :], reads=[R_xres[i]], writes=[R_xt[b]])
            rms_rstd(S, [R_xt[b]], xt[b][:], D, junk[:], R_junk, ss[b][:], R_ss[b], rstd[b][:], R_rstd[b])
            S.op("dve", lambda e, b=b, r=r: e.scalar_tensor_tensor(out=hx[b][:], in0=xt[b][:], scalar=rstd[b][:, 0:1],
                                                                   in1=gsc[:, r, :], op0=ALU.mult, op1=ALU.mult),
                 reads=[R_xt[b], R_rstd[b], R_gsc], writes=[R_hx[b]])
            S.op("pool", lambda e, b=b, r=r: e.tensor_tensor(out=hx[b][:], in0=hx[b][:], in1=sh[:, r, :], op=ALU.add),
                 reads=[R_hx[b], R_sh], writes=[R_hx[b]])
            for k in range(8):
                S.op("pe", lambda e, b=b, k=k: e.transpose(out=ptr[b][:, k, :], in_=hx[b][:, k * 128:(k + 1) * 128],
                                                          identity=cm[:, CM_ID:CM_ID + 128]),
                     reads=[R_hx[b], R_cm], writes=[R_ptr[b]])
            if not route:
                S.op("act", lambda e, b=b, i=i: e.activation(out=hT[:, :, i * 128:(i + 1) * 128], in_=ptr[b][:], func=AF.Copy),
                     reads=[R_ptr[b]], writes=[R_hT])
                continue
            S.op("act", lambda e, b=b: e.activation(out=h32[b][:], in_=ptr[b][:], func=AF.Copy),
                 reads=[R_ptr[b]], writes=[R_h32[b]])
            S.op("pool", lambda e, b=b: e.tensor_copy(out=hb[b][:], in_=hx[b][:]), reads=[R_hx[b]], writes=[R_hb[b]])
            for k in range(8):
                S.op("pe", lambda e, b=b, k=k: e.matmul(plg[b][:], lhsT=h32[b][:, k, :], rhs=wr[:, k, :], start=(k == 0), stop=False),
                     reads=[R_h32[b], R_wr], writes=[R_plg[b]])
            S.op("pe", lambda e, b=b: e.matmul(plg[b][:], lhsT=cm[0:1, CM_ONES:CM_ONES + 128], rhs=rb[:], start=False, stop=True),
                 reads=[R_cm, R_rb], writes=[R_plg[b]])
            gates_i = rt["gates"][:, i, :]
            S.op("dve", lambda e, b=b: e.tensor_copy(out=lg[:], in_=plg[b][:]), reads=[R_plg[b]], writes=[R_r])
            S.op("dve", lambda e: e.max(out=m8[:], in_=lg[:]), reads=[R_r], writes=[R_r])
            S.op("dve", lambda e: e.max_index(out=i8[:], in_max=m8[:], in_values=lg[:]), reads=[R_r], writes=[R_r])
            S.op("dve", lambda e: e.tensor_scalar(out=negm[:], in0=m8[:, 0:1], scalar1=-1.0, scalar2=None, op0=ALU.mult),
                 reads=[R_r], writes=[R_r])
            S.op("act", lambda e: e.activation(out=e4[:], in_=m8[:, 0:4], func=AF.Exp, bias=negm[:, 0:1], accum_out=se[:]),
                 reads=[R_r], writes=[R_r])
            S.op("dve", lambda e: e.reciprocal(out=se[:], in_=se[:]), reads=[R_r], writes=[R_r])
            S.op("dve", lambda e, g=gates_i: e.tensor_scalar(out=g, in0=e4[:], scalar1=se[:, 0:1], scalar2=None, op0=ALU.mult),
                 reads=[R_r], writes=[rt["R_gates"]])
            S.op("dve", lambda e: e.tensor_copy(out=idxf[:], in_=i8[:, 0:4]), reads=[R_r], writes=[R_r])
            for k in range(4):
                S.op("dve", lambda e, k=k: e.tensor_scalar(out=oh[:, k, :], in0=cm[:, CM_IOTA:CM_IOTA + NEXP], scalar1=idxf[:, k:k + 1],
                                                          scalar2=None, op0=ALU.is_equal),
                     reads=[R_r, R_cm], writes=[R_r])
            S.op("dve", lambda e: e.tensor_tensor(out=mask[:], in0=oh[:, 0, :], in1=oh[:, 1, :], op=ALU.add), reads=[R_r], writes=[R_mask])
            S.op("dve", lambda e: e.tensor_tensor(out=mask[:], in0=mask[:], in1=oh[:, 2, :], op=ALU.add), reads=[R_r, R_mask], writes=[R_mask])
            S.op("dve", lambda e: e.tensor_tensor(out=mask[:], in0=mask[:], in1=oh[:, 3, :], op=ALU.add), reads=[R_r, R_mask], writes=[R_mask])
            S.op("pe", lambda e, b=b: e.matmul(ppos[b][:], lhsT=cm[:, CM_LTRI:CM_LTRI + 128], rhs=mask[:], start=True, stop=False),
                 reads=[R_cm, R_mask], writes=[R_ppos[b]])
            S.op("pe", lambda e, b=b: e.matmul(ppos[b][:], lhsT=cm[:, CM_ONES:CM_ONES + 128], rhs=cum[:], start=False, stop=True),
                 reads=[R_cm, R_cum], writes=[R_ppos[b]])
            S.op("dve", lambda e: e.tensor_tensor(out=cum[:], in0=cum[:], in1=mask[:], op=ALU.add), reads=[R_mask, R_cum], writes=[R_cum])
            for k in range(4):
                S.op("dve", lambda e, b=b, k=k: e.tensor_tensor(out=j32[:], in0=oh[:, k, :], in1=ppos[b][:], op=ALU.mult),
                     reads=[R_r, R_ppos[b]], writes=[R_r])
                S.op("dve", lambda e, k=k: e.tensor_reduce(out=posk[:, k:k + 1], in_=j32[:], axis=AX.X, op=ALU.add),
                     reads=[R_r], writes=[R_r])
            S.op("dve", lambda e: e.scalar_tensor_tensor(out=slf[:], in0=idxf[:], scalar=float(CAP), in1=posk[:], op0=ALU.mult, op1=ALU.add),
                 reads=[R_r], writes=[R_r])
            S.op("dve", lambda e: e.tensor_scalar(out=valid[:], in0=posk[:], scalar1=float(CAP), scalar2=None, op0=ALU.is_lt), reads=[R_r], writes=[R_r])
            S.op("dve", lambda e: e.tensor_scalar(out=slf[:], in0=slf[:], scalar1=-float(NSLOT), scalar2=None, op0=ALU.add), reads=[R_r], writes=[R_r])
            S.op("dve", lambda e: e.tensor_tensor(out=slf[:], in0=slf[:], in1=valid[:], op=ALU.mult), reads=[R_r], writes=[R_r])
            S.op("dve", lambda e: e.tensor_scalar(out=slf[:], in0=slf[:], scalar1=float(NSLOT), scalar2=None, op0=ALU.add), reads=[R_r], writes=[R_r])
            S.op("dve", lambda e, g=gates_i: e.tensor_tensor(out=g, in0=g, in1=valid[:], op=ALU.mult), reads=[R_r, rt["R_gates"]], writes=[rt["R_gates"]])
            S.op("dve", lambda e, i=i: e.tensor_copy(out=rt["slots"][:, i, :], in_=slf[:]), reads=[R_r], writes=[rt["R_slots"]])
            for k in range(4):
                Rs = Res("sc")
                rt["R_scatter"].append(Rs)
                S.dma("pool", P.xg[:, :], hb[b][:], reads=[R_hb[b], rt["R_slots"]], writes=[Rs],
                      indirect=dict(out_offset=bass.IndirectOffsetOnAxis(ap=rt["slots"][:, i, k:k + 1], axis=0), in_offset=None))
        S.drain()
    return rt


def phase_proj(c, l):
    P, S, nc, sbt, pst = c["P"], c["S"], c["nc"], c["sbt"], c["pst"]
    cm, R_cm, hT, R_hT, cmb = c["cm"], c["R_cm"], c["hT"], c["R_hT"], c["cmb"]
    R_fm = c.setdefault("R_fm", Res("fm")); R_tokv = c.setdefault("R_tokv", Res("tokv"))
    with ExitStack() as st:
        sq = [sbt(st, "sq%d" % i, [128, 512], BF16) for i in range(2)]
        rs = [sbt(st, "rs%d" % i, [128, 512], F32) for i in range(2)]
        R_sq = [Res(), Res()]; R_rs = [Res(), Res()]
        cqn0 = sbt(st, "cqn0", [128, 512], BF16); cqn1 = sbt(st, "cqn1", [64, 512], BF16); ckvn = sbt(st, "ckvn", [128, 512], BF16)
        R_cqn, R_ckvn = Res(), Res()
        wx = sbt(st, "wx", [128, 8, NWEXT], BF16)
        wuqA = sbt(st, "wuqA", [128, 512], BF16); wuqB = sbt(st, "wuqB", [64, 512], BF16)
        wukv = sbt(st, "wukvx", [128, 512], BF16)
        colv = sbt(st, "colv", [128, NCV], F32)
        R_wx, R_w2, R_colv = Res("wx"), Res("w2nd"), Res("colv")
        src = P.wext[l].rearrange("(c p) n -> p c n", p=128)
        for k in range(8):
            for (a, b) in ((0, 2048), (2048, 4096), (4096, NWEXT)):
                S.dma("pool", wx[:, k, a:b], src[:, k, a:b], writes=[R_wx])
        S.dma("pool", wuqA[:], P.wuq[l, 0:128, :], writes=[R_w2])
        S.dma("pool", wuqB[:], P.wuq[l, 128:192, :], writes=[R_w2])
        S.dma("pool", wukv[:], P.wukv[l], writes=[R_w2])
        S.dma("sp", colv[:], P.colv[l], writes=[R_colv])
        tabs = [sbt(st, "tabs%d" % i, [128, 4, 512], F32) for i in range(2)]
        R_tabs = [Res(), Res()]
        NPS = 2
        pa = [pst(st, "pa%d" % i, [128, 512], F32) for i in range(NPS)]
        pb = [pst(st, "pb%d" % i, [128, 512], F32) for i in range(NPS)]
        pc = [pst(st, "pc%d" % i, [128, 512], F32) for i in range(NPS)]
        pd = [pst(st, "pd%d" % i, [128, 512], F32) for i in range(NPS)]
        R_pa = [Res() for _ in range(NPS)]; R_pb = [Res() for _ in range(NPS)]
        R_pc = [Res() for _ in range(NPS)]; R_pd = [Res() for _ in range(NPS)]
        NW = 3
        t1 = [sbt(st, "t1_%d" % i, [128, 512], F32) for i in range(NW)]
        t2 = [sbt(st, "t2_%d" % i, [128, 512], F32) for i in range(NW)]
        ob = [sbt(st, "ob%d" % i, [128, 512], BF16) for i in range(NW)]
        R_t1 = [Res() for _ in range(NW)]; R_t2 = [Res() for _ in range(NW)]; R_ob = [Res() for _ in range(NW)]
        tvs = [sbt(st, "tvs%d" % i, [128, 1152], BF16) for i in range(2)]
        R_tvs = [Res(), Res()]
        cnt = dict(w=0, a=0, b=0, c=0, d=0, ev=0, q=0)

        def nxt(key, n):
            v = cnt[key]; cnt[key] = (v + 1) % n
            return v

        def group(ps, R_ps, u, tok0, W, M=128, wtile=None, rhs=None, R_rhs=None):
            for k in range(8):
                S.op("pe", lambda e, k=k: e.matmul(ps[0:M, 0:W], lhsT=wx[:, k, u * 128:u * 128 + M], rhs=hT[:, k, tok0:tok0 + W],
                                                   start=(k == 0), stop=(k == 7)),
                     reads=[R_wx, R_hT], writes=[R_ps])

        def store_fm(name, src_tile, R_src, tok0, W, M=128):
            q = ("sp", "act")[nxt("q", 2)]
            S.dma(q, P.fm[FM[name], 0:M, tok0:tok0 + W], src_tile[0:M, 0:W], reads=[R_src], writes=[Res()])

        def rope_out(name, p1, R_p1, p2, R_p2, tb, tcos, tok0, W, g=None, gs=None, rstd=None, R_rstd=None):
            w = nxt("w", NW)
            cos = tabs[tb][:, tcos, 0:W]; sin = tabs[tb][:, tcos + 1, 0:W]
            if g is None:
                S.op("dve", lambda e: e.tensor_tensor(out=t1[w][:, 0:W], in0=p1[:, 0:W], in1=cos, op=ALU.mult),
                     reads=[R_p1, R_tabs[tb]], writes=[R_t1[w]])
                S.op("dve", lambda e: e.tensor_tensor(out=t2[w][:, 0:W], in0=p2[:, 0:W], in1=sin, op=ALU.mult),
                     reads=[R_p2, R_tabs[tb]], writes=[R_t2[w]])
                S.op("pool", lambda e: e.tensor_tensor(out=ob[w][:, 0:W], in0=t1[w][:, 0:W], in1=t2[w][:, 0:W], op=ALU.add),
                     reads=[R_t1[w], R_t2[w]], writes=[R_ob[w]])
            else:
                S.op("dve", lambda e: e.scalar_tensor_tensor(out=t1[w][:, 0:W], in0=p1[:, 0:W], scalar=g, in1=cos, op0=ALU.mult, op1=ALU.mult),
                     reads=[R_p1, R_tabs[tb], R_colv], writes=[R_t1[w]])
                S.op("dve", lambda e: e.scalar_tensor_tensor(out=t2[w][:, 0:W], in0=p2[:, 0:W], scalar=gs, in1=sin, op0=ALU.mult, op1=ALU.mult),
                     reads=[R_p2, R_tabs[tb], R_colv], writes=[R_t2[w]])
                S.op("pool", lambda e: e.tensor_tensor(out=t1[w][:, 0:W], in0=t1[w][:, 0:W], in1=t2[w][:, 0:W], op=ALU.add),
                     reads=[R_t1[w], R_t2[w]], writes=[R_t1[w]])
                S.op("pool", lambda e: e.tensor_tensor(out=ob[w][:, 0:W], in0=t1[w][:, 0:W], in1=rstd[:, 0:W], op=ALU.mult),
                     reads=[R_t1[w], R_rstd], writes=[R_ob[w]])
            store_fm(name, ob[w], R_ob[w], tok0, W)

        def rstd_from_ms(ps, R_ps, scale, si, W, M=128):
            S.op("dve", lambda e: e.tensor_scalar(out=rs[si][0:M, 0:W], in0=ps[0:M, 0:W], scalar1=scale, scalar2=EPS, op0=ALU.mult, op1=ALU.add),
                 reads=[R_ps], writes=[R_rs[si]])
            S.op("act", lambda e: e.activation(out=rs[si][0:M, 0:W], in_=rs[si][0:M, 0:W], func=AF.Sqrt), reads=[R_rs[si]], writes=[R_rs[si]])
            S.op("dve", lambda e: e.reciprocal(out=rs[si][0:M, 0:W], in_=rs[si][0:M, 0:W]), reads=[R_rs[si]], writes=[R_rs[si]])

        def do_block(bi, tok0, W):
            tb = bi % 2
            S.dma("sp", tabs[tb][:, :, 0:W], P.ropet[:, :, tok0:tok0 + W].rearrange("t p n -> p t n"), writes=[R_tabs[tb]])
            for name, u, tc in (("rq0", 0, 0), ("rq1", 1, 0), ("rk0", 2, 0), ("rk1", 3, 0),
                                ("dq0", 4, 2), ("dq1", 5, 2), ("dk0", 6, 2), ("dk1", 7, 2), ("mkr", 11, 2)):
                a = nxt("a", NPS); b = nxt("b", NPS)
                group(pa[a], R_pa[a], u, tok0, W)
                group(pb[b], R_pb[b], u + U_SW, tok0, W)
                rope_out(name, pa[a], R_pa[a], pb[b], R_pb[b], tb, tc, tok0, W)
            for name, u, cg in (("gq0", 8, CV_GQG), ("gq1", 9, CV_GQG), ("gk", 10, CV_GKG)):
                a = nxt("a", NPS); b = nxt("b", NPS); cc = nxt("c", NPS)
                group(pa[a], R_pa[a], u, tok0, W)
                group(pb[b], R_pb[b], u + U_SW, tok0, W)
                S.op("act", lambda e, a=a: e.activation(out=sq[0][:, 0:W], in_=pa[a][:, 0:W], func=AF.Square),
                     reads=[R_pa[a]], writes=[R_sq[0]])
                for h0 in range(0, W, 256):
                    S.op("pe", lambda e, cc=cc, h0=h0: e.matmul(pc[cc][:, h0:h0 + 256], lhsT=cmb[:, CM_BLK64:CM_BLK64 + 128], rhs=sq[0][:, h0:h0 + 256], start=True, stop=True),
                         reads=[R_cm, R_sq[0]], writes=[R_pc[cc]])
                if "dbgC" in P.debug and bi == 0 and name == "gq0":
                    dd = nc.dram_tensor("dbgC", [3, 128, 512], F32, kind="ExternalOutput").ap()
                    S.op("dve", lambda e, cc=cc: e.tensor_copy(out=t1[0][:], in_=pc[cc][:]), reads=[R_pc[cc]], writes=[R_t1[0]])
                    S.dma("sp", dd[1], t1[0][:], reads=[R_t1[0]], writes=[Res()])
                    S.op("dve", lambda e, a=a: e.tensor_copy(out=t2[0][:], in_=pa[a][:]), reads=[R_pa[a]], writes=[R_t2[0]])
                    S.dma("sp", dd[2], t2[0][:], reads=[R_t2[0]], writes=[Res()])
                rstd_from_ms(pc[cc], R_pc[cc], 1.0, 0, W)
                rope_out(name, pa[a], R_pa[a], pb[b], R_pb[b], tb, 0, tok0, W, g=colv[:, cg:cg + 1], gs=colv[:, cg + 1:cg + 2],
                         rstd=rs[0], R_rstd=R_rs[0])
            a = nxt("a", NPS); b = nxt("b", NPS); cc = nxt("c", NPS)
            group(pa[a], R_pa[a], U_CQ0, tok0, W)
            group(pb[b], R_pb[b], U_CQ1, tok0, W, M=64)
            S.op("act", lambda e, a=a: e.activation(out=sq[0][:, 0:W], in_=pa[a][:, 0:W], func=AF.Square), reads=[R_pa[a]], writes=[R_sq[0]])
            S.op("act", lambda e, b=b: e.activation(out=sq[1][0:64, 0:W], in_=pb[b][0:64, 0:W], func=AF.Square), reads=[R_pb[b]], writes=[R_sq[1]])
            for h0 in range(0, W, 256):
                S.op("pe", lambda e, cc=cc, h0=h0: e.matmul(pc[cc][:, h0:h0 + 256], lhsT=cmb[:, CM_ONES:CM_ONES + 128], rhs=sq[0][:, h0:h0 + 256], start=True, stop=False),
                     reads=[R_cm, R_sq[0]], writes=[R_pc[cc]])
                S.op("pe", lambda e, cc=cc, h0=h0: e.matmul(pc[cc][:, h0:h0 + 256], lhsT=cmb[0:64, CM_ONES:CM_ONES + 128], rhs=sq[1][0:64, h0:h0 + 256], start=False, stop=True),
                     reads=[R_cm, R_sq[1]], writes=[R_pc[cc]])
            rstd_from_ms(pc[cc], R_pc[cc], 1.0 / 192, 0, W)
            S.op("dve", lambda e, a=a: e.scalar_tensor_tensor(out=cqn0[:, 0:W], in0=pa[a][:, 0:W], scalar=colv[:, CV_QN0:CV_QN0 + 1], in1=rs[0][:, 0:W],
                                                             op0=ALU.mult, op1=ALU.mult), reads=[R_pa[a], R_rs[0], R_colv], writes=[R_cqn])
            S.op("dve", lambda e, b=b: e.scalar_tensor_tensor(out=cqn1[:, 0:W], in0=pb[b][0:64, 0:W], scalar=colv[0:64, CV_QN1:CV_QN1 + 1], in1=rs[0][0:64, 0:W],
                                                             op0=ALU.mult, op1=ALU.mult), reads=[R_pb[b], R_rs[0], R_colv], writes=[R_cqn])
            a = nxt("a", NPS); cc = nxt("c", NPS)
            group(pa[a], R_pa[a], U_CKV, tok0, W)
            S.op("act", lambda e, a=a: e.activation(out=sq[0][:, 0:W], in_=pa[a][:, 0:W], func=AF.Square), reads=[R_pa[a]], writes=[R_sq[0]])
            for h0 in range(0, W, 256):
                S.op("pe", lambda e, cc=cc, h0=h0: e.matmul(pc[cc][:, h0:h0 + 256], lhsT=cmb[:, CM_ONES:CM_ONES + 128], rhs=sq[0][:, h0:h0 + 256], start=True, stop=True),
                     reads=[R_cm, R_sq[0]], writes=[R_pc[cc]])
            rstd_from_ms(pc[cc], R_pc[cc], 1.0 / 128, 1, W)
            S.op("dve", lambda e, a=a: e.scalar_tensor_tensor(out=ckvn[:, 0:W], in0=pa[a][:, 0:W], scalar=colv[:, CV_KVG:CV_KVG + 1], in1=rs[1][:, 0:W],
                                                             op0=ALU.mult, op1=ALU.mult), reads=[R_pa[a], R_rs[1], R_colv], writes=[R_ckvn])

            def uq(ps, R_ps, c0):
                S.op("pe", lambda e: e.matmul(ps[:, 0:W], lhsT=wuqA[:, c0:c0 + 128], rhs=cqn0[:, 0:W], start=True, stop=False),
                     reads=[R_w2, R_cqn], writes=[R_ps])
                S.op("pe", lambda e: e.matmul(ps[:, 0:W], lhsT=wuqB[:, c0:c0 + 128], rhs=cqn1[:, 0:W], start=False, stop=True),
                     reads=[R_w2, R_cqn], writes=[R_ps])

            def plain_out(name, ps, R_ps):
                w = nxt("w", NW)
                if nxt("ev", 2):
                    S.op("act", lambda e: e.activation(out=ob[w][:, 0:W], in_=ps[:, 0:W], func=AF.Copy), reads=[R_ps], writes=[R_ob[w]])
                else:
                    S.op("dve", lambda e: e.tensor_copy(out=ob[w][:, 0:W], in_=ps[:, 0:W]), reads=[R_ps], writes=[R_ob[w]])
                store_fm(name, ob[w], R_ob[w], tok0, W)

            for name, c0 in (("mqn0", 0), ("mqn1", 128)):
                d = nxt("d", NPS)
                uq(pd[d], R_pd[d], c0)
                plain_out(name, pd[d], R_pd[d])
            a = nxt("a", NPS); b = nxt("b", NPS)
            uq(pa[a], R_pa[a], 256)
            uq(pb[b], R_pb[b], 384)
            rope_out("mqr", pa[a], R_pa[a], pb[b], R_pb[b], tb, 2, tok0, W)
            for name, c0 in (("mkn0", 0), ("mkn1", 128)):
                d = nxt("d", NPS)
                S.op("pe", lambda e, d=d, c0=c0: e.matmul(pd[d][:, 0:W], lhsT=wukv[:, c0:c0 + 128], rhs=ckvn[:, 0:W], start=True, stop=True),
                     reads=[R_w2, R_ckvn], writes=[R_pd[d]])
                plain_out(name, pd[d], R_pd[d])
            for j in range(W // 128):
                t0 = tok0 + j * 128
                v = (t0 // 128) % 2
                d = nxt("d", NPS); cc = nxt("c", NPS); a = nxt("a", NPS)
                for k in range(8):
                    S.op("pe", lambda e, k=k, d=d, t0=t0: e.matmul(pd[d][:, 0:512], lhsT=hT[:, k, t0:t0 + 128], rhs=wx[:, k, TOK_OFF:TOK_OFF + 512],
                                                                 start=(k == 0), stop=(k == 7)), reads=[R_wx, R_hT], writes=[R_pd[d]])
                for k in range(8):
                    S.op("pe", lambda e, k=k, cc=cc, t0=t0: e.matmul(pc[cc][:, 0:384], lhsT=hT[:, k, t0:t0 + 128], rhs=wx[:, k, TOK_OFF + 512:TOK_OFF + 896],
                                                                   start=(k == 0), stop=(k == 7)), reads=[R_wx, R_hT], writes=[R_pc[cc]])
                S.op("pe", lambda e, a=a, j=j: e.matmul(pa[a][:, 0:256], lhsT=ckvn[:, j * 128:(j + 1) * 128], rhs=wukv[:, 256:512], start=True, stop=True),
                     reads=[R_w2, R_ckvn], writes=[R_pa[a]])
                S.op("dve", lambda e, d=d, v=v: e.tensor_copy(out=tvs[v][:, TV_RV:TV_RV + 256], in_=pd[d][:, 0:256]), reads=[R_pd[d]], writes=[R_tvs[v]])
                S.op("act", lambda e, d=d, v=v: e.activation(out=tvs[v][:, TV_RG:TV_RG + 256], in_=pd[d][:, 256:512], func=AF.Silu), reads=[R_pd[d]], writes=[R_tvs[v]])
                S.op("dve", lambda e, cc=cc, v=v: e.tensor_copy(out=tvs[v][:, TV_DV:TV_DV + 384], in_=pc[cc][:, 0:384]), reads=[R_pc[cc]], writes=[R_tvs[v]])
                S.op("act", lambda e, a=a, v=v: e.activation(out=tvs[v][:, TV_MV:TV_MV + 256], in_=pa[a][:, 0:256], func=AF.Copy), reads=[R_pa[a]], writes=[R_tvs[v]])
                S.dma("sp", P.tokv[t0:t0 + 128, :], tvs[v][:], reads=[R_tvs[v]], writes=[Res()])

        for bi, (tok0, W) in enumerate(TB):
            do_block(bi, tok0, W)
        S.drain()


QBLOCKS = [(0, 256, 2)] + [(256 + 512 * j, 512, NT) for j in range(8)]


def phase_attention(c, l):
    P, S, nc, sbt, pst = c["P"], c["S"], c["nc"], c["sbt"], c["pst"]
    for grp in ATT_GROUPS:
        with ExitStack() as st:
            names = dict(diff=("dq0", "dq1", "dk0", "dk1"), gqa=("gq0", "gq1", "gk"),
                         mla=("mqn0", "mqn1", "mqr", "mkn0", "mkn1", "mkr"))[grp]
            fmt = {}
            R_in = Res("attn_in")
            for i, n in enumerate(names):
                fmt[n] = sbt(st, "fm_" + n, [128, T], BF16)
                S.dma(("sp", "act")[i % 2], fmt[n][:], P.fm[FM[n]], writes=[R_in])
            nh, voff, moff = dict(diff=(4, TV_DV, 256), gqa=(2, TV_GV, 512), mla=(4, TV_MV, 768))[grp]
            vraw = sbt(st, "vraw", [128, NT, nh * 64], BF16)
            vaug = sbt(st, "vaug", [128, NT, nh, 65], BF16)
            S.dma("sp", vraw[:], P.tokv[:, voff:voff + nh * 64].rearrange("(i p) c -> p i c", p=128), writes=[R_in])
            S.op("pool", lambda e: e.memset(vaug[:], 1.0), writes=[R_in])
            S.op("pool", lambda e: e.tensor_copy(out=vaug[:, :, :, 0:64], in_=vraw[:].rearrange("p i (h d) -> p i h d", h=nh)),
                 reads=[R_in], writes=[R_in])
            kz = {}
            for n in dict(diff=("dk0", "dk1"), gqa=(), mla=("mkr",))[grp]:
                kz[n] = sbt(st, "kz_" + n, [128, T], BF16)
                S.op("pool", lambda e, n=n: e.tensor_copy(out=kz[n][64:128, :], in_=fmt[n][64:128, :]), reads=[R_in], writes=[R_in])
                S.op("pool", lambda e, n=n: e.memset(kz[n][64:96, :], 0.0), reads=[R_in], writes=[R_in])
            mixo = sbt(st, "mixo", [128, NT, 256], BF16)
            R_mixo = Res("mixo")
            stp = [pst(st, "stp%d" % i, [128, 512], F32) for i in range(2)]
            po = [pst(st, "po%d" % i, [128, 512], F32) for i in range(4)]
            R_stp = [Res(), Res()]; R_po = [Res() for _ in range(4)]
            NE = 4
            eb = [sbt(st, "eb%d" % i, [128, 512], BF16) for i in range(NE)]
            R_eb = [Res() for _ in range(NE)]
            rden = sbt(st, "rden", [128, 4], F32); R_rden = Res()
            cnt = dict(s=0, e=0)
            if grp == "diff":
                d1 = sbt(st, "d1", [128, NT, 64], F32); R_d1 = Res()
                lamr = sbt(st, "lamr", [128, 128], F32); lam2 = sbt(st, "lam2", [128, 2], F32)
                neglam = sbt(st, "neglam", [128, 1], F32); li = sbt(st, "li", [128, 1], F32)
                subb = sbt(st, "subb", [128, 64], F32)
                o2 = sbt(st, "o2", [128, 64], F32); cmbt = sbt(st, "cmbt", [128, 64], F32); jk = sbt(st, "jk", [128, 64], F32)
                ssd = sbt(st, "ssd", [128, 1], F32)
                R_lam = Res(); R_t = Res()
                S.dma("sp", lamr[:], bc(P.rowv[l:l + 1, RV_LAM:RV_LAM + 128], 128), writes=[R_lam])
                S.dma("sp", li[:], bc(P.rowv[l:l + 1, RV_LAMINIT:RV_LAMINIT + 1], 128), writes=[R_lam])
                S.dma("sp", subb[:], bc(P.rowv[l:l + 1, RV_SUBLN:RV_SUBLN + 64], 128), writes=[R_lam])
                S.op("dve", lambda e: e.tensor_tensor(out=lamr[:, 0:32], in0=lamr[:, 0:32], in1=lamr[:, 32:64], op=ALU.mult), reads=[R_lam], writes=[R_lam])
                S.op("dve", lambda e: e.tensor_tensor(out=lamr[:, 64:96], in0=lamr[:, 64:96], in1=lamr[:, 96:128], op=ALU.mult), reads=[R_lam], writes=[R_lam])
                S.op("dve", lambda e: e.tensor_reduce(out=lam2[:, 0:1], in_=lamr[:, 0:32], axis=AX.X, op=ALU.add), reads=[R_lam], writes=[R_lam])
                S.op("dve", lambda e: e.tensor_reduce(out=lam2[:, 1:2], in_=lamr[:, 64:96], axis=AX.X, op=ALU.add), reads=[R_lam], writes=[R_lam])
                S.op("act", lambda e: e.activation(out=lam2[:], in_=lam2[:], func=AF.Exp), reads=[R_lam], writes=[R_lam])
                S.op("dve", lambda e: e.tensor_tensor(out=neglam[:], in0=lam2[:, 1:2], in1=lam2[:, 0:1], op=ALU.subtract), reads=[R_lam], writes=[R_lam])
                S.op("dve", lambda e: e.tensor_tensor(out=neglam[:], in0=neglam[:], in1=li[:], op=ALU.subtract), reads=[R_lam], writes=[R_lam])
                S.op("dve", lambda e: e.tensor_scalar(out=li[:], in0=li[:], scalar1=-1.0, scalar2=1.0, op0=ALU.mult, op1=ALU.add), reads=[R_lam], writes=[R_lam])
                S.op("dve", lambda e: e.tensor_scalar(out=subb[:], in0=subb[:], scalar1=li[:, 0:1], scalar2=None, op0=ALU.mult), reads=[R_lam], writes=[R_lam])

            nmaps = [0]

            def run_map(parts, vh, scale, finish):
                nmaps[0] += 1
                if ATT_MAXMAPS is not None and nmaps[0] > ATT_MAXMAPS:
                    return
                dbg = "dbgAT" in P.debug and nmaps[0] == 1
                if dbg:
                    dd = nc.dram_tensor("dbgAT", [3, 128, 512], F32, kind="ExternalOutput").ap()
                    dv = nc.dram_tensor("dbgV", [128, NT * nh * 65], BF16, kind="ExternalOutput").ap()
                    dt1 = sbt(st, "dt1", [128, 512], F32); dt2 = sbt(st, "dt2", [128, 512], F32); dt3 = sbt(st, "dt3", [128, 512], F32)
                    S.dma("sp", dv, vaug[:].rearrange("p a b c -> p (a b c)"), reads=[R_in], writes=[Res()])
                for (q0, N, nkt) in (QBLOCKS[:2] if ATT_MAXMAPS is not None else QBLOCKS):
                    nqs = N // 128
                    for kt in range(nkt):
                        sb_ = cnt["s"]; cnt["s"] = (sb_ + 1) % 2
                        eb_ = cnt["e"]; cnt["e"] = (eb_ + 1) % NE
                        for pi, (Kt, Qt, r0, nr) in enumerate(parts):
                            S.op("pe", lambda e, Kt=Kt, Qt=Qt, r0=r0, nr=nr, kt=kt, sb_=sb_, pi=pi, q0=q0, N=N: e.matmul(
                                stp[sb_][:, 0:N], lhsT=Kt[r0:r0 + nr, kt * 128:(kt + 1) * 128], rhs=Qt[r0:r0 + nr, q0:q0 + N],
                                start=(pi == 0), stop=(pi == len(parts) - 1)), reads=[R_in], writes=[R_stp[sb_]])
                        S.op("act", lambda e, sb_=sb_, eb_=eb_, N=N: e.activation(out=eb[eb_][:, 0:N], in_=stp[sb_][:, 0:N], func=AF.Exp, scale=scale),
                             reads=[R_stp[sb_]], writes=[R_eb[eb_]])
                        if dbg and q0 == 0 and kt == 0:
                            S.op("dve", lambda e, sb_=sb_: e.tensor_copy(out=dt1[:], in_=stp[sb_][:]), reads=[R_stp[sb_]], writes=[Res()])
                            S.op("dve", lambda e, eb_=eb_: e.tensor_copy(out=dt2[:], in_=eb[eb_][:]), reads=[R_eb[eb_]], writes=[Res()])
                        for qs in range(nqs):
                            S.op("pe", lambda e, qs=qs, eb_=eb_, kt=kt, nkt=nkt: e.matmul(
                                po[qs][:, 0:65], lhsT=eb[eb_][:, qs * 128:(qs + 1) * 128], rhs=vaug[:, kt, vh, :],
                                start=(kt == 0), stop=(kt == nkt - 1)), reads=[R_eb[eb_], R_in], writes=[R_po[qs]])
                    if dbg and q0 == 0:
                        Rd = Res()
                        S.op("dve", lambda e: e.tensor_copy(out=dt3[:], in_=po[0][:]), reads=[R_po[0]], writes=[Rd])
                        S.dma("sp", dd[0], dt1[:], reads=[Rd], writes=[Res()])
                        S.dma("sp", dd[1], dt2[:], reads=[Rd], writes=[Res()])
                        S.dma("sp", dd[2], dt3[:], reads=[Rd], writes=[Res()])
                    for qs in range(nqs):
                        ti = (q0 // 128) + qs
                        S.op("dve", lambda e, qs=qs: e.tensor_copy(out=rden[:, qs:qs + 1], in_=po[qs][:, 64:65]), reads=[R_po[qs]], writes=[R_rden])
                        S.op("dve", lambda e, qs=qs: e.reciprocal(out=rden[:, qs:qs + 1], in_=rden[:, qs:qs + 1]), reads=[R_rden], writes=[R_rden])
                        finish(qs, ti)

            def fin_plain(col):
                def f(qs, ti):
                    S.op("dve", lambda e: e.tensor_scalar(out=mixo[:, ti, col:col + 64], in0=po[qs][:, 0:64], scalar1=rden[:, qs:qs + 1],
                                                          scalar2=None, op0=ALU.mult), reads=[R_po[qs], R_rden], writes=[R_mixo])
                return f

            def fin_d1(qs, ti):
                S.op("dve", lambda e: e.tensor_scalar(out=d1[:, ti, :], in0=po[qs][:, 0:64], scalar1=rden[:, qs:qs + 1], scalar2=None, op0=ALU.mult),
                     reads=[R_po[qs], R_rden], writes=[R_d1])

            def fin_d2(col):
                def f(qs, ti):
                    S.op("dve", lambda e: e.tensor_scalar(out=o2[:], in0=po[qs][:, 0:64], scalar1=rden[:, qs:qs + 1], scalar2=None, op0=ALU.mult),
                         reads=[R_po[qs], R_rden], writes=[R_t])
                    S.op("dve", lambda e: e.scalar_tensor_tensor(out=cmbt[:], in0=o2[:], scalar=neglam[:, 0:1], in1=d1[:, ti, :], op0=ALU.mult, op1=ALU.add),
                         reads=[R_t, R_d1, R_lam], writes=[R_t])
                    S.op("dve", lambda e: e.tensor_tensor(out=jk[:], in0=cmbt[:], in1=cmbt[:], op=ALU.mult), reads=[R_t], writes=[R_t])
                    S.op("dve", lambda e: e.tensor_reduce(out=ssd[:], in_=jk[:], axis=AX.X, op=ALU.add), reads=[R_t], writes=[R_t])
                    S.op("dve", lambda e: e.tensor_scalar(out=ssd[:], in0=ssd[:], scalar1=1.0 / 64, scalar2=EPS, op0=ALU.mult, op1=ALU.add), reads=[R_t], writes=[R_t])
                    S.op("act", lambda e: e.activation(out=ssd[:], in_=ssd[:], func=AF.Sqrt), reads=[R_t], writes=[R_t])
                    S.op("dve", lambda e: e.reciprocal(out=ssd[:], in_=ssd[:]), reads=[R_t], writes=[R_t])
                    S.op("dve", lambda e: e.scalar_tensor_tensor(out=mixo[:, ti, col:col + 64], in0=cmbt[:], scalar=ssd[:, 0:1], in1=subb[:], op0=ALU.mult, op1=ALU.mult),
                         reads=[R_t, R_lam], writes=[R_mixo])
                return f

            if grp == "diff":
                for h in range(4):
                    Kt, Qt, base = fmt["dk%d" % (h // 2)], fmt["dq%d" % (h // 2)], 64 * (h % 2)
                    run_map([(Kt, Qt, base, 32)], h, 32 ** -0.5, fin_d1)
                    if base + 32 == 96:
                        run_map([(kz["dk%d" % (h // 2)], Qt, 64, 64)], h, 32 ** -0.5, fin_d2(h * 64))
                    else:
                        run_map([(Kt, Qt, base + 32, 32)], h, 32 ** -0.5, fin_d2(h * 64))
            elif grp == "gqa":
                for hq in range(4):
                    n, rep = hq // 2, hq % 2
                    run_map([(fmt["gk"], fmt["gq%d" % rep], 64 * n, 64)], n, 64 ** -0.5, fin_plain(hq * 64))
            else:
                for h in range(4):
                    run_map([(fmt["mkn%d" % (h // 2)], fmt["mqn%d" % (h // 2)], 64 * (h % 2), 64),
                             (fmt["mkr"], fmt["mqr"], 32 * h, 32) if h < 3 else (kz["mkr"], fmt["mqr"], 64, 64)], h, 96 ** -0.5, fin_plain(h * 64))
            S.dma("sp", P.mixd[:, moff:moff + 256].rearrange("(i p) c -> p i c", p=128), mixo[:], reads=[R_mixo], writes=[Res()])
            S.drain()


def phase_outproj(c, l):
    P, S, nc, sbt, pst = c["P"], c["S"], c["nc"], c["sbt"], c["pst"]
    cmb, R_cmb, R_xres = c["cmb"], c["R_cmb"], c["R_xres"]
    with ExitStack() as st:
        G, R_G = load_gate_bcast(c, l, st, 2, RV_G1, "a")
        wo = sbt(st, "wo", [128, 8, D], BF16); R_wo = Res()
        src = P.wout[l].rearrange("(c p) n -> p c n", p=128)
        for k in range(8):
            S.dma("pool", wo[:, k, :], src[:, k, :], writes=[R_wo])
        NB = 2
        mx = [sbt(st, "mx%d" % i, [128, D], BF16) for i in range(NB)]
        mT = [sbt(st, "mT%d" % i, [128, 8, 128], BF16) for i in range(NB)]
        xt = [sbt(st, "xt%d" % i, [128, D], F32) for i in range(NB)]
        tt = [sbt(st, "tt%d" % i, [128, D], F32) for i in range(NB)]
        junk = sbt(st, "junk", [128, D], F32)
        ss = [sbt(st, "ss%d" % i, [128, 1], F32) for i in range(NB)]
        rstd = [sbt(st, "rstd%d" % i, [128, 1], F32) for i in range(NB)]
        ptm = [pst(st, "ptm%d" % i, [128, 8, 128], BF16) for i in range(NB)]
        py = [pst(st, "py%d" % i, [128, D], F32) for i in range(NB)]
        R = lambda: [Res() for _ in range(NB)]
        R_mx, R_mT, R_xt, R_tt, R_ss, R_rstd, R_ptm, R_py = R(), R(), R(), R(), R(), R(), R(), R()
        R_junk = Res()

        def tile(i):
            b = i % NB
            r = 1 if i < 2 else 0
            S.dma("sp", mx[b][:], P.mixd[i * 128:(i + 1) * 128, :], writes=[R_mx[b]])
            S.dma("act", xt[b][:], P.xres[i * 128:(i + 1) * 128, :], reads=[R_xres[i]], writes=[R_xt[b]])
            for k in range(8):
                S.op("pe", lambda e, k=k: e.transpose(out=ptm[b][:, k, :], in_=mx[b][:, k * 128:(k + 1) * 128], identity=cmb[:, CM_ID:CM_ID + 128]),
                     reads=[R_mx[b], R_cmb], writes=[R_ptm[b]])
            S.op("act", lambda e: e.activation(out=mT[b][:], in_=ptm[b][:], func=AF.Copy), reads=[R_ptm[b]], writes=[R_mT[b]])
            for half in range(2):
                for k in range(8):
                    S.op("pe", lambda e, k=k, half=half: e.matmul(py[b][:, half * 512:(half + 1) * 512], lhsT=mT[b][:, k, :],
                                                                 rhs=wo[:, k, half * 512:(half + 1) * 512], start=(k == 0), stop=(k == 7)),
                         reads=[R_mT[b], R_wo], writes=[R_py[b]])
            rms_rstd(S, [R_py[b]], py[b][:], D, junk[:], R_junk, ss[b][:], R_ss[b], rstd[b][:], R_rstd[b])
            S.op("dve", lambda e: e.scalar_tensor_tensor(out=tt[b][:], in0=py[b][:], scalar=rstd[b][:, 0:1], in1=G[:, r, :], op0=ALU.mult, op1=ALU.mult),
                 reads=[R_py[b], R_rstd[b], R_G], writes=[R_tt[b]])
            S.op("pool", lambda e: e.tensor_tensor(out=tt[b][:], in0=tt[b][:], in1=xt[b][:], op=ALU.add), reads=[R_tt[b], R_xt[b]], writes=[R_tt[b]])
            S.dma("sp", P.xres[i * 128:(i + 1) * 128, :], tt[b][:], reads=[R_tt[b]], writes=[R_xres[i]])

        for i in range(NT):
            tile(i)
        S.drain()


def phase_experts(c, l):
    P, S, nc, sbt, pst = c["P"], c["S"], c["nc"], c["sbt"], c["pst"]
    cmb, R_cmb = c["cmb"], c["R_cmb"]
    NS = CAP // 128
    with ExitStack() as st:
        w1b = [sbt(st, "w1b%d" % i, [128, 8, 2 * D], BF16) for i in range(2)]
        w2b = [sbt(st, "w2b%d" % i, [128, 8, D], BF16) for i in range(2)]
        b1 = [sbt(st, "b1_%d" % i, [128, 16], F32) for i in range(2)]
        b2b = [sbt(st, "b2b%d" % i, [128, D], F32) for i in range(2)]
        R_w = [Res(), Res()]
        xs = [sbt(st, "xs%d" % i, [128, D], BF16) for i in range(2)]
        xT = sbt(st, "xT", [128, 8, CAP], BF16); aT = sbt(st, "aT", [128, 8, CAP], BF16)
        R_xs = [Res(), Res()]; R_xT = Res(); R_aT = Res()
        NW = 2
        gs = [sbt(st, "gs%d" % i, [128, 512], F32) for i in range(NW)]
        sg = [sbt(st, "sg%d" % i, [128, 512], F32) for i in range(NW)]
        ls = [sbt(st, "ls%d" % i, [128, 512], F32) for i in range(NW)]
        R_gs, R_sg, R_ls = [Res() for _ in range(NW)], [Res() for _ in range(NW)], [Res() for _ in range(NW)]
        ys = [sbt(st, "ys%d" % i, [128, D], F32) for i in range(2)]; R_ys = [Res(), Res()]
        ptx = [pst(st, "ptx%d" % i, [128, 8, 128], BF16) for i in range(2)]
        pg = [pst(st, "pg%d" % i, [128, 512], F32) for i in range(2)]
        pl = [pst(st, "pl%d" % i, [128, 512], F32) for i in range(2)]
        pyy = [pst(st, "pyy%d" % i, [128, 512], F32) for i in range(2)]
        R_ptx, R_pg, R_pl, R_pyy = [Res(), Res()], [Res(), Res()], [Res(), Res()], [Res(), Res()]
        cnt = dict(x=0, w=0, g=0, y=0, ys=0)

        def nxt(key, n):
            v = cnt[key]; cnt[key] = (v + 1) % n
            return v

        def load_w(e):
            b = e % 2
            s1 = P.w1[l, e].rearrange("(c p) n -> p c n", p=128)
            s2 = P.w2[l, e].rearrange("(c p) n -> p c n", p=128)
            for k in range(8):
                S.dma("pool", w1b[b][:, k, :], s1[:, k, :], writes=[R_w[b]])
            for k in range(0, 8, 2):
                S.dma("pool", w2b[b][:, k:k + 2, :], s2[:, k:k + 2, :], writes=[R_w[b]])
            S.dma("sp", b1[b][:], P.b1c[l, e], writes=[R_w[b]])
            S.dma("sp", b2b[b][:], bc(P.b2[l, e:e + 1, :], 128), writes=[R_w[b]])

        def expert(e):
            b = e % 2
            for s in range(NS):
                x = nxt("x", 2)
                S.dma("sp" if s % 2 else "act", xs[x][:], P.xg[e * CAP + s * 128:e * CAP + (s + 1) * 128, :], writes=[R_xs[x]])
                for k in range(8):
                    S.op("pe", lambda e_, k=k, x=x: e_.transpose(out=ptx[x][:, k, :], in_=xs[x][:, k * 128:(k + 1) * 128], identity=cmb[:, CM_ID:CM_ID + 128]),
                         reads=[R_xs[x], R_cmb], writes=[R_ptx[x]])
                S.op("act" if s % 2 else "dve", (lambda e_, x=x, s=s: e_.activation(out=xT[:, :, s * 128:(s + 1) * 128], in_=ptx[x][:], func=AF.Copy)) if s % 2 else
                     (lambda e_, x=x, s=s: e_.tensor_copy(out=xT[:, :, s * 128:(s + 1) * 128], in_=ptx[x][:])), reads=[R_ptx[x]], writes=[R_xT])
            for fc in range(8):
                for (c0, W) in ((0, 512), (512, 512), (1024, CAP - 1024)):
                    g = nxt("g", 2); w = nxt("w", NW)
                    for k in range(8):
                        S.op("pe", lambda e_, k=k, g=g, fc=fc, c0=c0, W=W: e_.matmul(pg[g][:, 0:W], lhsT=w1b[b][:, k, fc * 256:(fc + 1) * 256:2],
                                                                                     rhs=xT[:, k, c0:c0 + W], start=(k == 0), stop=(k == 7)),
                             reads=[R_w[b], R_xT], writes=[R_pg[g]])
                    for k in range(8):
                        S.op("pe", lambda e_, k=k, g=g, fc=fc, c0=c0, W=W: e_.matmul(pl[g][:, 0:W], lhsT=w1b[b][:, k, fc * 256 + 1:(fc + 1) * 256:2],
                                                                                     rhs=xT[:, k, c0:c0 + W], start=(k == 0), stop=(k == 7)),
                             reads=[R_w[b], R_xT], writes=[R_pl[g]])
                    S.op("dve", lambda e_, g=g, w=w, fc=fc, W=W: e_.tensor_scalar(out=gs[w][:, 0:W], in0=pg[g][:, 0:W], scalar1=b1[b][:, 2 * fc:2 * fc + 1], scalar2=7.0,
                                                                               op0=ALU.add, op1=ALU.min), reads=[R_pg[g], R_w[b]], writes=[R_gs[w]])
                    S.op("act", lambda e_, w=w, W=W: e_.activation(out=sg[w][:, 0:W], in_=gs[w][:, 0:W], func=AF.Sigmoid, scale=1.702), reads=[R_gs[w]], writes=[R_sg[w]])
                    S.op("dve", lambda e_, g=g, w=w, fc=fc, W=W: e_.tensor_scalar(out=ls[w][:, 0:W], in0=pl[g][:, 0:W], scalar1=b1[b][:, 2 * fc + 1:2 * fc + 2], scalar2=7.0,
                                                                               op0=ALU.add, op1=ALU.min), reads=[R_pl[g], R_w[b]], writes=[R_ls[w]])
                    S.op("pool", lambda e_, w=w, W=W: e_.tensor_scalar(out=ls[w][:, 0:W], in0=ls[w][:, 0:W], scalar1=-7.0, scalar2=1.0, op0=ALU.max, op1=ALU.add),
                         reads=[R_ls[w]], writes=[R_ls[w]])
                    S.op("pool", lambda e_, w=w, W=W: e_.tensor_tensor(out=gs[w][:, 0:W], in0=gs[w][:, 0:W], in1=sg[w][:, 0:W], op=ALU.mult),
                         reads=[R_gs[w], R_sg[w]], writes=[R_gs[w]])
                    S.op("pool", lambda e_, w=w, W=W, fc=fc, c0=c0: e_.tensor_tensor(out=aT[:, fc, c0:c0 + W], in0=gs[w][:, 0:W], in1=ls[w][:, 0:W], op=ALU.mult),
                         reads=[R_gs[w], R_ls[w]], writes=[R_aT])
            for s in range(NS):
                yb = nxt("ys", 2)
                for half in range(2):
                    y = nxt("y", 2)
                    for fc in range(8):
                        S.op("pe", lambda e_, fc=fc, y=y, s=s, half=half: e_.matmul(pyy[y][:], lhsT=aT[:, fc, s * 128:(s + 1) * 128],
                                                                                   rhs=w2b[b][:, fc, half * 512:(half + 1) * 512], start=(fc == 0), stop=(fc == 7)),
                             reads=[R_aT, R_w[b]], writes=[R_pyy[y]])
                    S.op("dve", lambda e_, y=y, yb=yb, half=half: e_.tensor_tensor(out=ys[yb][:, half * 512:(half + 1) * 512], in0=pyy[y][:],
                                                                                  in1=b2b[b][:, half * 512:(half + 1) * 512], op=ALU.add),
                         reads=[R_pyy[y], R_w[b]], writes=[R_ys[yb]])
                S.dma("sp", P.yg[e * CAP + s * 128:e * CAP + (s + 1) * 128, :], ys[yb][:], reads=[R_ys[yb]], writes=[Res()])

        load_w(0)
        for e in range(NEXP):
            if e + 1 < NEXP:
                load_w(e + 1)
            expert(e)
        S.drain()


def phase_combine(c, l, rt):
    P, S, nc, sbt, pst = c["P"], c["S"], c["nc"], c["sbt"], c["pst"]
    R_xres = c["R_xres"]
    with ExitStack() as st:
        G, R_G = load_gate_bcast(c, l, st, 5, RV_G3, "f")
        NB = 2
        yk = [[sbt(st, "yk%d_%d" % (i, k), [128, D], F32) for k in range(4)] for i in range(NB)]
        R_yk = [[Res() for k in range(4)] for i in range(NB)]
        acc = [sbt(st, "acc%d" % i, [128, D], F32) for i in range(NB)]
        xt = [sbt(st, "xt%d" % i, [128, D], F32) for i in range(NB)]
        junk = sbt(st, "junk", [128, D], F32); R_junk = Res()
        ss = [sbt(st, "ss%d" % i, [128, 1], F32) for i in range(NB)]
        rstd = [sbt(st, "rstd%d" % i, [128, 1], F32) for i in range(NB)]
        R = lambda: [Res() for _ in range(NB)]
        R_acc, R_xt, R_ss, R_rstd = R(), R(), R(), R()
        gates, slots = rt["gates"], rt["slots"]

        def tile(i):
            b = i % NB
            r = 1 if i < 2 else 0
            S.dma("sp", xt[b][:], P.xres[i * 128:(i + 1) * 128, :], reads=[R_xres[i]], writes=[R_xt[b]])
            for k in range(4):
                S.dma("pool", yk[b][k][:], P.yg[:, :], reads=[rt["R_slots"]], writes=[R_yk[b][k]],
                      indirect=dict(out_offset=None, in_offset=bass.IndirectOffsetOnAxis(ap=slots[:, i, k:k + 1], axis=0)))
            S.op("dve", lambda e: e.tensor_scalar(out=acc[b][:], in0=yk[b][0][:], scalar1=gates[:, i, 0:1], scalar2=None, op0=ALU.mult),
                 reads=[R_yk[b][0], rt["R_gates"]], writes=[R_acc[b]])
            for k in range(1, 4):
                S.op("dve", lambda e, k=k: e.scalar_tensor_tensor(out=acc[b][:], in0=yk[b][k][:], scalar=gates[:, i, k:k + 1], in1=acc[b][:],
                                                                                      op0=ALU.mult, op1=ALU.add),
                     reads=[R_yk[b][k], rt["R_gates"], R_acc[b]], writes=[R_acc[b]])
            rms_rstd(S, [R_acc[b]], acc[b][:], D, junk[:], R_junk, ss[b][:], R_ss[b], rstd[b][:], R_rstd[b])
            S.op("dve", lambda e: e.scalar_tensor_tensor(out=acc[b][:], in0=acc[b][:], scalar=rstd[b][:, 0:1], in1=G[:, r, :], op0=ALU.mult, op1=ALU.mult),
                 reads=[R_acc[b], R_rstd[b], R_G], writes=[R_acc[b]])
            S.op("pool", lambda e: e.tensor_tensor(out=acc[b][:], in0=acc[b][:], in1=xt[b][:], op=ALU.add), reads=[R_acc[b], R_xt[b]], writes=[R_acc[b]])
            S.dma("sp", P.xres[i * 128:(i + 1) * 128, :], acc[b][:], reads=[R_acc[b]], writes=[R_xres[i]])

        for i in range(NT):
            tile(i)
        S.drain()


def phase_retention(c, l):
    P, S, nc, sbt, pst = c["P"], c["S"], c["nc"], c["sbt"], c["pst"]
    cm, R_cm, cmb, R_cmb = c["cm"], c["R_cm"], c["cmb"], c["R_cmb"]
    with ExitStack() as st:
        QT = [sbt(st, "rQT%d" % p, [128, T], BF16) for p in range(2)]
        KT = [sbt(st, "rKT%d" % p, [128, T], BF16) for p in range(2)]
        QfT = [sbt(st, "rQfT%d" % p, [128, T], BF16) for p in range(2)]
        QbT = [sbt(st, "rQbT%d" % p, [128, T], BF16) for p in range(2)]
        V = sbt(st, "rV", [128, NT, 256], BF16); Gt = sbt(st, "rG", [128, NT, 256], BF16)
        Kzf = sbt(st, "Kzf", [128, NT, 256], BF16); Kzb = sbt(st, "Kzb", [128, NT, 256], BF16)
        SfAll = sbt(st, "SfAll", [128, NT, 2, 64], BF16); SbAll = sbt(st, "SbAll", [128, NT, 2, 64], BF16)
        mixo = sbt(st, "rmixo", [128, NT, 256], BF16)
        R_in, R_q, R_kz, R_sall, R_mixo = Res(), Res(), Res(), Res(), Res()
        for p in range(2):
            S.dma("sp", QT[p][:], P.fm[FM["rq%d" % p]], writes=[R_in])
            S.dma("act", KT[p][:], P.fm[FM["rk%d" % p]], writes=[R_in])
        S.dma("sp", V[:], P.tokv[:, TV_RV:TV_RV + 256].rearrange("(i p) c -> p i c", p=128), writes=[R_in])
        S.dma("act", Gt[:], P.tokv[:, TV_RG:TV_RG + 256].rearrange("(i p) c -> p i c", p=128), writes=[R_in])
        lg = sbt(st, "lg", [128, 8], F32); lgc = sbt(st, "lgc", [128, 4], F32); gC = sbt(st, "gC", [128, 4], F32)
        DT = sbt(st, "DT", [128, 4, 128], F32); e1 = sbt(st, "e1", [128, 128], F32); e2 = sbt(st, "e2", [128, 128], F32)
        Xi = sbt(st, "Xi", [128, 4, 128], BF16)
        Zt = sbt(st, "Zt", [128, 8], F32)
        gnw = sbt(st, "gnw", [128, 256], F32); gnb = sbt(st, "gnb", [128, 256], F32)
        R_t = Res()
        S.dma("sp", lg[:], bc(P.rowv[l:l + 1, RV_DECAY:RV_DECAY + 8], 128), writes=[R_t])
        S.dma("sp", gnw[:], bc(P.rowv[l:l + 1, RV_GNW:RV_GNW + 256], 128), writes=[R_t])
        S.dma("sp", gnb[:], bc(P.rowv[l:l + 1, RV_GNB:RV_GNB + 256], 128), writes=[R_t])
        S.op("act", lambda e: e.activation(out=lg[:], in_=lg[:], func=AF.Exp), reads=[R_t], writes=[R_t])
        S.op("dve", lambda e: e.tensor_scalar(out=lg[:], in0=lg[:], scalar1=-1.0, scalar2=None, op0=ALU.mult), reads=[R_t], writes=[R_t])
        for h in range(4):
            S.op("act", lambda e, h=h: e.activation(out=e1[:], in_=cm[:, CM_DPOS:CM_DPOS + 128], func=AF.Exp, scale=lg[:, h:h + 1]), reads=[R_t, R_cm], writes=[R_t])
            S.op("dve", lambda e: e.tensor_tensor(out=e1[:], in0=e1[:], in1=cm[:, CM_MF:CM_MF + 128], op=ALU.mult), reads=[R_t, R_cm], writes=[R_t])
            S.op("act", lambda e, h=h: e.activation(out=e2[:], in_=cm[:, CM_DNEG:CM_DNEG + 128], func=AF.Exp, scale=lg[:, 4 + h:5 + h]), reads=[R_t, R_cm], writes=[R_t])
            S.op("dve", lambda e: e.tensor_tensor(out=e2[:], in0=e2[:], in1=cm[:, CM_MB:CM_MB + 128], op=ALU.mult), reads=[R_t, R_cm], writes=[R_t])
            S.op("dve", lambda e, h=h: e.tensor_tensor(out=DT[:, h, :], in0=e1[:], in1=e2[:], op=ALU.add), reads=[R_t], writes=[R_t])
        for d in range(2):
            for p in range(2):
                S.op("dve", lambda e, d=d, p=p: e.tensor_copy(out=lgc[0:64, 2 * d + p:2 * d + p + 1], in_=lg[0:64, 4 * d + 2 * p:4 * d + 2 * p + 1]), reads=[R_t], writes=[R_t])
                S.op("dve", lambda e, d=d, p=p: e.tensor_copy(out=lgc[64:128, 2 * d + p:2 * d + p + 1], in_=lg[64:128, 4 * d + 2 * p + 1:4 * d + 2 * p + 2]), reads=[R_t], writes=[R_t])
        for p in range(2):
            S.op("act", lambda e, p=p: e.activation(out=Xi[:, p, :], in_=cm[:, CM_NP1:CM_NP1 + 128], func=AF.Exp, scale=lgc[:, p:p + 1]), reads=[R_t, R_cm], writes=[R_t])
            S.op("act", lambda e, p=p: e.activation(out=Xi[:, 2 + p, :], in_=cm[:, CM_NREV:CM_NREV + 128], func=AF.Exp, scale=lgc[:, 2 + p:3 + p]), reads=[R_t, R_cm], writes=[R_t])
        S.op("act", lambda e: e.activation(out=Zt[:, 0:4], in_=lg[:, 0:4], func=AF.Exp, scale=cm[:, CM_PREV:CM_PREV + 1]), reads=[R_t, R_cm], writes=[R_t])
        S.op("act", lambda e: e.activation(out=Zt[:, 4:8], in_=lg[:, 4:8], func=AF.Exp, scale=cm[:, CM_PCOL:CM_PCOL + 1]), reads=[R_t, R_cm], writes=[R_t])
        S.op("act", lambda e: e.activation(out=gC[:], in_=lgc[:], func=AF.Exp, scale=128.0), reads=[R_t], writes=[R_t])
        if RET_STOP == 1:
            S.drain()
            return
        for p in range(2):
            S.op("pool", lambda e, p=p: e.tensor_tensor(out=QfT[p][:].rearrange("f (c n) -> f c n", n=128), in0=QT[p][:].rearrange("f (c n) -> f c n", n=128),
                                                      in1=Xi[:, p:p + 1, :].to_broadcast([128, NT, 128]), op=ALU.mult), reads=[R_in, R_t], writes=[R_q])
            S.op("pool", lambda e, p=p: e.tensor_tensor(out=QbT[p][:].rearrange("f (c n) -> f c n", n=128), in0=QT[p][:].rearrange("f (c n) -> f c n", n=128),
                                                      in1=Xi[:, 2 + p:3 + p, :].to_broadcast([128, NT, 128]), op=ALU.mult), reads=[R_in, R_t], writes=[R_q])
        if RET_STOP == 2:
            S.drain()
            return
        ptk = [pst(st, "ptk%d" % i, [128, 1024], BF16) for i in range(2)]; R_ptk = [Res(), Res()]
        for cidx in range(NT):
            b = cidx % 2
            for p in range(2):
                S.op("pe", lambda e, p=p, b=b, cidx=cidx: e.transpose(out=ptk[b][:, p * 128:(p + 1) * 128], in_=KT[p][:, cidx * 128:(cidx + 1) * 128], identity=cmb[:, CM_ID:CM_ID + 128]),
                     reads=[R_in, R_cmb], writes=[R_ptk[b]])
            S.op("dve", lambda e, b=b, cidx=cidx: e.tensor_tensor(out=Kzf[:, cidx, :].rearrange("p (h d) -> p h d", h=4), in0=ptk[b][:, 0:256].rearrange("p (h d) -> p h d", h=4),
                                                                in1=Zt[:, 0:4, None].to_broadcast([128, 4, 64]), op=ALU.mult), reads=[R_ptk[b], R_t], writes=[R_kz])
            S.op("dve", lambda e, b=b, cidx=cidx: e.tensor_tensor(out=Kzb[:, cidx, :].rearrange("p (h d) -> p h d", h=4), in0=ptk[b][:, 0:256].rearrange("p (h d) -> p h d", h=4),
                                                                in1=Zt[:, 4:8, None].to_broadcast([128, 4, 64]), op=ALU.mult), reads=[R_ptk[b], R_t], writes=[R_kz])
        if RET_STOP == 3:
            S.drain()
            return
        pu = [pst(st, "pu%d" % i, [128, 512], F32) for i in range(2)]; R_pu = [Res(), Res()]
        Sst = sbt(st, "Sst", [128, 2, 64], F32); R_S = Res()
        ucnt = [0]
        for d, Kz, SAll, order in ((0, Kzf, SfAll, list(range(NT))), (1, Kzb, SbAll, [1, 0] + list(range(NT - 1, 1, -1)))):
            S.op("pool", lambda e: e.memset(Sst[:], 0.0), reads=[R_S], writes=[R_S])
            for cidx in order:
                S.op("act", lambda e, SAll=SAll, cidx=cidx: e.activation(out=SAll[:, cidx, :, :], in_=Sst[:], func=AF.Copy), reads=[R_S], writes=[R_sall])
                for p in range(2):
                    u = ucnt[0]; ucnt[0] = (u + 1) % 2
                    S.op("pe", lambda e, u=u, Kz=Kz, cidx=cidx, p=p: e.matmul(pu[u][:, 0:128], lhsT=Kz[:, cidx, p * 128:(p + 1) * 128], rhs=V[:, cidx, p * 128:(p + 1) * 128], start=True, stop=True),
                         reads=[R_kz, R_in], writes=[R_pu[u]])
                    for half in range(2):
                        rows = slice(64 * half, 64 * half + 64)
                        S.op("dve", lambda e, u=u, p=p, d=d, rows=rows: e.scalar_tensor_tensor(out=Sst[rows, p, :], in0=Sst[rows, p, :], scalar=gC[rows, 2 * d + p:2 * d + p + 1],
                                                                                            in1=pu[u][rows, rows], op0=ALU.mult, op1=ALU.add),
                             reads=[R_S, R_pu[u], R_t], writes=[R_S])
        if RET_STOP == 4:
            S.drain()
            return
        paE = pst(st, "rpaE", [128, 512], F32); paO = pst(st, "rpaO", [128, 512], F32)
        poE = pst(st, "rpoE", [128, 512], F32); poO = pst(st, "rpoO", [128, 512], F32)
        pa_ = (paE, paO); po_ = (poE, poO)
        R_pa = [Res(), Res()]; R_po = [Res(), Res()]
        Wt = [sbt(st, "Wt%d" % i, [128, 4, 128], BF16) for i in range(2)]; R_W = [Res(), Res()]
        ob = [sbt(st, "rob%d" % i, [128, 4, 64], F32) for i in range(2)]; xc = [sbt(st, "rxc%d" % i, [128, 4, 64], F32) for i in range(2)]
        sqv = [sbt(st, "rsq%d" % i, [128, 4, 64], F32) for i in range(2)]
        mu = [sbt(st, "rmu%d" % i, [128, 4], F32) for i in range(2)]; var = [sbt(st, "rvar%d" % i, [128, 4], F32) for i in range(2)]
        R_o = [Res(), Res()]

        def chunk(cidx):
            b = cidx % 2
            cs = slice(cidx * 128, (cidx + 1) * 128)
            for h in (0, 2, 1, 3):
                p, par, rows = h // 2, h % 2, slice(64 * (h % 2), 64 * (h % 2) + 64)
                S.op("pe", lambda e, h=h, p=p, par=par, rows=rows: e.matmul(pa_[par][:, p * 128:(p + 1) * 128], lhsT=KT[p][rows, cs], rhs=QT[p][rows, cs], start=True, stop=True),
                     reads=[R_in], writes=[R_pa[par]])
            for h in range(4):
                p, par = h // 2, h % 2
                S.op("dve", lambda e, h=h, p=p, par=par: e.tensor_tensor(out=Wt[b][:, h, :], in0=pa_[par][:, p * 128:(p + 1) * 128], in1=DT[:, h, :], op=ALU.mult),
                     reads=[R_pa[par], R_t], writes=[R_W[b]])
            for h in (0, 2, 1, 3):
                p, par, rows = h // 2, h % 2, slice(64 * (h % 2), 64 * (h % 2) + 64)
                dst = po_[par][:, p * 64:(p + 1) * 64]
                S.op("pe", lambda e, h=h, dst=dst: e.matmul(dst, lhsT=Wt[b][:, h, :], rhs=V[:, cidx, h * 64:(h + 1) * 64], start=True, stop=False),
                     reads=[R_W[b], R_in], writes=[R_po[par]])
                S.op("pe", lambda e, h=h, p=p, rows=rows, dst=dst: e.matmul(dst, lhsT=QfT[p][rows, cs], rhs=SfAll[rows, cidx, p, :], start=False, stop=False),
                     reads=[R_q, R_sall], writes=[R_po[par]])
                S.op("pe", lambda e, h=h, p=p, rows=rows, dst=dst: e.matmul(dst, lhsT=QbT[p][rows, cs], rhs=SbAll[rows, cidx, p, :], start=False, stop=True),
                     reads=[R_q, R_sall], writes=[R_po[par]])
            Ro = R_o[b]
            for h in range(4):
                p, par = h // 2, h % 2
                S.op("act", lambda e, h=h, p=p, par=par: e.activation(out=ob[b][:, h, :], in_=po_[par][:, p * 64:(p + 1) * 64], func=AF.Copy, scale=0.125),
                     reads=[R_po[par]], writes=[Ro])
            S.op("dve", lambda e: e.tensor_reduce(out=mu[b][:], in_=ob[b][:], axis=AX.X, op=ALU.add), reads=[Ro], writes=[Ro])
            S.op("dve", lambda e: e.tensor_scalar(out=mu[b][:], in0=mu[b][:], scalar1=1.0 / 64, scalar2=None, op0=ALU.mult), reads=[Ro], writes=[Ro])
            S.op("pool", lambda e: e.tensor_tensor(out=xc[b][:], in0=ob[b][:], in1=mu[b][:, :, None].to_broadcast([128, 4, 64]), op=ALU.subtract), reads=[Ro], writes=[Ro])
            S.op("pool", lambda e: e.tensor_tensor(out=sqv[b][:], in0=xc[b][:], in1=xc[b][:], op=ALU.mult), reads=[Ro], writes=[Ro])
            S.op("dve", lambda e: e.tensor_reduce(out=var[b][:], in_=sqv[b][:], axis=AX.X, op=ALU.add), reads=[Ro], writes=[Ro])
            S.op("dve", lambda e: e.tensor_scalar(out=var[b][:], in0=var[b][:], scalar1=1.0 / 64, scalar2=EPS, op0=ALU.mult, op1=ALU.add), reads=[Ro], writes=[Ro])
            S.op("act", lambda e: e.activation(out=var[b][:], in_=var[b][:], func=AF.Sqrt), reads=[Ro], writes=[Ro])
            S.op("dve", lambda e: e.reciprocal(out=var[b][:], in_=var[b][:]), reads=[Ro], writes=[Ro])
            S.op("pool", lambda e: e.tensor_tensor(out=xc[b][:], in0=xc[b][:], in1=var[b][:, :, None].to_broadcast([128, 4, 64]), op=ALU.mult), reads=[Ro], writes=[Ro])
            xcf = xc[b][:].rearrange("p h d -> p (h d)")
            S.op("pool", lambda e: e.tensor_tensor(out=xcf, in0=xcf, in1=gnw[:], op=ALU.mult), reads=[Ro, R_t], writes=[Ro])
            S.op("pool", lambda e: e.tensor_tensor(out=xcf, in0=xcf, in1=gnb[:], op=ALU.add), reads=[Ro, R_t], writes=[Ro])
            S.op("pool", lambda e: e.tensor_tensor(out=mixo[:, cidx, :], in0=xcf, in1=Gt[:, cidx, :], op=ALU.mult), reads=[Ro, R_in], writes=[R_mixo])

        for cidx in range(NT):
            chunk(cidx)
        S.dma("sp", P.mixd[:, 0:256].rearrange("(i p) c -> p i c", p=128), mixo[:], reads=[R_mixo], writes=[Res()])
        S.drain()


_PROG_CACHE = {}


def kernel(**inputs):
    n = 8
    if 1 not in _PROG_CACHE:
        _PROG_CACHE[1] = build_program(1)
    P = _PROG_CACHE[1]
    cmn, tabs = build_consts()
    x = np.asarray(inputs["x"], np.float32)
    ctxv = np.asarray(inputs["ctx"], np.float32)
    xin = [np.ascontiguousarray(np.concatenate([ctxv[b], x[b]], 0)) for b in range(n)]
    cvec = [np.ascontiguousarray(np.concatenate([_col(inputs["c"][b]), _col(inputs["c_ctx"])], 1)) for b in range(n)]
    for l in range(DEPTH):
        la = prep_layer_arrays(inputs, [l])
        in_maps = [dict(xin=xin[b], cvec=cvec[b], cmat=cmn, ropet=tabs, **la) for b in range(n)]
        res = run_bass_kernel_spmd(P.nc, in_maps, core_ids=list(range(n)))
        xin = [np.asarray(r["yout"], np.float32) for r in res.results]
    return np.stack([xi[NCTX:] for xi in xin], 0).astype(np.float32)
```

```python
import os
import math
import numpy as np
from contextlib import ExitStack
import concourse.bass as bass
import concourse.mybir as mybir
from concourse.bass_utils import run_bass_kernel_spmd

F32 = mybir.dt.float32
BF16 = mybir.dt.bfloat16
I32 = mybir.dt.int32
U32 = mybir.dt.uint32
AF = mybir.ActivationFunctionType
ALU = mybir.AluOpType
AX = mybir.AxisListType

D = 1024
NCTX = 256
NLAT = 4096
T = NCTX + NLAT
NT = T // 128
DEPTH = 4
GRID_W = 64
EPS = 1e-6
NEXP = 32
CAP = 1152
NSLOT = NEXP * CAP
TB = [(i * 512, 512) for i in range(8)] + [(4096, 256)]

ENGS = ("pe", "act", "dve", "pool", "sp")
N_DMA_SEMS = 8
SAME_ENGINE_SYNC = True


class Res:
    __slots__ = ("name", "w", "r")

    def __init__(self, name=""):
        self.name = name
        self.w = None
        self.r = []


class Sched:
    def __init__(self, nc, stack):
        self.nc = nc
        self.sems = {}
        self.cnt = {}
        for e in ENGS:
            self.sems[e] = stack.enter_context(nc.semaphore("s_" + e))
            self.cnt[e] = 0
        self.dma_next = {}
        for q in ("sp", "pool", "act"):
            for i in range(N_DMA_SEMS):
                k = ("dma", q, i)
                self.sems[k] = stack.enter_context(nc.semaphore("d_%s_%d" % (q, i)))
                self.cnt[k] = 0
            self.dma_next[q] = 0
        self.known = {e: {} for e in ENGS}
        self.lists = {e: [] for e in ENGS}
        self.n_ops = 0

    def _need(self, eng, deps):
        out = {}
        for d in deps:
            if d is None:
                continue
            k, v = d
            if k == eng and not (SAME_ENGINE_SYNC and eng != "pe"):
                continue
            if self.known[eng].get(k, 0) >= v:
                continue
            if out.get(k, 0) < v:
                out[k] = v
        for k, v in out.items():
            self.known[eng][k] = v
        return list(out.items())

    @staticmethod
    def _deps(reads, writes):
        deps = []
        for r in reads:
            deps.append(r.w)
        for w in writes:
            deps.append(w.w)
            deps.extend(w.r)
        return deps

    @staticmethod
    def _mark(tag, reads, writes):
        for r in reads:
            r.r.append(tag)
            if len(r.r) > 64:
                best = {}
                for k, v in r.r:
                    if best.get(k, 0) < v:
                        best[k] = v
                r.r = list(best.items())
        for w in writes:
            w.w = tag
            w.r = []

    def op(self, eng, fn, reads=(), writes=()):
        waits = self._need(eng, self._deps(reads, writes))
        self.cnt[eng] += 1
        v = self.cnt[eng]
        sem = self.sems[eng]
        sems = self.sems

        def emit(e, fn=fn, waits=waits, sem=sem):
            for k, val in waits:
                e.wait_ge(sems[k], val)
            fn(e).then_inc(sem, 1)
        self.lists[eng].append(emit)
        self._mark((eng, v), reads, writes)
        self.n_ops += 1

    def dma(self, q, out, in_, reads=(), writes=(), indirect=None, **kw):
        i = self.dma_next[q]
        self.dma_next[q] = (i + 1) % N_DMA_SEMS
        k = ("dma", q, i)
        deps = self._deps(reads, writes)
        if self.cnt[k] > 0:
            deps.append((k, self.cnt[k]))
        waits = self._need(q, deps)
        self.cnt[k] += 16
        v = self.cnt[k]
        sem = self.sems[k]
        sems = self.sems

        def emit(e, waits=waits, sem=sem, out=out, in_=in_, kw=kw, indirect=indirect):
            for kk, val in waits:
                e.wait_ge(sems[kk], val)
            if indirect is None:
                e.dma_start(out=out, in_=in_, **kw).then_inc(sem, 16)
            else:
                e.indirect_dma_start(out=out, in_=in_, **indirect).then_inc(sem, 16)
        self.lists[q].append(emit)
        self._mark((k, v), reads, writes)
        self.n_ops += 1

    def wait_all(self, eng, resources):
        deps = []
        for r in resources:
            deps.append(r.w)
            deps.extend(r.r)
        waits = self._need(eng, deps)
        sems = self.sems

        def emit(e, waits=waits):
            for kk, val in waits:
                e.wait_ge(sems[kk], val)
        self.lists[eng].append(emit)

    def drain(self):
        deps = [(k, v) for k, v in self.cnt.items() if isinstance(k, tuple) and v > 0]
        waits = self._need("sp", deps)
        sems = self.sems

        def emit(e, waits=waits):
            for kk, val in waits:
                e.wait_ge(sems[kk], val)
        self.lists["sp"].append(emit)
        self.flush()
        for e in ENGS:
            for k, v in self.cnt.items():
                self.known[e][k] = v

    def flush(self):
        nc = self.nc
        lists = self.lists
        self.lists = {e: [] for e in ENGS}
        if not any(lists.values()):
            return
        with nc.Block() as block:
            @block.tensor
            def _(e):
                for f in lists["pe"]:
                    f(e)

            @block.scalar
            def _(e):
                for f in lists["act"]:
                    f(e)

            @block.vector
            def _(e):
                for f in lists["dve"]:
                    f(e)

            @block.gpsimd
            def _(e):
                for f in lists["pool"]:
                    f(e)

            @block.sync
            def _(e):
                for f in lists["sp"]:
                    f(e)


def _col(v):
    v = np.asarray(v, np.float32)
    return np.ascontiguousarray(v.reshape(-1, 128).T)


def _swap_blocks(cols, blk):
    cols = np.asarray(cols)
    out = cols.reshape(-1, 2, blk // 2)[:, ::-1, :].reshape(-1)
    return out


def build_wext_index():
    r = np.arange
    units = []
    units.append(r(0, 128)); units.append(r(128, 256))
    units.append(r(256, 384)); units.append(r(384, 512))
    units.append(r(1024, 1152)); units.append(r(1152, 1280))
    units.append(r(1280, 1408)); units.append(r(1408, 1536))
    gq = 1792
    units.append(np.concatenate([r(gq, gq + 64), r(gq + 128, gq + 192)]))
    units.append(np.concatenate([r(gq + 64, gq + 128), r(gq + 192, gq + 256)]))
    units.append(r(2048, 2176))
    units.append(np.tile(r(2624, 2656), 4))
    blks = [64, 64, 64, 64, 32, 32, 32, 32, 64, 64, 64, 32]
    sw = [_swap_blocks(u, b) for u, b in zip(units, blks)]
    feat = units + sw
    feat.append(r(2304, 2432))
    feat.append(r(2432, 2496))
    feat.append(r(2496, 2624))
    cols = []
    for u in feat:
        if len(u) < 128:
            u = np.concatenate([u, np.zeros(128 - len(u), np.int64)])
        cols.append(u)
    tokc = np.concatenate([r(512, 1024), r(1536, 1792), r(2176, 2304)])
    return np.concatenate(cols + [tokc]).astype(np.int64)


WEXT_IDX = build_wext_index()
NFEAT_UNITS = 27
NWEXT = WEXT_IDX.shape[0]
TOK_OFF = NFEAT_UNITS * 128
U_RQ, U_RK, U_DQ, U_DK, U_GQ, U_GK, U_MKR = 0, 2, 4, 6, 8, 10, 11
U_SW = 12
U_CQ0, U_CQ1, U_CKV = 24, 25, 26

_r = np.arange
WUQ_IDX = np.concatenate([
    _r(0, 64), _r(96, 160), _r(192, 256), _r(288, 352),
    _r(64, 96), _r(160, 192), _r(256, 288), _r(352, 384),
    _swap_blocks(np.concatenate([_r(64, 96), _r(160, 192), _r(256, 288), _r(352, 384)]), 32)])
WUKV_IDX = np.concatenate([
    _r(0, 64), _r(128, 192), _r(256, 320), _r(384, 448),
    _r(64, 128), _r(192, 256), _r(320, 384), _r(448, 512)])

RV_G1, RV_G3, RV_GNW, RV_GNB, RV_SUBLN, RV_ADAB, RV_RB, RV_DECAY, RV_LAM, RV_LAMINIT = (
    0, 1024, 2048, 2304, 2560, 2624, 8768, 8800, 8808, 8936)
RV_G0, RV_G2 = 8944, 9968
NRV = 10992
CV_GQG, CV_GQGS, CV_GKG, CV_GKGS, CV_QN0, CV_QN1, CV_KVG, CV_G0, CV_G2 = 0, 1, 2, 3, 4, 5, 6, 8, 16
NCV = 24

CM_ID, CM_DPOS, CM_DNEG, CM_MF, CM_MB, CM_LTRI, CM_ONES, CM_NP1, CM_NREV, CM_BLK64, CM_IOTA, CM_PCOL, CM_PREV, CM_SEL = (
    0, 128, 256, 384, 512, 640, 768, 896, 1024, 1152, 1280, 1312, 1313, 1314)
NCM = 1314 + 256


def build_consts():
    cm = np.zeros((128, NCM), np.float32)
    m = np.arange(128)[:, None].astype(np.float32)
    n = np.arange(128)[None, :].astype(np.float32)
    cm[:, CM_ID:CM_ID + 128] = np.eye(128)
    cm[:, CM_DPOS:CM_DPOS + 128] = np.maximum(n - m, 0)
    cm[:, CM_DNEG:CM_DNEG + 128] = np.maximum(m - n, 0)
    cm[:, CM_MF:CM_MF + 128] = (n >= m)
    cm[:, CM_MB:CM_MB + 128] = (m >= n)
    cm[:, CM_LTRI:CM_LTRI + 128] = (m < n)
    cm[:, CM_ONES:CM_ONES + 128] = 1.0
    cm[:, CM_NP1:CM_NP1 + 128] = n + 1.0
    cm[:, CM_NREV:CM_NREV + 128] = 128.0 - n
    cm[:, CM_BLK64:CM_BLK64 + 128] = ((np.arange(128)[:, None] // 64) == (np.arange(128)[None, :] // 64)) / 64.0
    cm[:, CM_IOTA:CM_IOTA + 32] = np.arange(32)[None, :]
    cm[:, CM_PCOL] = np.arange(128)
    cm[:, CM_PREV] = 127.0 - np.arange(128)
    cm[0, CM_SEL:CM_SEL + 128] = 1.0
    cm[1, CM_SEL + 128:CM_SEL + 256] = 1.0
    rows = NLAT // GRID_W
    row = np.repeat(np.arange(rows), GRID_W).astype(np.float32)
    colp = np.tile(np.arange(GRID_W), rows).astype(np.float32)
    tabs = np.zeros((4, 128, T), np.float32)
    for ti, rot in ((0, 64), (2, 32)):
        nf = rot // 4
        half = rot // 2
        inv = (10000.0 ** (-np.arange(nf, dtype=np.float32) / nf)).astype(np.float32)
        ang = np.concatenate([row[:, None] * inv, colp[:, None] * inv], axis=-1).astype(np.float32)
        cos = np.cos(ang).astype(np.float32)
        sin = np.sin(ang).astype(np.float32)
        f = np.arange(128)
        j = f % half
        sign = np.where((f % rot) < half, -1.0, 1.0).astype(np.float32)
        tabs[ti, :, :NCTX] = 1.0
        tabs[ti, :, NCTX:] = cos[:, j].T
        tabs[ti + 1, :, NCTX:] = (sin[:, j] * sign[None, :]).T
    return cm, tabs


def prep_layer_arrays(inp, ls):
    f = lambda k: np.asarray(inp[k], np.float32)
    w_in = f("w_in")[ls]
    L = len(ls)
    out = {}
    out["wext"] = np.ascontiguousarray(w_in[:, :, WEXT_IDX])
    out["wuq"] = np.ascontiguousarray(f("mla_w_uq")[ls][:, :, WUQ_IDX])
    out["wukv"] = np.ascontiguousarray(f("mla_w_ukv")[ls][:, :, WUKV_IDX])
    out["adaw"] = np.ascontiguousarray(f("ada_w")[ls])
    out["wout"] = np.ascontiguousarray(f("w_out")[ls])
    out["wr"] = np.ascontiguousarray(f("router_w")[ls])
    out["w1"] = np.ascontiguousarray(f("exp_w1")[ls])
    out["w2"] = np.ascontiguousarray(f("exp_w2")[ls])
    out["b2"] = np.ascontiguousarray(f("exp_b2")[ls])
    b1 = f("exp_b1")[ls]
    b1 = b1.reshape(L, NEXP, 8, 128, 2).transpose(0, 1, 3, 2, 4).reshape(L, NEXP, 128, 16)
    out["b1c"] = np.ascontiguousarray(b1)
    rv = np.zeros((L, NRV), np.float32)
    cv = np.zeros((L, 128, NCV), np.float32)
    ng = f("norm_g")[ls]
    for i, l in enumerate(ls):
        rv[i, RV_G1:RV_G1 + 1024] = ng[i, 1]
        rv[i, RV_G3:RV_G3 + 1024] = ng[i, 3]
        rv[i, RV_G0:RV_G0 + 1024] = ng[i, 0]
        rv[i, RV_G2:RV_G2 + 1024] = ng[i, 2]
        rv[i, RV_GNW:RV_GNW + 256] = f("ret_gn_w")[l]
        rv[i, RV_GNB:RV_GNB + 256] = f("ret_gn_b")[l]
        rv[i, RV_SUBLN:RV_SUBLN + 64] = f("diff_subln")[l]
        rv[i, RV_ADAB:RV_ADAB + 6144] = f("ada_b")[l]
        rv[i, RV_RB:RV_RB + 32] = f("router_b")[l]
        rv[i, RV_DECAY:RV_DECAY + 8] = f("ret_log_decay")[l].reshape(-1)
        rv[i, RV_LAM:RV_LAM + 128] = f("diff_lambda")[l].reshape(-1)
        rv[i, RV_LAMINIT] = 0.8 - 0.6 * math.exp(-0.3 * l)
        qk = f("gqa_qk_norm")[l]
        sw = _swap_blocks(np.arange(64), 64)
        cv[i, :, CV_GQG] = np.tile(qk[0], 2)
        cv[i, :, CV_GQGS] = np.tile(qk[0][sw], 2)
        cv[i, :, CV_GKG] = np.tile(qk[1], 2)
        cv[i, :, CV_GKGS] = np.tile(qk[1][sw], 2)
        qn = f("mla_q_norm")[l]
        cv[i, :, CV_QN0] = qn[:128]
        cv[i, :64, CV_QN1] = qn[128:]
        cv[i, :, CV_KVG] = f("mla_kv_norm")[l]
        cv[i, :, CV_G0:CV_G0 + 8] = _col(ng[i, 0])
        cv[i, :, CV_G2:CV_G2 + 8] = _col(ng[i, 2])
    out["rowv"] = rv
    out["colv"] = cv
    return out


class Prog:
    def __init__(self, L, debug=(), as_input=()):
        self.L = L
        self.debug = set(debug)
        self.as_input = set(as_input)
        nc = self.nc = bass.Bass("TRN2", target_bir_lowering=False)
        di = lambda n, s, d=F32: nc.dram_tensor(n, list(s), d, kind="ExternalInput").ap()
        self.xin = di("xin", [T, D])
        self.cvec = di("cvec", [128, 16])
        self.cmat = di("cmat", [128, NCM])
        self.ropet = di("ropet", [4, 128, T])
        self.adaw = di("adaw", [L, D, 6 * D])
        self.rowv = di("rowv", [L, NRV])
        self.colv = di("colv", [L, 128, NCV])
        self.wext = di("wext", [L, D, NWEXT])
        self.wuq = di("wuq", [L, 192, 512])
        self.wukv = di("wukv", [L, 128, 512])
        self.wout = di("wout", [L, D, D])
        self.wr = di("wr", [L, D, NEXP])
        self.w1 = di("w1", [L, NEXP, D, 2 * D])
        self.b1c = di("b1c", [L, NEXP, 128, 16])
        self.w2 = di("w2", [L, NEXP, D, D])
        self.b2 = di("b2", [L, NEXP, D])
        self.yout = nc.dram_tensor("yout", [T, D], F32, kind="ExternalOutput").ap()
        self.dbg = {}
        sc = lambda n, s, d: self._scratch(n, s, d)
        self.xres = sc("xres", [T, D], F32)
        self.fm = sc("fm", [17, 128, T], BF16)
        self.tokv = sc("tokv", [T, 1152], BF16)
        self.mixd = sc("mixd", [T, D], BF16)
        self.xg = sc("xg", [NSLOT + 128, D], BF16)
        self.yg = sc("yg", [NSLOT + 128, D], F32)
        self.modd = sc("modd", [L, 2, 6 * D], F32)

    def _scratch(self, n, s, d):
        if n in getattr(self, "as_input", ()):
            return self.nc.dram_tensor(n, list(s), d, kind="ExternalInput").ap()
        kind = "ExternalOutput" if n in self.debug else "Internal"
        t = self.nc.dram_tensor(n, list(s), d, kind=kind).ap()
        if n in self.debug:
            self.dbg[n] = t
        return t


FM = dict(rq0=0, rq1=1, rk0=2, rk1=3, dq0=4, dq1=5, dk0=6, dk1=7, gq0=8, gq1=9, gk=10,
          mqn0=11, mqn1=12, mqr=13, mkn0=14, mkn1=15, mkr=16)
TV_RV, TV_RG, TV_DV, TV_GV, TV_MV = 0, 256, 512, 768, 896


ATT_GROUPS = ("diff", "gqa", "mla")
RET_STOP = int(os.environ.get("RET_STOP", "0"))
ATT_MAXMAPS = None


def build_program(L, debug=(), stop_after=None, as_input=(), start_at=None):
    P = Prog(L, debug, as_input)
    nc = P.nc
    with ExitStack() as top:
        S = Sched(nc, top)
        uid = [0]

        def sbt(st, n, s, d):
            uid[0] += 1
            return st.enter_context(nc.sbuf_tensor("%s_s%d" % (n, uid[0]), list(s), d))

        def pst(st, n, s, d):
            uid[0] += 1
            return st.enter_context(nc.psum_tensor("%s_p%d" % (n, uid[0]), list(s), d))
        cm = sbt(top, "cm", [128, NCM], F32)
        cmb = sbt(top, "cmb", [128, NCM], BF16)
        cvec = sbt(top, "cvec", [128, 16], F32)
        R_cm, R_cmb, R_cvec, R_hT = Res("cm"), Res("cmb"), Res("cvec"), Res("hT")
        S.dma("sp", cm[:], P.cmat[:, :], writes=[R_cm])
        S.dma("pool", cmb[:], P.cmat[:, :], writes=[R_cmb])
        S.dma("sp", cvec[:], P.cvec[:, :], writes=[R_cvec])
        R_xres = [Res("xres%d" % i) for i in range(NT)]
        for i in range(NT):
            S.dma("sp" if i % 2 else "act", P.xres[i * 128:(i + 1) * 128, :], P.xin[i * 128:(i + 1) * 128, :],
                  writes=[R_xres[i]])
        with ExitStack() as zst:
            zt = sbt(zst, "zt", [128, 8 * D], BF16)
            R_z = Res()
            S.op("pool", lambda e: e.memset(zt[:], 0.0), writes=[R_z])
            for j in range(NSLOT // 1024):
                S.dma("sp" if j % 2 else "act", P.xg[j * 1024:(j + 1) * 1024, :].rearrange("(p a) d -> p (a d)", a=8), zt[:], reads=[R_z], writes=[Res()])
            ztf = sbt(zst, "ztf", [128, D], F32)
            S.op("pool", lambda e: e.memset(ztf[:], 0.0), writes=[R_z])
            S.dma("sp", P.yg[NSLOT:NSLOT + 128, :], ztf[:], reads=[R_z], writes=[Res()])
            S.drain()
        S.op("act", lambda e: e.activation(out=cvec[:], in_=cvec[:], func=AF.Silu), reads=[R_cvec], writes=[R_cvec])
        S.drain()
        ctx = dict(P=P, S=S, sbt=sbt, pst=pst, cm=cm, cmb=cmb, cvec=cvec, hT=None, R_cm=R_cm, R_cmb=R_cmb,
                   R_cvec=R_cvec, R_hT=R_hT, R_xres=R_xres, nc=nc)
        ctx["start_at"] = start_at
        for l in range(L):
            layer(ctx, l, first=(l == 0), stop_after=stop_after)
            if stop_after:
                break
        if not stop_after:
            R_out = Res("yout")
            for i in range(NT):
                S.dma("sp" if i % 2 else "act", P.yout[i * 128:(i + 1) * 128, :], P.xres[i * 128:(i + 1) * 128, :],
                      reads=[R_xres[i]], writes=[R_out] if i == 0 else [Res("o%d" % i)])
        S.drain()
    return P


def bc(ap, parts):
    return ap.to_broadcast([parts, ap.shape[-1]])


def layer(c, l, first, stop_after=None):
    P, S, nc = c["P"], c["S"], c["nc"]
    sa = c.get("start_at")
    phase_adaln(c, l)
    S.drain()
    if stop_after == "A":
        return
    if sa is None:
        with ExitStack() as hst:
            c["hT"] = c["sbt"](hst, "hT", [128, 8, T], BF16)
            phase_modulate(c, l, which="a")
            S.drain()
            phase_proj(c, l)
            S.drain()
        c["hT"] = None
    if stop_after == "C":
        return
    if sa in (None, "R"):
        phase_retention(c, l)
        S.drain()
    if stop_after == "R":
        return
    if sa in (None, "R", "AT"):
        phase_attention(c, l)
    S.drain()
    if stop_after == "AT":
        return
    phase_outproj(c, l)
    S.drain()
    if stop_after == "E":
        return
    with ExitStack() as st:
        rt = phase_modulate(c, l, which="f", keep=st)
        S.drain()
        phase_experts(c, l)
        S.drain()
        if stop_after == "F":
            return
        phase_combine(c, l, rt)
        S.drain()


def phase_adaln(c, l):
    P, S, nc, sbt, pst = c["P"], c["S"], c["nc"], c["sbt"], c["pst"]
    cvec, R_cvec = c["cvec"], c["R_cvec"]
    with ExitStack() as st:
        aw = [sbt(st, "aw%d" % i, [128, 8, 512], F32) for i in range(2)]
        R_aw = [Res("aw0"), Res("aw1")]
        msb = sbt(st, "msb", [2, 6 * D], F32)
        adab = sbt(st, "adab", [2, 6 * D], F32)
        pm = [pst(st, "pm%d" % i, [2, 512], F32) for i in range(2)]
        R_pm = [Res("pm0"), Res("pm1")]
        R_msb, R_adab = Res("msb"), Res("adab")
        S.dma("act", adab[:], bc(P.rowv[l:l + 1, RV_ADAB:RV_ADAB + 6 * D], 2), writes=[R_adab])
        src = P.adaw[l].rearrange("(c p) n -> p c n", p=128)
        for j in range(12):
            b = j % 2
            S.dma("sp", aw[b][:], src[:, :, j * 512:(j + 1) * 512], writes=[R_aw[b]])
            for h0 in (0, 256):
                for k in range(8):
                    S.op("pe", lambda e, k=k, b=b, h0=h0: e.matmul(pm[b][:, h0:h0 + 256], lhsT=cvec[:, k::8], rhs=aw[b][:, k, h0:h0 + 256],
                                                                 start=(k == 0), stop=(k == 7)),
                         reads=[R_cvec, R_aw[b]], writes=[R_pm[b]])
            S.op("dve", lambda e, j=j, b=b: e.tensor_tensor(out=msb[:, j * 512:(j + 1) * 512], in0=pm[b][:],
                                                           in1=adab[:, j * 512:(j + 1) * 512], op=ALU.add),
                 reads=[R_pm[b], R_adab], writes=[R_msb])
        R_modd = c.setdefault("R_modd", {})
        R_modd[l] = Res("modd%d" % l)
        S.dma("sp", P.modd[l], msb[:], reads=[R_msb], writes=[R_modd[l]])
        S.drain()


def load_mod_bcast(c, l, st, part_sc, part_sh, rv_g, name):
    P, S, sbt = c["P"], c["S"], c["sbt"]
    gsc = sbt(st, "gsc" + name, [128, 2, D], F32)
    sh = sbt(st, "sh" + name, [128, 2, D], F32)
    gb = sbt(st, "gb" + name, [128, D], F32)
    R1, R2, R3 = Res("gsc"), Res("sh"), Res("gb")
    S.dma("sp", gb[:], bc(P.rowv[l:l + 1, rv_g:rv_g + D], 128), writes=[R3])
    for r in range(2):
        S.dma("sp", gsc[:, r, :], bc(P.modd[l, r:r + 1, part_sc * D:(part_sc + 1) * D], 128),
              reads=[c["R_modd"][l]], writes=[R1])
        S.dma("act", sh[:, r, :], bc(P.modd[l, r:r + 1, part_sh * D:(part_sh + 1) * D], 128),
              reads=[c["R_modd"][l]], writes=[R2])
        S.op("dve", lambda e, r=r: e.scalar_tensor_tensor(out=gsc[:, r, :], in0=gsc[:, r, :], scalar=1.0, in1=gb[:],
                                                         op0=ALU.add, op1=ALU.mult),
             reads=[R1, R3], writes=[R1])
    return gsc, sh, R1, R2


def load_gate_bcast(c, l, st, part_g, rv_g, name):
    P, S, sbt = c["P"], c["S"], c["sbt"]
    G = sbt(st, "G" + name, [128, 2, D], F32)
    gb = sbt(st, "Gb" + name, [128, D], F32)
    R1, R3 = Res("G"), Res("Gb")
    S.dma("sp", gb[:], bc(P.rowv[l:l + 1, rv_g:rv_g + D], 128), writes=[R3])
    for r in range(2):
        S.dma("act", G[:, r, :], bc(P.modd[l, r:r + 1, part_g * D:(part_g + 1) * D], 128),
              reads=[c["R_modd"][l]], writes=[R1])
        S.op("dve", lambda e, r=r: e.tensor_tensor(out=G[:, r, :], in0=G[:, r, :], in1=gb[:], op=ALU.mult),
             reads=[R1, R3], writes=[R1])
    return G, R1


def rms_rstd(S, e_reads, x_ap, n, junk, R_junk, ss, R_ss, rstd, R_rstd):
    S.op("act", lambda e: e.activation(out=junk, in_=x_ap, func=AF.Square, accum_out=ss),
         reads=e_reads, writes=[R_junk, R_ss])
    S.op("dve", lambda e: e.tensor_scalar(out=rstd, in0=ss, scalar1=1.0 / n, scalar2=EPS, op0=ALU.mult, op1=ALU.add),
         reads=[R_ss], writes=[R_rstd])
    S.op("act", lambda e: e.activation(out=rstd, in_=rstd, func=AF.Sqrt), reads=[R_rstd], writes=[R_rstd])
    S.op("dve", lambda e: e.reciprocal(out=rstd, in_=rstd), reads=[R_rstd], writes=[R_rstd])


def phase_modulate(c, l, which, keep=None):
    P, S, nc, sbt, pst = c["P"], c["S"], c["nc"], c["sbt"], c["pst"]
    cm, R_cm, hT, R_hT, R_xres = c["cm"], c["R_cm"], c["hT"], c["R_hT"], c["R_xres"]
    route = which == "f"
    rt = None
    if route:
        rt = dict()
        rt["gates"] = sbt(keep, "gatesall", [128, NT, 4], F32)
        rt["slots"] = sbt(keep, "slotsall", [128, NT, 4], U32)
        rt["R_gates"] = Res(); rt["R_slots"] = Res()
    with ExitStack() as st:
        if which == "a":
            gsc, sh, R_gsc, R_sh = load_mod_bcast(c, l, st, 1, 0, RV_G0, "a")
        else:
            gsc, sh, R_gsc, R_sh = load_mod_bcast(c, l, st, 4, 3, RV_G2, "f")
        NB = 2
        xt = [sbt(st, "xt%d" % i, [128, D], F32) for i in range(NB)]
        hx = [sbt(st, "hx%d" % i, [128, D], F32) for i in range(NB)]
        junk = sbt(st, "junk", [128, D], F32)
        ss = [sbt(st, "ss%d" % i, [128, 1], F32) for i in range(NB)]
        rstd = [sbt(st, "rstd%d" % i, [128, 1], F32) for i in range(NB)]
        ptr = [pst(st, "ptr%d" % i, [128, 8, 128], F32) for i in range(NB)]
        R_xt = [Res() for _ in range(NB)]; R_hx = [Res() for _ in range(NB)]; R_junk = Res()
        R_ss = [Res() for _ in range(NB)]; R_rstd = [Res() for _ in range(NB)]; R_ptr = [Res() for _ in range(NB)]
        if route:
            h32 = [sbt(st, "h32_%d" % i, [128, 8, 128], F32) for i in range(NB)]
            hb = [sbt(st, "hb%d" % i, [128, D], BF16) for i in range(NB)]
            R_h32 = [Res() for _ in range(NB)]; R_hb = [Res() for _ in range(NB)]
            wr = sbt(st, "wr", [128, 8, NEXP], F32)
            rb = sbt(st, "rb", [1, NEXP], F32)
            R_wr, R_rb = Res(), Res()
            S.dma("sp", wr[:], P.wr[l].rearrange("(c p) n -> p c n", p=128), writes=[R_wr])
            S.dma("sp", rb[:], P.rowv[l:l + 1, RV_RB:RV_RB + NEXP], writes=[R_rb])
            plg = [pst(st, "plg%d" % i, [128, NEXP], F32) for i in range(NB)]
            ppos = [pst(st, "ppos%d" % i, [128, NEXP], F32) for i in range(NB)]
            R_plg = [Res() for _ in range(NB)]; R_ppos = [Res() for _ in range(NB)]
            lg = sbt(st, "lg", [128, NEXP], F32); m8 = sbt(st, "m8", [128, 8], F32); i8 = sbt(st, "i8", [128, 8], U32)
            idxf = sbt(st, "idxf", [128, 4], F32); negm = sbt(st, "negm", [128, 1], F32)
            e4 = sbt(st, "e4", [128, 4], F32); se = sbt(st, "se", [128, 1], F32)
            oh = sbt(st, "oh", [128, 4, NEXP], F32); mask = sbt(st, "mask", [128, NEXP], F32)
            cum = sbt(st, "cum", [128, NEXP], F32); posk = sbt(st, "posk", [128, 4], F32)
            j32 = sbt(st, "j32", [128, NEXP], F32); slf = sbt(st, "slf", [128, 4], F32); valid = sbt(st, "valid", [128, 4], F32)
            R_r = Res("route_tmp"); R_cum = Res("cum"); R_mask = Res("mask")
            S.op("pool", lambda e: e.memset(cum[:], 0.0), writes=[R_cum])
            R_xg = c.setdefault("R_xg", Res("xg"))
            rt["R_scatter"] = []
        for i in range(NT):
            b = i % NB
            r = 1 if i < 2 else 0
            S.dma("sp" if i % 2 else "act", xt[b][:], P.xres[i * 128:(i + 1) * 128, :], reads=[R_xres[i]], writes=[R_xt[b]])
            rms_rstd(S, [R_xt[b]], xt[b][:], D, junk[:], R_junk, ss[b][:], R_ss[b], rstd[b][:], R_rstd[b])
            S.op("dve", lambda e, b=b, r=r: e.scalar_tensor_tensor(out=hx[b][:], in0=xt[b][:], scalar=rstd[b][:, 0:1],
                                                                   in1=gsc[:, r, :], op0=ALU.mult, op1=ALU.mult),
                 reads=[R_xt[b], R_rstd[b], R_gsc], writes=[R_hx[b]])
            S.op("pool", lambda e, b=b, r=r: e.tensor_tensor(out=hx[b][:], in0=hx[b][:], in1=sh[:, r, :], op=ALU.add),
                 reads=[R_hx[b], R_sh], writes=[R_hx[b]])
            for k in range(8):
                S.op("pe", lambda e, b=b, k=k: e.transpose(out=ptr[b][:, k, :], in_=hx[b][:, k * 128:(k + 1) * 128],
                                                          identity=cm[:, CM_ID:CM_ID + 128]),
                     reads=[R_hx[b], R_cm], writes=[R_ptr[b]])
            if not route:
                S.op("act", lambda e, b=b, i=i: e.activation(out=hT[:, :, i * 128:(i + 1) * 128], in_=ptr[b][:], func=AF.Copy),
                     reads=[R_ptr[b]], writes=[R_hT])
                continue
            S.op("act", lambda e, b=b: e.activation(out=h32[b][:], in_=ptr[b][:], func=AF.Copy),
                 reads=[R_ptr[b]], writes=[R_h32[b]])
            S.op("pool", lambda e, b=b: e.tensor_copy(out=hb[b][:], in_=hx[b][:]), reads=[R_hx[b]], writes=[R_hb[b]])
            for k in range(8):
                S.op("pe", lambda e, b=b, k=k: e.matmul(plg[b][:], lhsT=h32[b][:, k, :], rhs=wr[:, k, :], start=(k == 0), stop=False),
                     reads=[R_h32[b], R_wr], writes=[R_plg[b]])
            S.op("pe", lambda e, b=b: e.matmul(plg[b][:], lhsT=cm[0:1, CM_ONES:CM_ONES + 128], rhs=rb[:], start=False, stop=True),
                 reads=[R_cm, R_rb], writes=[R_plg[b]])
            gates_i = rt["gates"][:, i, :]
            S.op("dve", lambda e, b=b: e.tensor_copy(out=lg[:], in_=plg[b][:]), reads=[R_plg[b]], writes=[R_r])
            S.op("dve", lambda e: e.max(out=m8[:], in_=lg[:]), reads=[R_r], writes=[R_r])
            S.op("dve", lambda e: e.max_index(out=i8[:], in_max=m8[:], in_values=lg[:]), reads=[R_r], writes=[R_r])
            S.op("dve", lambda e: e.tensor_scalar(out=negm[:], in0=m8[:, 0:1], scalar1=-1.0, scalar2=None, op0=ALU.mult),
                 reads=[R_r], writes=[R_r])
            S.op("act", lambda e: e.activation(out=e4[:], in_=m8[:, 0:4], func=AF.Exp, bias=negm[:, 0:1], accum_out=se[:]),
                 reads=[R_r], writes=[R_r])
            S.op("dve", lambda e: e.reciprocal(out=se[:], in_=se[:]), reads=[R_r], writes=[R_r])
            S.op("dve", lambda e, g=gates_i: e.tensor_scalar(out=g, in0=e4[:], scalar1=se[:, 0:1], scalar2=None, op0=ALU.mult),
                 reads=[R_r], writes=[rt["R_gates"]])
            S.op("dve", lambda e: e.tensor_copy(out=idxf[:], in_=i8[:, 0:4]), reads=[R_r], writes=[R_r])
            for k in range(4):
                S.op("dve", lambda e, k=k: e.tensor_scalar(out=oh[:, k, :], in0=cm[:, CM_IOTA:CM_IOTA + NEXP], scalar1=idxf[:, k:k + 1],
                                                          scalar2=None, op0=ALU.is_equal),
                     reads=[R_r, R_cm], writes=[R_r])
            S.op("dve", lambda e: e.tensor_tensor(out=mask[:], in0=oh[:, 0, :], in1=oh[:, 1, :], op=ALU.add), reads=[R_r], writes=[R_mask])
            S.op("dve", lambda e: e.tensor_tensor(out=mask[:], in0=mask[:], in1=oh[:, 2, :], op=ALU.add), reads=[R_r, R_mask], writes=[R_mask])
            S.op("dve", lambda e: e.tensor_tensor(out=mask[:], in0=mask[:], in1=oh[:, 3, :], op=ALU.add), reads=[R_r, R_mask], writes=[R_mask])
            S.op("pe", lambda e, b=b: e.matmul(ppos[b][:], lhsT=cm[:, CM_LTRI:CM_LTRI + 128], rhs=mask[:], start=True, stop=False),
                 reads=[R_cm, R_mask], writes=[R_ppos[b]])
            S.op("pe", lambda e, b=b: e.matmul(ppos[b][:], lhsT=cm[:, CM_ONES:CM_ONES + 128], rhs=cum[:], start=False, stop=True),
                 reads=[R_cm, R_cum], writes=[R_ppos[b]])
            S.op("dve", lambda e: e.tensor_tensor(out=cum[:], in0=cum[:], in1=mask[:], op=ALU.add), reads=[R_mask, R_cum], writes=[R_cum])
            for k in range(4):
                S.op("dve", lambda e, b=b, k=k: e.tensor_tensor(out=j32[:], in0=oh[:, k, :], in1=ppos[b][:], op=ALU.mult),
                     reads=[R_r, R_ppos[b]], writes=[R_r])
                S.op("dve", lambda e, k=k: e.tensor_reduce(out=posk[:, k:k + 1], in_=j32[:], axis=AX.X, op=ALU.add),
                     reads=[R_r], writes=[R_r])
            S.op("dve", lambda e: e.scalar_tensor_tensor(out=slf[:], in0=idxf[:], scalar=float(CAP), in1=posk[:], op0=ALU.mult, op1=ALU.add),
                 reads=[R_r], writes=[R_r])
            S.op("dve", lambda e: e.tensor_scalar(out=valid[:], in0=posk[:], scalar1=float(CAP), scalar2=None, op0=ALU.is_lt), reads=[R_r], writes=[R_r])
            S.op("dve", lambda e: e.tensor_scalar(out=slf[:], in0=slf[:], scalar1=-float(NSLOT), scalar2=None, op0=ALU.add), reads=[R_r], writes=[R_r])
            S.op("dve", lambda e: e.tensor_tensor(out=slf[:], in0=slf[:], in1=valid[:], op=ALU.mult), reads=[R_r], writes=[R_r])
            S.op("dve", lambda e: e.tensor_scalar(out=slf[:], in0=slf[:], scalar1=float(NSLOT), scalar2=None, op0=ALU.add), reads=[R_r], writes=[R_r])
            S.op("dve", lambda e, g=gates_i: e.tensor_tensor(out=g, in0=g, in1=valid[:], op=ALU.mult), reads=[R_r, rt["R_gates"]], writes=[rt["R_gates"]])
            S.op("dve", lambda e, i=i: e.tensor_copy(out=rt["slots"][:, i, :], in_=slf[:]), reads=[R_r], writes=[rt["R_slots"]])
            for k in range(4):
                Rs = Res("sc")
                rt["R_scatter"].append(Rs)
                S.dma("pool", P.xg[:, :], hb[b][:], reads=[R_hb[b], rt["R_slots"]], writes=[Rs],
                      indirect=dict(out_offset=bass.IndirectOffsetOnAxis(ap=rt["slots"][:, i, k:k + 1], axis=0), in_offset=None))
        S.drain()
    return rt


def phase_proj(c, l):
    P, S, nc, sbt, pst = c["P"], c["S"], c["nc"], c["sbt"], c["pst"]
    cm, R_cm, hT, R_hT, cmb = c["cm"], c["R_cm"], c["hT"], c["R_hT"], c["cmb"]
    R_fm = c.setdefault("R_fm", Res("fm")); R_tokv = c.setdefault("R_tokv", Res("tokv"))
    with ExitStack() as st:
        sq = [sbt(st, "sq%d" % i, [128, 512], BF16) for i in range(2)]
        rs = [sbt(st, "rs%d" % i, [128, 512], F32) for i in range(2)]
        R_sq = [Res(), Res()]; R_rs = [Res(), Res()]
        cqn0 = sbt(st, "cqn0", [128, 512], BF16); cqn1 = sbt(st, "cqn1", [64, 512], BF16); ckvn = sbt(st, "ckvn", [128, 512], BF16)
        R_cqn, R_ckvn = Res(), Res()
        wx = sbt(st, "wx", [128, 8, NWEXT], BF16)
        wuqA = sbt(st, "wuqA", [128, 512], BF16); wuqB = sbt(st, "wuqB", [64, 512], BF16)
        wukv = sbt(st, "wukvx", [128, 512], BF16)
        colv = sbt(st, "colv", [128, NCV], F32)
        R_wx, R_w2, R_colv = Res("wx"), Res("w2nd"), Res("colv")
        src = P.wext[l].rearrange("(c p) n -> p c n", p=128)
        for k in range(8):
            for (a, b) in ((0, 2048), (2048, 4096), (4096, NWEXT)):
                S.dma("pool", wx[:, k, a:b], src[:, k, a:b], writes=[R_wx])
        S.dma("pool", wuqA[:], P.wuq[l, 0:128, :], writes=[R_w2])
        S.dma("pool", wuqB[:], P.wuq[l, 128:192, :], writes=[R_w2])
        S.dma("pool", wukv[:], P.wukv[l], writes=[R_w2])
        S.dma("sp", colv[:], P.colv[l], writes=[R_colv])
        tabs = [sbt(st, "tabs%d" % i, [128, 4, 512], F32) for i in range(2)]
        R_tabs = [Res(), Res()]
        NPS = 2
        pa = [pst(st, "pa%d" % i, [128, 512], F32) for i in range(NPS)]
        pb = [pst(st, "pb%d" % i, [128, 512], F32) for i in range(NPS)]
        pc = [pst(st, "pc%d" % i, [128, 512], F32) for i in range(NPS)]
        pd = [pst(st, "pd%d" % i, [128, 512], F32) for i in range(NPS)]
        R_pa = [Res() for _ in range(NPS)]; R_pb = [Res() for _ in range(NPS)]
        R_pc = [Res() for _ in range(NPS)]; R_pd = [Res() for _ in range(NPS)]
        NW = 3
        t1 = [sbt(st, "t1_%d" % i, [128, 512], F32) for i in range(NW)]
        t2 = [sbt(st, "t2_%d" % i, [128, 512], F32) for i in range(NW)]
        ob = [sbt(st, "ob%d" % i, [128, 512], BF16) for i in range(NW)]
        R_t1 = [Res() for _ in range(NW)]; R_t2 = [Res() for _ in range(NW)]; R_ob = [Res() for _ in range(NW)]
        tvs = [sbt(st, "tvs%d" % i, [128, 1152], BF16) for i in range(2)]
        R_tvs = [Res(), Res()]
        cnt = dict(w=0, a=0, b=0, c=0, d=0, ev=0, q=0)

        def nxt(key, n):
            v = cnt[key]; cnt[key] = (v + 1) % n
            return v

        def group(ps, R_ps, u, tok0, W, M=128, wtile=None, rhs=None, R_rhs=None):
            for k in range(8):
                S.op("pe", lambda e, k=k: e.matmul(ps[0:M, 0:W], lhsT=wx[:, k, u * 128:u * 128 + M], rhs=hT[:, k, tok0:tok0 + W],
                                                   start=(k == 0), stop=(k == 7)),
                     reads=[R_wx, R_hT], writes=[R_ps])

        def store_fm(name, src_tile, R_src, tok0, W, M=128):
            q = ("sp", "act")[nxt("q", 2)]
            S.dma(q, P.fm[FM[name], 0:M, tok0:tok0 + W], src_tile[0:M, 0:W], reads=[R_src], writes=[Res()])

        def rope_out(name, p1, R_p1, p2, R_p2, tb, tcos, tok0, W, g=None, gs=None, rstd=None, R_rstd=None):
            w = nxt("w", NW)
            cos = tabs[tb][:, tcos, 0:W]; sin = tabs[tb][:, tcos + 1, 0:W]
            if g is None:
                S.op("dve", lambda e: e.tensor_tensor(out=t1[w][:, 0:W], in0=p1[:, 0:W], in1=cos, op=ALU.mult),
                     reads=[R_p1, R_tabs[tb]], writes=[R_t1[w]])
                S.op("dve", lambda e: e.tensor_tensor(out=t2[w][:, 0:W], in0=p2[:, 0:W], in1=sin, op=ALU.mult),
                     reads=[R_p2, R_tabs[tb]], writes=[R_t2[w]])
                S.op("pool", lambda e: e.tensor_tensor(out=ob[w][:, 0:W], in0=t1[w][:, 0:W], in1=t2[w][:, 0:W], op=ALU.add),
                     reads=[R_t1[w], R_t2[w]], writes=[R_ob[w]])
            else:
                S.op("dve", lambda e: e.scalar_tensor_tensor(out=t1[w][:, 0:W], in0=p1[:, 0:W], scalar=g, in1=cos, op0=ALU.mult, op1=ALU.mult),
                     reads=[R_p1, R_tabs[tb], R_colv], writes=[R_t1[w]])
                S.op("dve", lambda e: e.scalar_tensor_tensor(out=t2[w][:, 0:W], in0=p2[:, 0:W], scalar=gs, in1=sin, op0=ALU.mult, op1=ALU.mult),
                     reads=[R_p2, R_tabs[tb], R_colv], writes=[R_t2[w]])
                S.op("pool", lambda e: e.tensor_tensor(out=t1[w][:, 0:W], in0=t1[w][:, 0:W], in1=t2[w][:, 0:W], op=ALU.add),
                     reads=[R_t1[w], R_t2[w]], writes=[R_t1[w]])
                S.op("pool", lambda e: e.tensor_tensor(out=ob[w][:, 0:W], in0=t1[w][:, 0:W], in1=rstd[:, 0:W], op=ALU.mult),
                     reads=[R_t1[w], R_rstd], writes=[R_ob[w]])
            store_fm(name, ob[w], R_ob[w], tok0, W)

        def rstd_from_ms(ps, R_ps, scale, si, W, M=128):
            S.op("dve", lambda e: e.tensor_scalar(out=rs[si][0:M, 0:W], in0=ps[0:M, 0:W], scalar1=scale, scalar2=EPS, op0=ALU.mult, op1=ALU.add),
                 reads=[R_ps], writes=[R_rs[si]])
            S.op("act", lambda e: e.activation(out=rs[si][0:M, 0:W], in_=rs[si][0:M, 0:W], func=AF.Sqrt), reads=[R_rs[si]], writes=[R_rs[si]])
            S.op("dve", lambda e: e.reciprocal(out=rs[si][0:M, 0:W], in_=rs[si][0:M, 0:W]), reads=[R_rs[si]], writes=[R_rs[si]])

        def do_block(bi, tok0, W):
            tb = bi % 2
            S.dma("sp", tabs[tb][:, :, 0:W], P.ropet[:, :, tok0:tok0 + W].rearrange("t p n -> p t n"), writes=[R_tabs[tb]])
            for name, u, tc in (("rq0", 0, 0), ("rq1", 1, 0), ("rk0", 2, 0), ("rk1", 3, 0),
                                ("dq0", 4, 2), ("dq1", 5, 2), ("dk0", 6, 2), ("dk1", 7, 2), ("mkr", 11, 2)):
                a = nxt("a", NPS); b = nxt("b", NPS)
                group(pa[a], R_pa[a], u, tok0, W)
                group(pb[b], R_pb[b], u + U_SW, tok0, W)
                rope_out(name, pa[a], R_pa[a], pb[b], R_pb[b], tb, tc, tok0, W)
            for name, u, cg in (("gq0", 8, CV_GQG), ("gq1", 9, CV_GQG), ("gk", 10, CV_GKG)):
                a = nxt("a", NPS); b = nxt("b", NPS); cc = nxt("c", NPS)
                group(pa[a], R_pa[a], u, tok0, W)
                group(pb[b], R_pb[b], u + U_SW, tok0, W)
                S.op("act", lambda e, a=a: e.activation(out=sq[0][:, 0:W], in_=pa[a][:, 0:W], func=AF.Square),
                     reads=[R_pa[a]], writes=[R_sq[0]])
                for h0 in range(0, W, 256):
                    S.op("pe", lambda e, cc=cc, h0=h0: e.matmul(pc[cc][:, h0:h0 + 256], lhsT=cmb[:, CM_BLK64:CM_BLK64 + 128], rhs=sq[0][:, h0:h0 + 256], start=True, stop=True),
                         reads=[R_cm, R_sq[0]], writes=[R_pc[cc]])
                if "dbgC" in P.debug and bi == 0 and name == "gq0":
                    dd = nc.dram_tensor("dbgC", [3, 128, 512], F32, kind="ExternalOutput").ap()
                    S.op("dve", lambda e, cc=cc: e.tensor_copy(out=t1[0][:], in_=pc[cc][:]), reads=[R_pc[cc]], writes=[R_t1[0]])
                    S.dma("sp", dd[1], t1[0][:], reads=[R_t1[0]], writes=[Res()])
                    S.op("dve", lambda e, a=a: e.tensor_copy(out=t2[0][:], in_=pa[a][:]), reads=[R_pa[a]], writes=[R_t2[0]])
                    S.dma("sp", dd[2], t2[0][:], reads=[R_t2[0]], writes=[Res()])
                rstd_from_ms(pc[cc], R_pc[cc], 1.0, 0, W)
                rope_out(name, pa[a], R_pa[a], pb[b], R_pb[b], tb, 0, tok0, W, g=colv[:, cg:cg + 1], gs=colv[:, cg + 1:cg + 2],
                         rstd=rs[0], R_rstd=R_rs[0])
            a = nxt("a", NPS); b = nxt("b", NPS); cc = nxt("c", NPS)
            group(pa[a], R_pa[a], U_CQ0, tok0, W)
            group(pb[b], R_pb[b], U_CQ1, tok0, W, M=64)
            S.op("act", lambda e, a=a: e.activation(out=sq[0][:, 0:W], in_=pa[a][:, 0:W], func=AF.Square), reads=[R_pa[a]], writes=[R_sq[0]])
            S.op("act", lambda e, b=b: e.activation(out=sq[1][0:64, 0:W], in_=pb[b][0:64, 0:W], func=AF.Square), reads=[R_pb[b]], writes=[R_sq[1]])
            for h0 in range(0, W, 256):
                S.op("pe", lambda e, cc=cc, h0=h0: e.matmul(pc[cc][:, h0:h0 + 256], lhsT=cmb[:, CM_ONES:CM_ONES + 128], rhs=sq[0][:, h0:h0 + 256], start=True, stop=False),
                     reads=[R_cm, R_sq[0]], writes=[R_pc[cc]])
                S.op("pe", lambda e, cc=cc, h0=h0: e.matmul(pc[cc][:, h0:h0 + 256], lhsT=cmb[0:64, CM_ONES:CM_ONES + 128], rhs=sq[1][0:64, h0:h0 + 256], start=False, stop=True),
                     reads=[R_cm, R_sq[1]], writes=[R_pc[cc]])
            rstd_from_ms(pc[cc], R_pc[cc], 1.0 / 192, 0, W)
            S.op("dve", lambda e, a=a: e.scalar_tensor_tensor(out=cqn0[:, 0:W], in0=pa[a][:, 0:W], scalar=colv[:, CV_QN0:CV_QN0 + 1], in1=rs[0][:, 0:W],
                                                             op0=ALU.mult, op1=ALU.mult), reads=[R_pa[a], R_rs[0], R_colv], writes=[R_cqn])
            S.op("dve", lambda e, b=b: e.scalar_tensor_tensor(out=cqn1[:, 0:W], in0=pb[b][0:64, 0:W], scalar=colv[0:64, CV_QN1:CV_QN1 + 1], in1=rs[0][0:64, 0:W],
                                                             op0=ALU.mult, op1=ALU.mult), reads=[R_pb[b], R_rs[0], R_colv], writes=[R_cqn])
            a = nxt("a", NPS); cc = nxt("c", NPS)
            group(pa[a], R_pa[a], U_CKV, tok0, W)
            S.op("act", lambda e, a=a: e.activation(out=sq[0][:, 0:W], in_=pa[a][:, 0:W], func=AF.Square), reads=[R_pa[a]], writes=[R_sq[0]])
            for h0 in range(0, W, 256):
                S.op("pe", lambda e, cc=cc, h0=h0: e.matmul(pc[cc][:, h0:h0 + 256], lhsT=cmb[:, CM_ONES:CM_ONES + 128], rhs=sq[0][:, h0:h0 + 256], start=True, stop=True),
                     reads=[R_cm, R_sq[0]], writes=[R_pc[cc]])
            rstd_from_ms(pc[cc], R_pc[cc], 1.0 / 128, 1, W)
            S.op("dve", lambda e, a=a: e.scalar_tensor_tensor(out=ckvn[:, 0:W], in0=pa[a][:, 0:W], scalar=colv[:, CV_KVG:CV_KVG + 1], in1=rs[1][:, 0:W],
                                                             op0=ALU.mult, op1=ALU.mult), reads=[R_pa[a], R_rs[1], R_colv], writes=[R_ckvn])

            def uq(ps, R_ps, c0):
                S.op("pe", lambda e: e.matmul(ps[:, 0:W], lhsT=wuqA[:, c0:c0 + 128], rhs=cqn0[:, 0:W], start=True, stop=False),
                     reads=[R_w2, R_cqn], writes=[R_ps])
                S.op("pe", lambda e: e.matmul(ps[:, 0:W], lhsT=wuqB[:, c0:c0 + 128], rhs=cqn1[:, 0:W], start=False, stop=True),
                     reads=[R_w2, R_cqn], writes=[R_ps])

            def plain_out(name, ps, R_ps):
                w = nxt("w", NW)
                if nxt("ev", 2):
                    S.op("act", lambda e: e.activation(out=ob[w][:, 0:W], in_=ps[:, 0:W], func=AF.Copy), reads=[R_ps], writes=[R_ob[w]])
                else:
                    S.op("dve", lambda e: e.tensor_copy(out=ob[w][:, 0:W], in_=ps[:, 0:W]), reads=[R_ps], writes=[R_ob[w]])
                store_fm(name, ob[w], R_ob[w], tok0, W)

            for name, c0 in (("mqn0", 0), ("mqn1", 128)):
                d = nxt("d", NPS)
                uq(pd[d], R_pd[d], c0)
                plain_out(name, pd[d], R_pd[d])
            a = nxt("a", NPS); b = nxt("b", NPS)
            uq(pa[a], R_pa[a], 256)
            uq(pb[b], R_pb[b], 384)
            rope_out("mqr", pa[a], R_pa[a], pb[b], R_pb[b], tb, 2, tok0, W)
            for name, c0 in (("mkn0", 0), ("mkn1", 128)):
                d = nxt("d", NPS)
                S.op("pe", lambda e, d=d, c0=c0: e.matmul(pd[d][:, 0:W], lhsT=wukv[:, c0:c0 + 128], rhs=ckvn[:, 0:W], start=True, stop=True),
                     reads=[R_w2, R_ckvn], writes=[R_pd[d]])
                plain_out(name, pd[d], R_pd[d])
            for j in range(W // 128):
                t0 = tok0 + j * 128
                v = (t0 // 128) % 2
                d = nxt("d", NPS); cc = nxt("c", NPS); a = nxt("a", NPS)
                for k in range(8):
                    S.op("pe", lambda e, k=k, d=d, t0=t0: e.matmul(pd[d][:, 0:512], lhsT=hT[:, k, t0:t0 + 128], rhs=wx[:, k, TOK_OFF:TOK_OFF + 512],
                                                                 start=(k == 0), stop=(k == 7)), reads=[R_wx, R_hT], writes=[R_pd[d]])
                for k in range(8):
                    S.op("pe", lambda e, k=k, cc=cc, t0=t0: e.matmul(pc[cc][:, 0:384], lhsT=hT[:, k, t0:t0 + 128], rhs=wx[:, k, TOK_OFF + 512:TOK_OFF + 896],
                                                                   start=(k == 0), stop=(k == 7)), reads=[R_wx, R_hT], writes=[R_pc[cc]])
                S.op("pe", lambda e, a=a, j=j: e.matmul(pa[a][:, 0:256], lhsT=ckvn[:, j * 128:(j + 1) * 128], rhs=wukv[:, 256:512], start=True, stop=True),
                     reads=[R_w2, R_ckvn], writes=[R_pa[a]])
                S.op("dve", lambda e, d=d, v=v: e.tensor_copy(out=tvs[v][:, TV_RV:TV_RV + 256], in_=pd[d][:, 0:256]), reads=[R_pd[d]], writes=[R_tvs[v]])
                S.op("act", lambda e, d=d, v=v: e.activation(out=tvs[v][:, TV_RG:TV_RG + 256], in_=pd[d][:, 256:512], func=AF.Silu), reads=[R_pd[d]], writes=[R_tvs[v]])
                S.op("dve", lambda e, cc=cc, v=v: e.tensor_copy(out=tvs[v][:, TV_DV:TV_DV + 384], in_=pc[cc][:, 0:384]), reads=[R_pc[cc]], writes=[R_tvs[v]])
                S.op("act", lambda e, a=a, v=v: e.activation(out=tvs[v][:, TV_MV:TV_MV + 256], in_=pa[a][:, 0:256], func=AF.Copy), reads=[R_pa[a]], writes=[R_tvs[v]])
                S.dma("sp", P.tokv[t0:t0 + 128, :], tvs[v][:], reads=[R_tvs[v]], writes=[Res()])

        for bi, (tok0, W) in enumerate(TB):
            do_block(bi, tok0, W)
        S.drain()


QBLOCKS = [(0, 256, 2)] + [(256 + 512 * j, 512, NT) for j in range(8)]


def phase_attention(c, l):
    P, S, nc, sbt, pst = c["P"], c["S"], c["nc"], c["sbt"], c["pst"]
    for grp in ATT_GROUPS:
        with ExitStack() as st:
            names = dict(diff=("dq0", "dq1", "dk0", "dk1"), gqa=("gq0", "gq1", "gk"),
                         mla=("mqn0", "mqn1", "mqr", "mkn0", "mkn1", "mkr"))[grp]
            fmt = {}
            R_in = Res("attn_in")
            for i, n in enumerate(names):
                fmt[n] = sbt(st, "fm_" + n, [128, T], BF16)
                S.dma(("sp", "act")[i % 2], fmt[n][:], P.fm[FM[n]], writes=[R_in])
            nh, voff, moff = dict(diff=(4, TV_DV, 256), gqa=(2, TV_GV, 512), mla=(4, TV_MV, 768))[grp]
            vraw = sbt(st, "vraw", [128, NT, nh * 64], BF16)
            vaug = sbt(st, "vaug", [128, NT, nh, 65], BF16)
            S.dma("sp", vraw[:], P.tokv[:, voff:voff + nh * 64].rearrange("(i p) c -> p i c", p=128), writes=[R_in])
            S.op("pool", lambda e: e.memset(vaug[:], 1.0), writes=[R_in])
            S.op("pool", lambda e: e.tensor_copy(out=vaug[:, :, :, 0:64], in_=vraw[:].rearrange("p i (h d) -> p i h d", h=nh)),
                 reads=[R_in], writes=[R_in])
            kz = {}
            for n in dict(diff=("dk0", "dk1"), gqa=(), mla=("mkr",))[grp]:
                kz[n] = sbt(st, "kz_" + n, [128, T], BF16)
                S.op("pool", lambda e, n=n: e.tensor_copy(out=kz[n][64:128, :], in_=fmt[n][64:128, :]), reads=[R_in], writes=[R_in])
                S.op("pool", lambda e, n=n: e.memset(kz[n][64:96, :], 0.0), reads=[R_in], writes=[R_in])
            mixo = sbt(st, "mixo", [128, NT, 256], BF16)
            R_mixo = Res("mixo")
            stp = [pst(st, "stp%d" % i, [128, 512], F32) for i in range(2)]
            po = [pst(st, "po%d" % i, [128, 512], F32) for i in range(4)]
            R_stp = [Res(), Res()]; R_po = [Res() for _ in range(4)]
            NE = 4
            eb = [sbt(st, "eb%d" % i, [128, 512], BF16) for i in range(NE)]
            R_eb = [Res() for _ in range(NE)]
            rden = sbt(st, "rden", [128, 4], F32); R_rden = Res()
            cnt = dict(s=0, e=0)
            if grp == "diff":
                d1 = sbt(st, "d1", [128, NT, 64], F32); R_d1 = Res()
                lamr = sbt(st, "lamr", [128, 128], F32); lam2 = sbt(st, "lam2", [128, 2], F32)
                neglam = sbt(st, "neglam", [128, 1], F32); li = sbt(st, "li", [128, 1], F32)
                subb = sbt(st, "subb", [128, 64], F32)
                o2 = sbt(st, "o2", [128, 64], F32); cmbt = sbt(st, "cmbt", [128, 64], F32); jk = sbt(st, "jk", [128, 64], F32)
                ssd = sbt(st, "ssd", [128, 1], F32)
                R_lam = Res(); R_t = Res()
                S.dma("sp", lamr[:], bc(P.rowv[l:l + 1, RV_LAM:RV_LAM + 128], 128), writes=[R_lam])
                S.dma("sp", li[:], bc(P.rowv[l:l + 1, RV_LAMINIT:RV_LAMINIT + 1], 128), writes=[R_lam])
                S.dma("sp", subb[:], bc(P.rowv[l:l + 1, RV_SUBLN:RV_SUBLN + 64], 128), writes=[R_lam])
                S.op("dve", lambda e: e.tensor_tensor(out=lamr[:, 0:32], in0=lamr[:, 0:32], in1=lamr[:, 32:64], op=ALU.mult), reads=[R_lam], writes=[R_lam])
                S.op("dve", lambda e: e.tensor_tensor(out=lamr[:, 64:96], in0=lamr[:, 64:96], in1=lamr[:, 96:128], op=ALU.mult), reads=[R_lam], writes=[R_lam])
                S.op("dve", lambda e: e.tensor_reduce(out=lam2[:, 0:1], in_=lamr[:, 0:32], axis=AX.X, op=ALU.add), reads=[R_lam], writes=[R_lam])
                S.op("dve", lambda e: e.tensor_reduce(out=lam2[:, 1:2], in_=lamr[:, 64:96], axis=AX.X, op=ALU.add), reads=[R_lam], writes=[R_lam])
                S.op("act", lambda e: e.activation(out=lam2[:], in_=lam2[:], func=AF.Exp), reads=[R_lam], writes=[R_lam])
                S.op("dve", lambda e: e.tensor_tensor(out=neglam[:], in0=lam2[:, 1:2], in1=lam2[:, 0:1], op=ALU.subtract), reads=[R_lam], writes=[R_lam])
                S.op("dve", lambda e: e.tensor_tensor(out=neglam[:], in0=neglam[:], in1=li[:], op=ALU.subtract), reads=[R_lam], writes=[R_lam])
                S.op("dve", lambda e: e.tensor_scalar(out=li[:], in0=li[:], scalar1=-1.0, scalar2=1.0, op0=ALU.mult, op1=ALU.add), reads=[R_lam], writes=[R_lam])
                S.op("dve", lambda e: e.tensor_scalar(out=subb[:], in0=subb[:], scalar1=li[:, 0:1], scalar2=None, op0=ALU.mult), reads=[R_lam], writes=[R_lam])

            nmaps = [0]

            def run_map(parts, vh, scale, finish):
                nmaps[0] += 1
                if ATT_MAXMAPS is not None and nmaps[0] > ATT_MAXMAPS:
                    return
                dbg = "dbgAT" in P.debug and nmaps[0] == 1
                if dbg:
                    dd = nc.dram_tensor("dbgAT", [3, 128, 512], F32, kind="ExternalOutput").ap()
                    dv = nc.dram_tensor("dbgV", [128, NT * nh * 65], BF16, kind="ExternalOutput").ap()
                    dt1 = sbt(st, "dt1", [128, 512], F32); dt2 = sbt(st, "dt2", [128, 512], F32); dt3 = sbt(st, "dt3", [128, 512], F32)
                    S.dma("sp", dv, vaug[:].rearrange("p a b c -> p (a b c)"), reads=[R_in], writes=[Res()])
                for (q0, N, nkt) in (QBLOCKS[:2] if ATT_MAXMAPS is not None else QBLOCKS):
                    nqs = N // 128
                    for kt in range(nkt):
                        sb_ = cnt["s"]; cnt["s"] = (sb_ + 1) % 2
                        eb_ = cnt["e"]; cnt["e"] = (eb_ + 1) % NE
                        for pi, (Kt, Qt, r0, nr) in enumerate(parts):
                            S.op("pe", lambda e, Kt=Kt, Qt=Qt, r0=r0, nr=nr, kt=kt, sb_=sb_, pi=pi, q0=q0, N=N: e.matmul(
                                stp[sb_][:, 0:N], lhsT=Kt[r0:r0 + nr, kt * 128:(kt + 1) * 128], rhs=Qt[r0:r0 + nr, q0:q0 + N],
                                start=(pi == 0), stop=(pi == len(parts) - 1)), reads=[R_in], writes=[R_stp[sb_]])
                        S.op("act", lambda e, sb_=sb_, eb_=eb_, N=N: e.activation(out=eb[eb_][:, 0:N], in_=stp[sb_][:, 0:N], func=AF.Exp, scale=scale),
                             reads=[R_stp[sb_]], writes=[R_eb[eb_]])
                        if dbg and q0 == 0 and kt == 0:
                            S.op("dve", lambda e, sb_=sb_: e.tensor_copy(out=dt1[:], in_=stp[sb_][:]), reads=[R_stp[sb_]], writes=[Res()])
                            S.op("dve", lambda e, eb_=eb_: e.tensor_copy(out=dt2[:], in_=eb[eb_][:]), reads=[R_eb[eb_]], writes=[Res()])
                        for qs in range(nqs):
                            S.op("pe", lambda e, qs=qs, eb_=eb_, kt=kt, nkt=nkt: e.matmul(
                                po[qs][:, 0:65], lhsT=eb[eb_][:, qs * 128:(qs + 1) * 128], rhs=vaug[:, kt, vh, :],
                                start=(kt == 0), stop=(kt == nkt - 1)), reads=[R_eb[eb_], R_in], writes=[R_po[qs]])
                    if dbg and q0 == 0:
                        Rd = Res()
                        S.op("dve", lambda e: e.tensor_copy(out=dt3[:], in_=po[0][:]), reads=[R_po[0]], writes=[Rd])
                        S.dma("sp", dd[0], dt1[:], reads=[Rd], writes=[Res()])
                        S.dma("sp", dd[1], dt2[:], reads=[Rd], writes=[Res()])
                        S.dma("sp", dd[2], dt3[:], reads=[Rd], writes=[Res()])
                    for qs in range(nqs):
                        ti = (q0 // 128) + qs
                        S.op("dve", lambda e, qs=qs: e.tensor_copy(out=rden[:, qs:qs + 1], in_=po[qs][:, 64:65]), reads=[R_po[qs]], writes=[R_rden])
                        S.op("dve", lambda e, qs=qs: e.reciprocal(out=rden[:, qs:qs + 1], in_=rden[:, qs:qs + 1]), reads=[R_rden], writes=[R_rden])
                        finish(qs, ti)

            def fin_plain(col):
                def f(qs, ti):
                    S.op("dve", lambda e: e.tensor_scalar(out=mixo[:, ti, col:col + 64], in0=po[qs][:, 0:64], scalar1=rden[:, qs:qs + 1],
                                                          scalar2=None, op0=ALU.mult), reads=[R_po[qs], R_rden], writes=[R_mixo])
                return f

            def fin_d1(qs, ti):
                S.op("dve", lambda e: e.tensor_scalar(out=d1[:, ti, :], in0=po[qs][:, 0:64], scalar1=rden[:, qs:qs + 1], scalar2=None, op0=ALU.mult),
                     reads=[R_po[qs], R_rden], writes=[R_d1])

            def fin_d2(col):
                def f(qs, ti):
                    S.op("dve", lambda e: e.tensor_scalar(out=o2[:], in0=po[qs][:, 0:64], scalar1=rden[:, qs:qs + 1], scalar2=None, op0=ALU.mult),
                         reads=[R_po[qs], R_rden], writes=[R_t])
                    S.op("dve", lambda e: e.scalar_tensor_tensor(out=cmbt[:], in0=o2[:], scalar=neglam[:, 0:1], in1=d1[:, ti, :], op0=ALU.mult, op1=ALU.add),
                         reads=[R_t, R_d1, R_lam], writes=[R_t])
                    S.op("dve", lambda e: e.tensor_tensor(out=jk[:], in0=cmbt[:], in1=cmbt[:], op=ALU.mult), reads=[R_t], writes=[R_t])
                    S.op("dve", lambda e: e.tensor_reduce(out=ssd[:], in_=jk[:], axis=AX.X, op=ALU.add), reads=[R_t], writes=[R_t])
                    S.op("dve", lambda e: e.tensor_scalar(out=ssd[:], in0=ssd[:], scalar1=1.0 / 64, scalar2=EPS, op0=ALU.mult, op1=ALU.add), reads=[R_t], writes=[R_t])
                    S.op("act", lambda e: e.activation(out=ssd[:], in_=ssd[:], func=AF.Sqrt), reads=[R_t], writes=[R_t])
                    S.op("dve", lambda e: e.reciprocal(out=ssd[:], in_=ssd[:]), reads=[R_t], writes=[R_t])
                    S.op("dve", lambda e: e.scalar_tensor_tensor(out=mixo[:, ti, col:col + 64], in0=cmbt[:], scalar=ssd[:, 0:1], in1=subb[:], op0=ALU.mult, op1=ALU.mult),
                         reads=[R_t, R_lam], writes=[R_mixo])
                return f

            if grp == "diff":
                for h in range(4):
                    Kt, Qt, base = fmt["dk%d" % (h // 2)], fmt["dq%d" % (h // 2)], 64 * (h % 2)
                    run_map([(Kt, Qt, base, 32)], h, 32 ** -0.5, fin_d1)
                    if base + 32 == 96:
                        run_map([(kz["dk%d" % (h // 2)], Qt, 64, 64)], h, 32 ** -0.5, fin_d2(h * 64))
                    else:
                        run_map([(Kt, Qt, base + 32, 32)], h, 32 ** -0.5, fin_d2(h * 64))
            elif grp == "gqa":
                for hq in range(4):
                    n, rep = hq // 2, hq % 2
                    run_map([(fmt["gk"], fmt["gq%d" % rep], 64 * n, 64)], n, 64 ** -0.5, fin_plain(hq * 64))
            else:
                for h in range(4):
                    run_map([(fmt["mkn%d" % (h // 2)], fmt["mqn%d" % (h // 2)], 64 * (h % 2), 64),
                             (fmt["mkr"], fmt["mqr"], 32 * h, 32) if h < 3 else (kz["mkr"], fmt["mqr"], 64, 64)], h, 96 ** -0.5, fin_plain(h * 64))
            S.dma("sp", P.mixd[:, moff:moff + 256].rearrange("(i p) c -> p i c", p=128), mixo[:], reads=[R_mixo], writes=[Res()])
            S.drain()


def phase_outproj(c, l):
    P, S, nc, sbt, pst = c["P"], c["S"], c["nc"], c["sbt"], c["pst"]
    cmb, R_cmb, R_xres = c["cmb"], c["R_cmb"], c["R_xres"]
    with ExitStack() as st:
        G, R_G = load_gate_bcast(c, l, st, 2, RV_G1, "a")
        wo = sbt(st, "wo", [128, 8, D], BF16); R_wo = Res()
        src = P.wout[l].rearrange("(c p) n -> p c n", p=128)
        for k in range(8):
            S.dma("pool", wo[:, k, :], src[:, k, :], writes=[R_wo])
        NB = 2
        mx = [sbt(st, "mx%d" % i, [128, D], BF16) for i in range(NB)]
        mT = [sbt(st, "mT%d" % i, [128, 8, 128], BF16) for i in range(NB)]
        xt = [sbt(st, "xt%d" % i, [128, D], F32) for i in range(NB)]
        tt = [sbt(st, "tt%d" % i, [128, D], F32) for i in range(NB)]
        junk = sbt(st, "junk", [128, D], F32)
        ss = [sbt(st, "ss%d" % i, [128, 1], F32) for i in range(NB)]
        rstd = [sbt(st, "rstd%d" % i, [128, 1], F32) for i in range(NB)]
        ptm = [pst(st, "ptm%d" % i, [128, 8, 128], BF16) for i in range(NB)]
        py = [pst(st, "py%d" % i, [128, D], F32) for i in range(NB)]
        R = lambda: [Res() for _ in range(NB)]
        R_mx, R_mT, R_xt, R_tt, R_ss, R_rstd, R_ptm, R_py = R(), R(), R(), R(), R(), R(), R(), R()
        R_junk = Res()

        def tile(i):
            b = i % NB
            r = 1 if i < 2 else 0
            S.dma("sp", mx[b][:], P.mixd[i * 128:(i + 1) * 128, :], writes=[R_mx[b]])
            S.dma("act", xt[b][:], P.xres[i * 128:(i + 1) * 128, :], reads=[R_xres[i]], writes=[R_xt[b]])
            for k in range(8):
                S.op("pe", lambda e, k=k: e.transpose(out=ptm[b][:, k, :], in_=mx[b][:, k * 128:(k + 1) * 128], identity=cmb[:, CM_ID:CM_ID + 128]),
                     reads=[R_mx[b], R_cmb], writes=[R_ptm[b]])
            S.op("act", lambda e: e.activation(out=mT[b][:], in_=ptm[b][:], func=AF.Copy), reads=[R_ptm[b]], writes=[R_mT[b]])
            for half in range(2):
                for k in range(8):
                    S.op("pe", lambda e, k=k, half=half: e.matmul(py[b][:, half * 512:(half + 1) * 512], lhsT=mT[b][:, k, :],
                                                                 rhs=wo[:, k, half * 512:(half + 1) * 512], start=(k == 0), stop=(k == 7)),
                         reads=[R_mT[b], R_wo], writes=[R_py[b]])
            rms_rstd(S, [R_py[b]], py[b][:], D, junk[:], R_junk, ss[b][:], R_ss[b], rstd[b][:], R_rstd[b])
            S.op("dve", lambda e: e.scalar_tensor_tensor(out=tt[b][:], in0=py[b][:], scalar=rstd[b][:, 0:1], in1=G[:, r, :], op0=ALU.mult, op1=ALU.mult),
                 reads=[R_py[b], R_rstd[b], R_G], writes=[R_tt[b]])
            S.op("pool", lambda e: e.tensor_tensor(out=tt[b][:], in0=tt[b][:], in1=xt[b][:], op=ALU.add), reads=[R_tt[b], R_xt[b]], writes=[R_tt[b]])
            S.dma("sp", P.xres[i * 128:(i + 1) * 128, :], tt[b][:], reads=[R_tt[b]], writes=[R_xres[i]])

        for i in range(NT):
            tile(i)
        S.drain()


def phase_experts(c, l):
    P, S, nc, sbt, pst = c["P"], c["S"], c["nc"], c["sbt"], c["pst"]
    cmb, R_cmb = c["cmb"], c["R_cmb"]
    NS = CAP // 128
    with ExitStack() as st:
        w1b = [sbt(st, "w1b%d" % i, [128, 8, 2 * D], BF16) for i in range(2)]
        w2b = [sbt(st, "w2b%d" % i, [128, 8, D], BF16) for i in range(2)]
        b1 = [sbt(st, "b1_%d" % i, [128, 16], F32) for i in range(2)]
        b2b = [sbt(st, "b2b%d" % i, [128, D], F32) for i in range(2)]
        R_w = [Res(), Res()]
        xs = [sbt(st, "xs%d" % i, [128, D], BF16) for i in range(2)]
        xT = sbt(st, "xT", [128, 8, CAP], BF16); aT = sbt(st, "aT", [128, 8, CAP], BF16)
        R_xs = [Res(), Res()]; R_xT = Res(); R_aT = Res()
        NW = 2
        gs = [sbt(st, "gs%d" % i, [128, 512], F32) for i in range(NW)]
        sg = [sbt(st, "sg%d" % i, [128, 512], F32) for i in range(NW)]
        ls = [sbt(st, "ls%d" % i, [128, 512], F32) for i in range(NW)]
        R_gs, R_sg, R_ls = [Res() for _ in range(NW)], [Res() for _ in range(NW)], [Res() for _ in range(NW)]
        ys = [sbt(st, "ys%d" % i, [128, D], F32) for i in range(2)]; R_ys = [Res(), Res()]
        ptx = [pst(st, "ptx%d" % i, [128, 8, 128], BF16) for i in range(2)]
        pg = [pst(st, "pg%d" % i, [128, 512], F32) for i in range(2)]
        pl = [pst(st, "pl%d" % i, [128, 512], F32) for i in range(2)]
        pyy = [pst(st, "pyy%d" % i, [128, 512], F32) for i in range(2)]
        R_ptx, R_pg, R_pl, R_pyy = [Res(), Res()], [Res(), Res()], [Res(), Res()], [Res(), Res()]
        cnt = dict(x=0, w=0, g=0, y=0, ys=0)

        def nxt(key, n):
            v = cnt[key]; cnt[key] = (v + 1) % n
            return v

        def load_w(e):
            b = e % 2
            s1 = P.w1[l, e].rearrange("(c p) n -> p c n", p=128)
            s2 = P.w2[l, e].rearrange("(c p) n -> p c n", p=128)
            for k in range(8):
                S.dma("pool", w1b[b][:, k, :], s1[:, k, :], writes=[R_w[b]])
            for k in range(0, 8, 2):
                S.dma("pool", w2b[b][:, k:k + 2, :], s2[:, k:k + 2, :], writes=[R_w[b]])
            S.dma("sp", b1[b][:], P.b1c[l, e], writes=[R_w[b]])
            S.dma("sp", b2b[b][:], bc(P.b2[l, e:e + 1, :], 128), writes=[R_w[b]])

        def expert(e):
            b = e % 2
            for s in range(NS):
                x = nxt("x", 2)
                S.dma("sp" if s % 2 else "act", xs[x][:], P.xg[e * CAP + s * 128:e * CAP + (s + 1) * 128, :], writes=[R_xs[x]])
                for k in range(8):
                    S.op("pe", lambda e_, k=k, x=x: e_.transpose(out=ptx[x][:, k, :], in_=xs[x][:, k * 128:(k + 1) * 128], identity=cmb[:, CM_ID:CM_ID + 128]),
                         reads=[R_xs[x], R_cmb], writes=[R_ptx[x]])
                S.op("act" if s % 2 else "dve", (lambda e_, x=x, s=s: e_.activation(out=xT[:, :, s * 128:(s + 1) * 128], in_=ptx[x][:], func=AF.Copy)) if s % 2 else
                     (lambda e_, x=x, s=s: e_.tensor_copy(out=xT[:, :, s * 128:(s + 1) * 128], in_=ptx[x][:])), reads=[R_ptx[x]], writes=[R_xT])
            for fc in range(8):
                for (c0, W) in ((0, 512), (512, 512), (1024, CAP - 1024)):
                    g = nxt("g", 2); w = nxt("w", NW)
                    for k in range(8):
                        S.op("pe", lambda e_, k=k, g=g, fc=fc, c0=c0, W=W: e_.matmul(pg[g][:, 0:W], lhsT=w1b[b][:, k, fc * 256:(fc + 1) * 256:2],
                                                                                     rhs=xT[:, k, c0:c0 + W], start=(k == 0), stop=(k == 7)),
                             reads=[R_w[b], R_xT], writes=[R_pg[g]])
                    for k in range(8):
                        S.op("pe", lambda e_, k=k, g=g, fc=fc, c0=c0, W=W: e_.matmul(pl[g][:, 0:W], lhsT=w1b[b][:, k, fc * 256 + 1:(fc + 1) * 256:2],
                                                                                     rhs=xT[:, k, c0:c0 + W], start=(k == 0), stop=(k == 7)),
                             reads=[R_w[b], R_xT], writes=[R_pl[g]])
                    S.op("dve", lambda e_, g=g, w=w, fc=fc, W=W: e_.tensor_scalar(out=gs[w][:, 0:W], in0=pg[g][:, 0:W], scalar1=b1[b][:, 2 * fc:2 * fc + 1], scalar2=7.0,
                                                                               op0=ALU.add, op1=ALU.min), reads=[R_pg[g], R_w[b]], writes=[R_gs[w]])
                    S.op("act", lambda e_, w=w, W=W: e_.activation(out=sg[w][:, 0:W], in_=gs[w][:, 0:W], func=AF.Sigmoid, scale=1.702), reads=[R_gs[w]], writes=[R_sg[w]])
                    S.op("dve", lambda e_, g=g, w=w, fc=fc, W=W: e_.tensor_scalar(out=ls[w][:, 0:W], in0=pl[g][:, 0:W], scalar1=b1[b][:, 2 * fc + 1:2 * fc + 2], scalar2=7.0,
                                                                               op0=ALU.add, op1=ALU.min), reads=[R_pl[g], R_w[b]], writes=[R_ls[w]])
                    S.op("pool", lambda e_, w=w, W=W: e_.tensor_scalar(out=ls[w][:, 0:W], in0=ls[w][:, 0:W], scalar1=-7.0, scalar2=1.0, op0=ALU.max, op1=ALU.add),
                         reads=[R_ls[w]], writes=[R_ls[w]])
                    S.op("pool", lambda e_, w=w, W=W: e_.tensor_tensor(out=gs[w][:, 0:W], in0=gs[w][:, 0:W], in1=sg[w][:, 0:W], op=ALU.mult),
                         reads=[R_gs[w], R_sg[w]], writes=[R_gs[w]])
                    S.op("pool", lambda e_, w=w, W=W, fc=fc, c0=c0: e_.tensor_tensor(out=aT[:, fc, c0:c0 + W], in0=gs[w][:, 0:W], in1=ls[w][:, 0:W], op=ALU.mult),
                         reads=[R_gs[w], R_ls[w]], writes=[R_aT])
            for s in range(NS):
                yb = nxt("ys", 2)
                for half in range(2):
                    y = nxt("y", 2)
                    for fc in range(8):
                        S.op("pe", lambda e_, fc=fc, y=y, s=s, half=half: e_.matmul(pyy[y][:], lhsT=aT[:, fc, s * 128:(s + 1) * 128],
                                                                                   rhs=w2b[b][:, fc, half * 512:(half + 1) * 512], start=(fc == 0), stop=(fc == 7)),
                             reads=[R_aT, R_w[b]], writes=[R_pyy[y]])
                    S.op("dve", lambda e_, y=y, yb=yb, half=half: e_.tensor_tensor(out=ys[yb][:, half * 512:(half + 1) * 512], in0=pyy[y][:],
                                                                                  in1=b2b[b][:, half * 512:(half + 1) * 512], op=ALU.add),
                         reads=[R_pyy[y], R_w[b]], writes=[R_ys[yb]])
                S.dma("sp", P.yg[e * CAP + s * 128:e * CAP + (s + 1) * 128, :], ys[yb][:], reads=[R_ys[yb]], writes=[Res()])

        load_w(0)
        for e in range(NEXP):
            if e + 1 < NEXP:
                load_w(e + 1)
            expert(e)
        S.drain()


def phase_combine(c, l, rt):
    P, S, nc, sbt, pst = c["P"], c["S"], c["nc"], c["sbt"], c["pst"]
    R_xres = c["R_xres"]
    with ExitStack() as st:
        G, R_G = load_gate_bcast(c, l, st, 5, RV_G3, "f")
        NB = 2
        yk = [[sbt(st, "yk%d_%d" % (i, k), [128, D], F32) for k in range(4)] for i in range(NB)]
        R_yk = [[Res() for k in range(4)] for i in range(NB)]
        acc = [sbt(st, "acc%d" % i, [128, D], F32) for i in range(NB)]
        xt = [sbt(st, "xt%d" % i, [128, D], F32) for i in range(NB)]
        junk = sbt(st, "junk", [128, D], F32); R_junk = Res()
        ss = [sbt(st, "ss%d" % i, [128, 1], F32) for i in range(NB)]
        rstd = [sbt(st, "rstd%d" % i, [128, 1], F32) for i in range(NB)]
        R = lambda: [Res() for _ in range(NB)]
        R_acc, R_xt, R_ss, R_rstd = R(), R(), R(), R()
        gates, slots = rt["gates"], rt["slots"]

        def tile(i):
            b = i % NB
            r = 1 if i < 2 else 0
            S.dma("sp", xt[b][:], P.xres[i * 128:(i + 1) * 128, :], reads=[R_xres[i]], writes=[R_xt[b]])
            for k in range(4):
                S.dma("pool", yk[b][k][:], P.yg[:, :], reads=[rt["R_slots"]], writes=[R_yk[b][k]],
                      indirect=dict(out_offset=None, in_offset=bass.IndirectOffsetOnAxis(ap=slots[:, i, k:k + 1], axis=0)))
            S.op("dve", lambda e: e.tensor_scalar(out=acc[b][:], in0=yk[b][0][:], scalar1=gates[:, i, 0:1], scalar2=None, op0=ALU.mult),
                 reads=[R_yk[b][0], rt["R_gates"]], writes=[R_acc[b]])
            for k in range(1, 4):
                S.op("dve", lambda e, k=k: e.scalar_tensor_tensor(out=acc[b][:], in0=yk[b][k][:], scalar=gates[:, i, k:k + 1], in1=acc[b][:],
                                                                                      op0=ALU.mult, op1=ALU.add),
                     reads=[R_yk[b][k], rt["R_gates"], R_acc[b]], writes=[R_acc[b]])
            rms_rstd(S, [R_acc[b]], acc[b][:], D, junk[:], R_junk, ss[b][:], R_ss[b], rstd[b][:], R_rstd[b])
            S.op("dve", lambda e: e.scalar_tensor_tensor(out=acc[b][:], in0=acc[b][:], scalar=rstd[b][:, 0:1], in1=G[:, r, :], op0=ALU.mult, op1=ALU.mult),
                 reads=[R_acc[b], R_rstd[b], R_G], writes=[R_acc[b]])
            S.op("pool", lambda e: e.tensor_tensor(out=acc[b][:], in0=acc[b][:], in1=xt[b][:], op=ALU.add), reads=[R_acc[b], R_xt[b]], writes=[R_acc[b]])
            S.dma("sp", P.xres[i * 128:(i + 1) * 128, :], acc[b][:], reads=[R_acc[b]], writes=[R_xres[i]])

        for i in range(NT):
            tile(i)
        S.drain()


def phase_retention(c, l):
    P, S, nc, sbt, pst = c["P"], c["S"], c["nc"], c["sbt"], c["pst"]
    cm, R_cm, cmb, R_cmb = c["cm"], c["R_cm"], c["cmb"], c["R_cmb"]
    with ExitStack() as st:
        QT = [sbt(st, "rQT%d" % p, [128, T], BF16) for p in range(2)]
        KT = [sbt(st, "rKT%d" % p, [128, T], BF16) for p in range(2)]
        QfT = [sbt(st, "rQfT%d" % p, [128, T], BF16) for p in range(2)]
        QbT = [sbt(st, "rQbT%d" % p, [128, T], BF16) for p in range(2)]
        V = sbt(st, "rV", [128, NT, 256], BF16); Gt = sbt(st, "rG", [128, NT, 256], BF16)
        Kzf = sbt(st, "Kzf", [128, NT, 256], BF16); Kzb = sbt(st, "Kzb", [128, NT, 256], BF16)
        SfAll = sbt(st, "SfAll", [128, NT, 2, 64], BF16); SbAll = sbt(st, "SbAll", [128, NT, 2, 64], BF16)
        mixo = sbt(st, "rmixo", [128, NT, 256], BF16)
        R_in, R_q, R_kz, R_sall, R_mixo = Res(), Res(), Res(), Res(), Res()
        for p in range(2):
            S.dma("sp", QT[p][:], P.fm[FM["rq%d" % p]], writes=[R_in])
            S.dma("act", KT[p][:], P.fm[FM["rk%d" % p]], writes=[R_in])
        S.dma("sp", V[:], P.tokv[:, TV_RV:TV_RV + 256].rearrange("(i p) c -> p i c", p=128), writes=[R_in])
        S.dma("act", Gt[:], P.tokv[:, TV_RG:TV_RG + 256].rearrange("(i p) c -> p i c", p=128), writes=[R_in])
        lg = sbt(st, "lg", [128, 8], F32); lgc = sbt(st, "lgc", [128, 4], F32); gC = sbt(st, "gC", [128, 4], F32)
        DT = sbt(st, "DT", [128, 4, 128], F32); e1 = sbt(st, "e1", [128, 128], F32); e2 = sbt(st, "e2", [128, 128], F32)
        Xi = sbt(st, "Xi", [128, 4, 128], BF16)
        Zt = sbt(st, "Zt", [128, 8], F32)
        gnw = sbt(st, "gnw", [128, 256], F32); gnb = sbt(st, "gnb", [128, 256], F32)
        R_t = Res()
        S.dma("sp", lg[:], bc(P.rowv[l:l + 1, RV_DECAY:RV_DECAY + 8], 128), writes=[R_t])
        S.dma("sp", gnw[:], bc(P.rowv[l:l + 1, RV_GNW:RV_GNW + 256], 128), writes=[R_t])
        S.dma("sp", gnb[:], bc(P.rowv[l:l + 1, RV_GNB:RV_GNB + 256], 128), writes=[R_t])
        S.op("act", lambda e: e.activation(out=lg[:], in_=lg[:], func=AF.Exp), reads=[R_t], writes=[R_t])
        S.op("dve", lambda e: e.tensor_scalar(out=lg[:], in0=lg[:], scalar1=-1.0, scalar2=None, op0=ALU.mult), reads=[R_t], writes=[R_t])
        for h in range(4):
            S.op("act", lambda e, h=h: e.activation(out=e1[:], in_=cm[:, CM_DPOS:CM_DPOS + 128], func=AF.Exp, scale=lg[:, h:h + 1]), reads=[R_t, R_cm], writes=[R_t])
            S.op("dve", lambda e: e.tensor_tensor(out=e1[:], in0=e1[:], in1=cm[:, CM_MF:CM_MF + 128], op=ALU.mult), reads=[R_t, R_cm], writes=[R_t])
            S.op("act", lambda e, h=h: e.activation(out=e2[:], in_=cm[:, CM_DNEG:CM_DNEG + 128], func=AF.Exp, scale=lg[:, 4 + h:5 + h]), reads=[R_t, R_cm], writes=[R_t])
            S.op("dve", lambda e: e.tensor_tensor(out=e2[:], in0=e2[:], in1=cm[:, CM_MB:CM_MB + 128], op=ALU.mult), reads=[R_t, R_cm], writes=[R_t])
            S.op("dve", lambda e, h=h: e.tensor_tensor(out=DT[:, h, :], in0=e1[:], in1=e2[:], op=ALU.add), reads=[R_t], writes=[R_t])
        for d in range(2):
            for p in range(2):
                S.op("dve", lambda e, d=d, p=p: e.tensor_copy(out=lgc[0:64, 2 * d + p:2 * d + p + 1], in_=lg[0:64, 4 * d + 2 * p:4 * d + 2 * p + 1]), reads=[R_t], writes=[R_t])
                S.op("dve", lambda e, d=d, p=p: e.tensor_copy(out=lgc[64:128, 2 * d + p:2 * d + p + 1], in_=lg[64:128, 4 * d + 2 * p + 1:4 * d + 2 * p + 2]), reads=[R_t], writes=[R_t])
        for p in range(2):
            S.op("act", lambda e, p=p: e.activation(out=Xi[:, p, :], in_=cm[:, CM_NP1:CM_NP1 + 128], func=AF.Exp, scale=lgc[:, p:p + 1]), reads=[R_t, R_cm], writes=[R_t])
            S.op("act", lambda e, p=p: e.activation(out=Xi[:, 2 + p, :], in_=cm[:, CM_NREV:CM_NREV + 128], func=AF.Exp, scale=lgc[:, 2 + p:3 + p]), reads=[R_t, R_cm], writes=[R_t])
        S.op("act", lambda e: e.activation(out=Zt[:, 0:4], in_=lg[:, 0:4], func=AF.Exp, scale=cm[:, CM_PREV:CM_PREV + 1]), reads=[R_t, R_cm], writes=[R_t])
        S.op("act", lambda e: e.activation(out=Zt[:, 4:8], in_=lg[:, 4:8], func=AF.Exp, scale=cm[:, CM_PCOL:CM_PCOL + 1]), reads=[R_t, R_cm], writes=[R_t])
        S.op("act", lambda e: e.activation(out=gC[:], in_=lgc[:], func=AF.Exp, scale=128.0), reads=[R_t], writes=[R_t])
        if RET_STOP == 1:
            S.drain()
            return
        for p in range(2):
            S.op("pool", lambda e, p=p: e.tensor_tensor(out=QfT[p][:].rearrange("f (c n) -> f c n", n=128), in0=QT[p][:].rearrange("f (c n) -> f c n", n=128),
                                                      in1=Xi[:, p:p + 1, :].to_broadcast([128, NT, 128]), op=ALU.mult), reads=[R_in, R_t], writes=[R_q])
            S.op("pool", lambda e, p=p: e.tensor_tensor(out=QbT[p][:].rearrange("f (c n) -> f c n", n=128), in0=QT[p][:].rearrange("f (c n) -> f c n", n=128),
                                                      in1=Xi[:, 2 + p:3 + p, :].to_broadcast([128, NT, 128]), op=ALU.mult), reads=[R_in, R_t], writes=[R_q])
        if RET_STOP == 2:
            S.drain()
            return
        ptk = [pst(st, "ptk%d" % i, [128, 1024], BF16) for i in range(2)]; R_ptk = [Res(), Res()]
        for cidx in range(NT):
            b = cidx % 2
            for p in range(2):
                S.op("pe", lambda e, p=p, b=b, cidx=cidx: e.transpose(out=ptk[b][:, p * 128:(p + 1) * 128], in_=KT[p][:, cidx * 128:(cidx + 1) * 128], identity=cmb[:, CM_ID:CM_ID + 128]),
                     reads=[R_in, R_cmb], writes=[R_ptk[b]])
            S.op("dve", lambda e, b=b, cidx=cidx: e.tensor_tensor(out=Kzf[:, cidx, :].rearrange("p (h d) -> p h d", h=4), in0=ptk[b][:, 0:256].rearrange("p (h d) -> p h d", h=4),
                                                                in1=Zt[:, 0:4, None].to_broadcast([128, 4, 64]), op=ALU.mult), reads=[R_ptk[b], R_t], writes=[R_kz])
            S.op("dve", lambda e, b=b, cidx=cidx: e.tensor_tensor(out=Kzb[:, cidx, :].rearrange("p (h d) -> p h d", h=4), in0=ptk[b][:, 0:256].rearrange("p (h d) -> p h d", h=4),
                                                                in1=Zt[:, 4:8, None].to_broadcast([128, 4, 64]), op=ALU.mult), reads=[R_ptk[b], R_t], writes=[R_kz])
        if RET_STOP == 3:
            S.drain()
            return
        pu = [pst(st, "pu%d" % i, [128, 512], F32) for i in range(2)]; R_pu = [Res(), Res()]
        Sst = sbt(st, "Sst", [128, 2, 64], F32); R_S = Res()
        ucnt = [0]
        for d, Kz, SAll, order in ((0, Kzf, SfAll, list(range(NT))), (1, Kzb, SbAll, [1, 0] + list(range(NT - 1, 1, -1)))):
            S.op("pool", lambda e: e.memset(Sst[:], 0.0), reads=[R_S], writes=[R_S])
            for cidx in order:
                S.op("act", lambda e, SAll=SAll, cidx=cidx: e.activation(out=SAll[:, cidx, :, :], in_=Sst[:], func=AF.Copy), reads=[R_S], writes=[R_sall])
                for p in range(2):
                    u = ucnt[0]; ucnt[0] = (u + 1) % 2
                    S.op("pe", lambda e, u=u, Kz=Kz, cidx=cidx, p=p: e.matmul(pu[u][:, 0:128], lhsT=Kz[:, cidx, p * 128:(p + 1) * 128], rhs=V[:, cidx, p * 128:(p + 1) * 128], start=True, stop=True),
                         reads=[R_kz, R_in], writes=[R_pu[u]])
                    for half in range(2):
                        rows = slice(64 * half, 64 * half + 64)
                        S.op("dve", lambda e, u=u, p=p, d=d, rows=rows: e.scalar_tensor_tensor(out=Sst[rows, p, :], in0=Sst[rows, p, :], scalar=gC[rows, 2 * d + p:2 * d + p + 1],
                                                                                            in1=pu[u][rows, rows], op0=ALU.mult, op1=ALU.add),
                             reads=[R_S, R_pu[u], R_t], writes=[R_S])
        if RET_STOP == 4:
            S.drain()
            return
        paE = pst(st, "rpaE", [128, 512], F32); paO = pst(st, "rpaO", [128, 512], F32)
        poE = pst(st, "rpoE", [128, 512], F32); poO = pst(st, "rpoO", [128, 512], F32)
        pa_ = (paE, paO); po_ = (poE, poO)
        R_pa = [Res(), Res()]; R_po = [Res(), Res()]
        Wt = [sbt(st, "Wt%d" % i, [128, 4, 128], BF16) for i in range(2)]; R_W = [Res(), Res()]
        ob = [sbt(st, "rob%d" % i, [128, 4, 64], F32) for i in range(2)]; xc = [sbt(st, "rxc%d" % i, [128, 4, 64], F32) for i in range(2)]
        sqv = [sbt(st, "rsq%d" % i, [128, 4, 64], F32) for i in range(2)]
        mu = [sbt(st, "rmu%d" % i, [128, 4], F32) for i in range(2)]; var = [sbt(st, "rvar%d" % i, [128, 4], F32) for i in range(2)]
        R_o = [Res(), Res()]

        def chunk(cidx):
            b = cidx % 2
            cs = slice(cidx * 128, (cidx + 1) * 128)
            for h in (0, 2, 1, 3):
                p, par, rows = h // 2, h % 2, slice(64 * (h % 2), 64 * (h % 2) + 64)
                S.op("pe", lambda e, h=h, p=p, par=par, rows=rows: e.matmul(pa_[par][:, p * 128:(p + 1) * 128], lhsT=KT[p][rows, cs], rhs=QT[p][rows, cs], start=True, stop=True),
                     reads=[R_in], writes=[R_pa[par]])
            for h in range(4):
                p, par = h // 2, h % 2
                S.op("dve", lambda e, h=h, p=p, par=par: e.tensor_tensor(out=Wt[b][:, h, :], in0=pa_[par][:, p * 128:(p + 1) * 128], in1=DT[:, h, :], op=ALU.mult),
                     reads=[R_pa[par], R_t], writes=[R_W[b]])
            for h in (0, 2, 1, 3):
                p, par, rows = h // 2, h % 2, slice(64 * (h % 2), 64 * (h % 2) + 64)
                dst = po_[par][:, p * 64:(p + 1) * 64]
                S.op("pe", lambda e, h=h, dst=dst: e.matmul(dst, lhsT=Wt[b][:, h, :], rhs=V[:, cidx, h * 64:(h + 1) * 64], start=True, stop=False),
                     reads=[R_W[b], R_in], writes=[R_po[par]])
                S.op("pe", lambda e, h=h, p=p, rows=rows, dst=dst: e.matmul(dst, lhsT=QfT[p][rows, cs], rhs=SfAll[rows, cidx, p, :], start=False, stop=False),
                     reads=[R_q, R_sall], writes=[R_po[par]])
                S.op("pe", lambda e, h=h, p=p, rows=rows, dst=dst: e.matmul(dst, lhsT=QbT[p][rows, cs], rhs=SbAll[rows, cidx, p, :], start=False, stop=True),
                     reads=[R_q, R_sall], writes=[R_po[par]])
            Ro = R_o[b]
            for h in range(4):
                p, par = h // 2, h % 2
                S.op("act", lambda e, h=h, p=p, par=par: e.activation(out=ob[b][:, h, :], in_=po_[par][:, p * 64:(p + 1) * 64], func=AF.Copy, scale=0.125),
                     reads=[R_po[par]], writes=[Ro])
            S.op("dve", lambda e: e.tensor_reduce(out=mu[b][:], in_=ob[b][:], axis=AX.X, op=ALU.add), reads=[Ro], writes=[Ro])
            S.op("dve", lambda e: e.tensor_scalar(out=mu[b][:], in0=mu[b][:], scalar1=1.0 / 64, scalar2=None, op0=ALU.mult), reads=[Ro], writes=[Ro])
            S.op("pool", lambda e: e.tensor_tensor(out=xc[b][:], in0=ob[b][:], in1=mu[b][:, :, None].to_broadcast([128, 4, 64]), op=ALU.subtract), reads=[Ro], writes=[Ro])
            S.op("pool", lambda e: e.tensor_tensor(out=sqv[b][:], in0=xc[b][:], in1=xc[b][:], op=ALU.mult), reads=[Ro], writes=[Ro])
            S.op("dve", lambda e: e.tensor_reduce(out=var[b][:], in_=sqv[b][:], axis=AX.X, op=ALU.add), reads=[Ro], writes=[Ro])
            S.op("dve", lambda e: e.tensor_scalar(out=var[b][:], in0=var[b][:], scalar1=1.0 / 64, scalar2=EPS, op0=ALU.mult, op1=ALU.add), reads=[Ro], writes=[Ro])
            S.op("act", lambda e: e.activation(out=var[b][:], in_=var[b][:], func=AF.Sqrt), reads=[Ro], writes=[Ro])
            S.op("dve", lambda e: e.reciprocal(out=var[b][:], in_=var[b][:]), reads=[Ro], writes=[Ro])
            S.op("pool", lambda e: e.tensor_tensor(out=xc[b][:], in0=xc[b][:], in1=var[b][:, :, None].to_broadcast([128, 4, 64]), op=ALU.mult), reads=[Ro], writes=[Ro])
            xcf = xc[b][:].rearrange("p h d -> p (h d)")
            S.op("pool", lambda e: e.tensor_tensor(out=xcf, in0=xcf, in1=gnw[:], op=ALU.mult), reads=[Ro, R_t], writes=[Ro])
            S.op("pool", lambda e: e.tensor_tensor(out=xcf, in0=xcf, in1=gnb[:], op=ALU.add), reads=[Ro, R_t], writes=[Ro])
            S.op("pool", lambda e: e.tensor_tensor(out=mixo[:, cidx, :], in0=xcf, in1=Gt[:, cidx, :], op=ALU.mult), reads=[Ro, R_in], writes=[R_mixo])

        for cidx in range(NT):
            chunk(cidx)
        S.dma("sp", P.mixd[:, 0:256].rearrange("(i p) c -> p i c", p=128), mixo[:], reads=[R_mixo], writes=[Res()])
        S.drain()


_PROG_CACHE = {}


def kernel(**inputs):
    n = 8
    if DEPTH not in _PROG_CACHE:
        _PROG_CACHE[DEPTH] = build_program(DEPTH)
    P = _PROG_CACHE[DEPTH]
    cmn, tabs = build_consts()
    x = np.asarray(inputs["x"], np.float32)
    ctxv = np.asarray(inputs["ctx"], np.float32)
    la = prep_layer_arrays(inputs, list(range(DEPTH)))
    in_maps = []
    for b in range(n):
        xin = np.ascontiguousarray(np.concatenate([ctxv[b], x[b]], 0))
        cvec = np.ascontiguousarray(np.concatenate([_col(inputs["c"][b]), _col(inputs["c_ctx"])], 1))
        in_maps.append(dict(xin=xin, cvec=cvec, cmat=cmn, ropet=tabs, **la))
    res = run_bass_kernel_spmd(P.nc, in_maps, core_ids=list(range(n)))
    return np.stack([np.asarray(r["yout"], np.float32)[NCTX:] for r in res.results], 0)
```

```python
import os
import math
import numpy as np
from contextlib import ExitStack
import concourse.bass as bass
import concourse.mybir as mybir
from concourse.bass_utils import run_bass_kernel_spmd

F32 = mybir.dt.float32
BF16 = mybir.dt.bfloat16
I32 = mybir.dt.int32
U32 = mybir.dt.uint32
AF = mybir.ActivationFunctionType
ALU = mybir.AluOpType
AX = mybir.AxisListType

D = 1024
NCTX = 256
NLAT = 4096
T = NCTX + NLAT
NT = T // 128
DEPTH = 4
GRID_W = 64
EPS = 1e-6
NEXP = 32
CAP = 1152
NSLOT = NEXP * CAP
TB = [(i * 512, 512) for i in range(8)] + [(4096, 256)]

ENGS = ("pe", "act", "dve", "pool", "sp")
N_DMA_SEMS = 8
SAME_ENGINE_SYNC = True


class Res:
    __slots__ = ("name", "w", "r")

    def __init__(self, name=""):
        self.name = name
        self.w = None
        self.r = []


class Sched:
    def __init__(self, nc, stack):
        self.nc = nc
        self.sems = {}
        self.cnt = {}
        for e in ENGS:
            self.sems[e] = stack.enter_context(nc.semaphore("s_" + e))
            self.cnt[e] = 0
        self.dma_next = {}
        for q in ("sp", "pool", "act"):
            for i in range(N_DMA_SEMS):
                k = ("dma", q, i)
                self.sems[k] = stack.enter_context(nc.semaphore("d_%s_%d" % (q, i)))
                self.cnt[k] = 0
            self.dma_next[q] = 0
        self.known = {e: {} for e in ENGS}
        self.lists = {e: [] for e in ENGS}
        self.n_ops = 0

    def _need(self, eng, deps):
        out = {}
        for d in deps:
            if d is None:
                continue
            k, v = d
            if k == eng and not (SAME_ENGINE_SYNC and eng != "pe"):
                continue
            if self.known[eng].get(k, 0) >= v:
                continue
            if out.get(k, 0) < v:
                out[k] = v
        for k, v in out.items():
            self.known[eng][k] = v
        return list(out.items())

    @staticmethod
    def _deps(reads, writes):
        deps = []
        for r in reads:
            deps.append(r.w)
        for w in writes:
            deps.append(w.w)
            deps.extend(w.r)
        return deps

    @staticmethod
    def _mark(tag, reads, writes):
        for r in reads:
            r.r.append(tag)
            if len(r.r) > 64:
                best = {}
                for k, v in r.r:
                    if best.get(k, 0) < v:
                        best[k] = v
                r.r = list(best.items())
        for w in writes:
            w.w = tag
            w.r = []

    def op(self, eng, fn, reads=(), writes=()):
        waits = self._need(eng, self._deps(reads, writes))
        self.cnt[eng] += 1
        v = self.cnt[eng]
        sem = self.sems[eng]
        sems = self.sems

        def emit(e, fn=fn, waits=waits, sem=sem):
            for k, val in waits:
                e.wait_ge(sems[k], val)
            fn(e).then_inc(sem, 1)
        self.lists[eng].append(emit)
        self._mark((eng, v), reads, writes)
        self.n_ops += 1

    def dma(self, q, out, in_, reads=(), writes=(), indirect=None, **kw):
        i = self.dma_next[q]
        self.dma_next[q] = (i + 1) % N_DMA_SEMS
        k = ("dma", q, i)
        deps = self._deps(reads, writes)
        if self.cnt[k] > 0:
            deps.append((k, self.cnt[k]))
        waits = self._need(q, deps)
        self.cnt[k] += 16
        v = self.cnt[k]
        sem = self.sems[k]
        sems = self.sems

        def emit(e, waits=waits, sem=sem, out=out, in_=in_, kw=kw, indirect=indirect):
            for kk, val in waits:
                e.wait_ge(sems[kk], val)
            if indirect is None:
                e.dma_start(out=out, in_=in_, **kw).then_inc(sem, 16)
            else:
                e.indirect_dma_start(out=out, in_=in_, **indirect).then_inc(sem, 16)
        self.lists[q].append(emit)
        self._mark((k, v), reads, writes)
        self.n_ops += 1

    def wait_all(self, eng, resources):
        deps = []
        for r in resources:
            deps.append(r.w)
            deps.extend(r.r)
        waits = self._need(eng, deps)
        sems = self.sems

        def emit(e, waits=waits):
            for kk, val in waits:
                e.wait_ge(sems[kk], val)
        self.lists[eng].append(emit)

    def drain(self):
        deps = [(k, v) for k, v in self.cnt.items() if isinstance(k, tuple) and v > 0]
        waits = self._need("sp", deps)
        sems = self.sems

        def emit(e, waits=waits):
            for kk, val in waits:
                e.wait_ge(sems[kk], val)
        self.lists["sp"].append(emit)
        self.flush()
        for e in ENGS:
            for k, v in self.cnt.items():
                self.known[e][k] = v

    def flush(self):
        nc = self.nc
        lists = self.lists
        self.lists = {e: [] for e in ENGS}
        if not any(lists.values()):
            return
        with nc.Block() as block:
            @block.tensor
            def _(e):
                for f in lists["pe"]:
                    f(e)

            @block.scalar
            def _(e):
                for f in lists["act"]:
                    f(e)

            @block.vector
            def _(e):
                for f in lists["dve"]:
                    f(e)

            @block.gpsimd
            def _(e):
                for f in lists["pool"]:
                    f(e)

            @block.sync
            def _(e):
                for f in lists["sp"]:
                    f(e)


def _col(v):
    v = np.asarray(v, np.float32)
    return np.ascontiguousarray(v.reshape(-1, 128).T)


def _swap_blocks(cols, blk):
    cols = np.asarray(cols)
    out = cols.reshape(-1, 2, blk // 2)[:, ::-1, :].reshape(-1)
    return out


def build_wext_index():
    r = np.arange
    units = []
    units.append(r(0, 128)); units.append(r(128, 256))
    units.append(r(256, 384)); units.append(r(384, 512))
    units.append(r(1024, 1152)); units.append(r(1152, 1280))
    units.append(r(1280, 1408)); units.append(r(1408, 1536))
    gq = 1792
    units.append(np.concatenate([r(gq, gq + 64), r(gq + 128, gq + 192)]))
    units.append(np.concatenate([r(gq + 64, gq + 128), r(gq + 192, gq + 256)]))
    units.append(r(2048, 2176))
    units.append(np.tile(r(2624, 2656), 4))
    blks = [64, 64, 64, 64, 32, 32, 32, 32, 64, 64, 64, 32]
    sw = [_swap_blocks(u, b) for u, b in zip(units, blks)]
    feat = units + sw
    feat.append(r(2304, 2432))
    feat.append(r(2432, 2496))
    feat.append(r(2496, 2624))
    cols = []
    for u in feat:
        if len(u) < 128:
            u = np.concatenate([u, np.zeros(128 - len(u), np.int64)])
        cols.append(u)
    tokc = np.concatenate([r(512, 1024), r(1536, 1792), r(2176, 2304)])
    return np.concatenate(cols + [tokc]).astype(np.int64)


WEXT_IDX = build_wext_index()
NFEAT_UNITS = 27
NWEXT = WEXT_IDX.shape[0]
TOK_OFF = NFEAT_UNITS * 128
U_RQ, U_RK, U_DQ, U_DK, U_GQ, U_GK, U_MKR = 0, 2, 4, 6, 8, 10, 11
U_SW = 12
U_CQ0, U_CQ1, U_CKV = 24, 25, 26

_r = np.arange
WUQ_IDX = np.concatenate([
    _r(0, 64), _r(96, 160), _r(192, 256), _r(288, 352),
    _r(64, 96), _r(160, 192), _r(256, 288), _r(352, 384),
    _swap_blocks(np.concatenate([_r(64, 96), _r(160, 192), _r(256, 288), _r(352, 384)]), 32)])
WUKV_IDX = np.concatenate([
    _r(0, 64), _r(128, 192), _r(256, 320), _r(384, 448),
    _r(64, 128), _r(192, 256), _r(320, 384), _r(448, 512)])

RV_G1, RV_G3, RV_GNW, RV_GNB, RV_SUBLN, RV_ADAB, RV_RB, RV_DECAY, RV_LAM, RV_LAMINIT = (
    0, 1024, 2048, 2304, 2560, 2624, 8768, 8800, 8808, 8936)
RV_G0, RV_G2 = 8944, 9968
NRV = 10992
CV_GQG, CV_GQGS, CV_GKG, CV_GKGS, CV_QN0, CV_QN1, CV_KVG, CV_G0, CV_G2 = 0, 1, 2, 3, 4, 5, 6, 8, 16
NCV = 24

CM_ID, CM_DPOS, CM_DNEG, CM_MF, CM_MB, CM_LTRI, CM_ONES, CM_NP1, CM_NREV, CM_BLK64, CM_IOTA, CM_PCOL, CM_PREV, CM_SEL = (
    0, 128, 256, 384, 512, 640, 768, 896, 1024, 1152, 1280, 1312, 1313, 1314)
NCM = 1314 + 256


def build_consts():
    cm = np.zeros((128, NCM), np.float32)
    m = np.arange(128)[:, None].astype(np.float32)
    n = np.arange(128)[None, :].astype(np.float32)
    cm[:, CM_ID:CM_ID + 128] = np.eye(128)
    cm[:, CM_DPOS:CM_DPOS + 128] = np.maximum(n - m, 0)
    cm[:, CM_DNEG:CM_DNEG + 128] = np.maximum(m - n, 0)
    cm[:, CM_MF:CM_MF + 128] = (n >= m)
    cm[:, CM_MB:CM_MB + 128] = (m >= n)
    cm[:, CM_LTRI:CM_LTRI + 128] = (m < n)
    cm[:, CM_ONES:CM_ONES + 128] = 1.0
    cm[:, CM_NP1:CM_NP1 + 128] = n + 1.0
    cm[:, CM_NREV:CM_NREV + 128] = 128.0 - n
    cm[:, CM_BLK64:CM_BLK64 + 128] = ((np.arange(128)[:, None] // 64) == (np.arange(128)[None, :] // 64)) / 64.0
    cm[:, CM_IOTA:CM_IOTA + 32] = np.arange(32)[None, :]
    cm[:, CM_PCOL] = np.arange(128)
    cm[:, CM_PREV] = 127.0 - np.arange(128)
    cm[0, CM_SEL:CM_SEL + 128] = 1.0
    cm[1, CM_SEL + 128:CM_SEL + 256] = 1.0
    rows = NLAT // GRID_W
    row = np.repeat(np.arange(rows), GRID_W).astype(np.float32)
    colp = np.tile(np.arange(GRID_W), rows).astype(np.float32)
    tabs = np.zeros((4, 128, T), np.float32)
    for ti, rot in ((0, 64), (2, 32)):
        nf = rot // 4
        half = rot // 2
        inv = (10000.0 ** (-np.arange(nf, dtype=np.float32) / nf)).astype(np.float32)
        ang = np.concatenate([row[:, None] * inv, colp[:, None] * inv], axis=-1).astype(np.float32)
        cos = np.cos(ang).astype(np.float32)
        sin = np.sin(ang).astype(np.float32)
        f = np.arange(128)
        j = f % half
        sign = np.where((f % rot) < half, -1.0, 1.0).astype(np.float32)
        tabs[ti, :, :NCTX] = 1.0
        tabs[ti, :, NCTX:] = cos[:, j].T
        tabs[ti + 1, :, NCTX:] = (sin[:, j] * sign[None, :]).T
    return cm, tabs


def prep_layer_arrays(inp, ls):
    f = lambda k: np.asarray(inp[k], np.float32)
    w_in = f("w_in")[ls]
    L = len(ls)
    out = {}
    out["wext"] = np.ascontiguousarray(w_in[:, :, WEXT_IDX])
    out["wuq"] = np.ascontiguousarray(f("mla_w_uq")[ls][:, :, WUQ_IDX])
    out["wukv"] = np.ascontiguousarray(f("mla_w_ukv")[ls][:, :, WUKV_IDX])
    out["adaw"] = np.ascontiguousarray(f("ada_w")[ls])
    out["wout"] = np.ascontiguousarray(f("w_out")[ls])
    out["wr"] = np.ascontiguousarray(f("router_w")[ls])
    out["w1"] = np.ascontiguousarray(f("exp_w1")[ls])
    out["w2"] = np.ascontiguousarray(f("exp_w2")[ls])
    out["b2"] = np.ascontiguousarray(f("exp_b2")[ls])
    b1 = f("exp_b1")[ls]
    b1 = b1.reshape(L, NEXP, 8, 128, 2).transpose(0, 1, 3, 2, 4).reshape(L, NEXP, 128, 16)
    out["b1c"] = np.ascontiguousarray(b1)
    rv = np.zeros((L, NRV), np.float32)
    cv = np.zeros((L, 128, NCV), np.float32)
    ng = f("norm_g")[ls]
    for i, l in enumerate(ls):
        rv[i, RV_G1:RV_G1 + 1024] = ng[i, 1]
        rv[i, RV_G3:RV_G3 + 1024] = ng[i, 3]
        rv[i, RV_G0:RV_G0 + 1024] = ng[i, 0]
        rv[i, RV_G2:RV_G2 + 1024] = ng[i, 2]
        rv[i, RV_GNW:RV_GNW + 256] = f("ret_gn_w")[l]
        rv[i, RV_GNB:RV_GNB + 256] = f("ret_gn_b")[l]
        rv[i, RV_SUBLN:RV_SUBLN + 64] = f("diff_subln")[l]
        rv[i, RV_ADAB:RV_ADAB + 6144] = f("ada_b")[l]
        rv[i, RV_RB:RV_RB + 32] = f("router_b")[l]
        rv[i, RV_DECAY:RV_DECAY + 8] = f("ret_log_decay")[l].reshape(-1)
        rv[i, RV_LAM:RV_LAM + 128] = f("diff_lambda")[l].reshape(-1)
        rv[i, RV_LAMINIT] = 0.8 - 0.6 * math.exp(-0.3 * l)
        qk = f("gqa_qk_norm")[l]
        sw = _swap_blocks(np.arange(64), 64)
        cv[i, :, CV_GQG] = np.tile(qk[0], 2)
        cv[i, :, CV_GQGS] = np.tile(qk[0][sw], 2)
        cv[i, :, CV_GKG] = np.tile(qk[1], 2)
        cv[i, :, CV_GKGS] = np.tile(qk[1][sw], 2)
        qn = f("mla_q_norm")[l]
        cv[i, :, CV_QN0] = qn[:128]
        cv[i, :64, CV_QN1] = qn[128:]
        cv[i, :, CV_KVG] = f("mla_kv_norm")[l]
        cv[i, :, CV_G0:CV_G0 + 8] = _col(ng[i, 0])
        cv[i, :, CV_G2:CV_G2 + 8] = _col(ng[i, 2])
    out["rowv"] = rv
    out["colv"] = cv
    return out


class Prog:
    def __init__(self, L, debug=(), as_input=()):
        self.L = L
        self.debug = set(debug)
        self.as_input = set(as_input)
        nc = self.nc = bass.Bass("TRN2", target_bir_lowering=False)
        di = lambda n, s, d=F32: nc.dram_tensor(n, list(s), d, kind="ExternalInput").ap()
        self.xin = di("xin", [T, D])
        self.cvec = di("cvec", [128, 16])
        self.cmat = di("cmat", [128, NCM])
        self.ropet = di("ropet", [4, 128, T])
        self.adaw = di("adaw", [L, D, 6 * D])
        self.rowv = di("rowv", [L, NRV])
        self.colv = di("colv", [L, 128, NCV])
        self.wext = di("wext", [L, D, NWEXT])
        self.wuq = di("wuq", [L, 192, 512])
        self.wukv = di("wukv", [L, 128, 512])
        self.wout = di("wout", [L, D, D])
        self.wr = di("wr", [L, D, NEXP])
        self.w1 = di("w1", [L, NEXP, D, 2 * D])
        self.b1c = di("b1c", [L, NEXP, 128, 16])
        self.w2 = di("w2", [L, NEXP, D, D])
        self.b2 = di("b2", [L, NEXP, D])
        self.yout = nc.dram_tensor("yout", [T, D], F32, kind="ExternalOutput").ap()
        self.dbg = {}
        sc = lambda n, s, d: self._scratch(n, s, d)
        self.xres = sc("xres", [T, D], F32)
        self.fm = sc("fm", [17, 128, T], BF16)
        self.tokv = sc("tokv", [T, 1152], BF16)
        self.mixd = sc("mixd", [T, D], BF16)
        self.xg = sc("xg", [NSLOT + 128, D], BF16)
        self.yg = sc("yg", [NSLOT + 128, D], F32)
        self.modd = sc("modd", [L, 2, 6 * D], F32)

    def _scratch(self, n, s, d):
        if n in getattr(self, "as_input", ()):
            return self.nc.dram_tensor(n, list(s), d, kind="ExternalInput").ap()
        kind = "ExternalOutput" if n in self.debug else "Internal"
        t = self.nc.dram_tensor(n, list(s), d, kind=kind).ap()
        if n in self.debug:
            self.dbg[n] = t
        return t


FM = dict(rq0=0, rq1=1, rk0=2, rk1=3, dq0=4, dq1=5, dk0=6, dk1=7, gq0=8, gq1=9, gk=10,
          mqn0=11, mqn1=12, mqr=13, mkn0=14, mkn1=15, mkr=16)
TV_RV, TV_RG, TV_DV, TV_GV, TV_MV = 0, 256, 512, 768, 896


ATT_GROUPS = ("diff", "gqa", "mla")
RET_STOP = int(os.environ.get("RET_STOP", "0"))
F_MODE = os.environ.get("F_MODE", "")
ATT_MAXMAPS = None


def build_program(L, debug=(), stop_after=None, as_input=(), start_at=None):
    P = Prog(L, debug, as_input)
    nc = P.nc
    with ExitStack() as top:
        S = Sched(nc, top)
        uid = [0]

        def sbt(st, n, s, d):
            uid[0] += 1
            return st.enter_context(nc.sbuf_tensor("%s_s%d" % (n, uid[0]), list(s), d))

        def pst(st, n, s, d):
            uid[0] += 1
            return st.enter_context(nc.psum_tensor("%s_p%d" % (n, uid[0]), list(s), d))
        cm = sbt(top, "cm", [128, NCM], F32)
        cmb = sbt(top, "cmb", [128, NCM], BF16)
        cvec = sbt(top, "cvec", [128, 16], F32)
        R_cm, R_cmb, R_cvec, R_hT = Res("cm"), Res("cmb"), Res("cvec"), Res("hT")
        S.dma("sp", cm[:], P.cmat[:, :], writes=[R_cm])
        S.dma("pool", cmb[:], P.cmat[:, :], writes=[R_cmb])
        S.dma("sp", cvec[:], P.cvec[:, :], writes=[R_cvec])
        R_xres = [Res("xres%d" % i) for i in range(NT)]
        for i in range(NT):
            S.dma("sp" if i % 2 else "act", P.xres[i * 128:(i + 1) * 128, :], P.xin[i * 128:(i + 1) * 128, :],
                  writes=[R_xres[i]])
        with ExitStack() as zst:
            zt = sbt(zst, "zt", [128, 8 * D], BF16)
            R_z = Res()
            S.op("pool", lambda e: e.memset(zt[:], 0.0), writes=[R_z])
            for j in range(NSLOT // 1024):
                S.dma("sp" if j % 2 else "act", P.xg[j * 1024:(j + 1) * 1024, :].rearrange("(p a) d -> p (a d)", a=8), zt[:], reads=[R_z], writes=[Res()])
            ztf = sbt(zst, "ztf", [128, D], F32)
            S.op("pool", lambda e: e.memset(ztf[:], 0.0), writes=[R_z])
            S.dma("sp", P.yg[NSLOT:NSLOT + 128, :], ztf[:], reads=[R_z], writes=[Res()])
            S.drain()
        S.op("act", lambda e: e.activation(out=cvec[:], in_=cvec[:], func=AF.Silu), reads=[R_cvec], writes=[R_cvec])
        S.drain()
        ctx = dict(P=P, S=S, sbt=sbt, pst=pst, cm=cm, cmb=cmb, cvec=cvec, hT=None, R_cm=R_cm, R_cmb=R_cmb,
                   R_cvec=R_cvec, R_hT=R_hT, R_xres=R_xres, nc=nc)
        ctx["start_at"] = start_at
        for l in range(L):
            layer(ctx, l, first=(l == 0), stop_after=stop_after)
            if stop_after:
                break
        if not stop_after:
            R_out = Res("yout")
            for i in range(NT):
                S.dma("sp" if i % 2 else "act", P.yout[i * 128:(i + 1) * 128, :], P.xres[i * 128:(i + 1) * 128, :],
                      reads=[R_xres[i]], writes=[R_out] if i == 0 else [Res("o%d" % i)])
        S.drain()
    return P


def bc(ap, parts):
    return ap.to_broadcast([parts, ap.shape[-1]])


def layer(c, l, first, stop_after=None):
    P, S, nc = c["P"], c["S"], c["nc"]
    sa = c.get("start_at")
    phase_adaln(c, l)
    S.drain()
    if stop_after == "A":
        return
    if sa is None:
        with ExitStack() as hst:
            c["hT"] = c["sbt"](hst, "hT", [128, 8, T], BF16)
            phase_modulate(c, l, which="a")
            S.drain()
            phase_proj(c, l)
            S.drain()
        c["hT"] = None
    if stop_after == "C":
        return
    if sa in (None, "R"):
        phase_retention(c, l)
        S.drain()
    if stop_after == "R":
        return
    if sa in (None, "R", "AT"):
        phase_attention(c, l)
    S.drain()
    if stop_after == "AT":
        return
    phase_outproj(c, l)
    S.drain()
    if stop_after == "E":
        return
    with ExitStack() as st:
        rt = phase_modulate(c, l, which="f", keep=st)
        S.drain()
        phase_experts(c, l)
        S.drain()
        if stop_after == "F":
            return
        phase_combine(c, l, rt)
        S.drain()


def phase_adaln(c, l):
    P, S, nc, sbt, pst = c["P"], c["S"], c["nc"], c["sbt"], c["pst"]
    cvec, R_cvec = c["cvec"], c["R_cvec"]
    with ExitStack() as st:
        aw = [sbt(st, "aw%d" % i, [128, 8, 512], F32) for i in range(2)]
        R_aw = [Res("aw0"), Res("aw1")]
        msb = sbt(st, "msb", [2, 6 * D], F32)
        adab = sbt(st, "adab", [2, 6 * D], F32)
        pm = [pst(st, "pm%d" % i, [2, 512], F32) for i in range(2)]
        R_pm = [Res("pm0"), Res("pm1")]
        R_msb, R_adab = Res("msb"), Res("adab")
        S.dma("act", adab[:], bc(P.rowv[l:l + 1, RV_ADAB:RV_ADAB + 6 * D], 2), writes=[R_adab])
        src = P.adaw[l].rearrange("(c p) n -> p c n", p=128)
        for j in range(12):
            b = j % 2
            S.dma("sp", aw[b][:], src[:, :, j * 512:(j + 1) * 512], writes=[R_aw[b]])
            for h0 in (0, 256):
                for k in range(8):
                    S.op("pe", lambda e, k=k, b=b, h0=h0: e.matmul(pm[b][:, h0:h0 + 256], lhsT=cvec[:, k::8], rhs=aw[b][:, k, h0:h0 + 256],
                                                                 start=(k == 0), stop=(k == 7)),
                         reads=[R_cvec, R_aw[b]], writes=[R_pm[b]])
            S.op("dve", lambda e, j=j, b=b: e.tensor_tensor(out=msb[:, j * 512:(j + 1) * 512], in0=pm[b][:],
                                                           in1=adab[:, j * 512:(j + 1) * 512], op=ALU.add),
                 reads=[R_pm[b], R_adab], writes=[R_msb])
        R_modd = c.setdefault("R_modd", {})
        R_modd[l] = Res("modd%d" % l)
        S.dma("sp", P.modd[l], msb[:], reads=[R_msb], writes=[R_modd[l]])
        S.drain()


def load_mod_bcast(c, l, st, part_sc, part_sh, rv_g, name):
    P, S, sbt = c["P"], c["S"], c["sbt"]
    gsc = sbt(st, "gsc" + name, [128, 2, D], F32)
    sh = sbt(st, "sh" + name, [128, 2, D], F32)
    gb = sbt(st, "gb" + name, [128, D], F32)
    R1, R2, R3 = Res("gsc"), Res("sh"), Res("gb")
    S.dma("sp", gb[:], bc(P.rowv[l:l + 1, rv_g:rv_g + D], 128), writes=[R3])
    for r in range(2):
        S.dma("sp", gsc[:, r, :], bc(P.modd[l, r:r + 1, part_sc * D:(part_sc + 1) * D], 128),
              reads=[c["R_modd"][l]], writes=[R1])
        S.dma("act", sh[:, r, :], bc(P.modd[l, r:r + 1, part_sh * D:(part_sh + 1) * D], 128),
              reads=[c["R_modd"][l]], writes=[R2])
        S.op("dve", lambda e, r=r: e.scalar_tensor_tensor(out=gsc[:, r, :], in0=gsc[:, r, :], scalar=1.0, in1=gb[:],
                                                         op0=ALU.add, op1=ALU.mult),
             reads=[R1, R3], writes=[R1])
    return gsc, sh, R1, R2


def load_gate_bcast(c, l, st, part_g, rv_g, name):
    P, S, sbt = c["P"], c["S"], c["sbt"]
    G = sbt(st, "G" + name, [128, 2, D], F32)
    gb = sbt(st, "Gb" + name, [128, D], F32)
    R1, R3 = Res("G"), Res("Gb")
    S.dma("sp", gb[:], bc(P.rowv[l:l + 1, rv_g:rv_g + D], 128), writes=[R3])
    for r in range(2):
        S.dma("act", G[:, r, :], bc(P.modd[l, r:r + 1, part_g * D:(part_g + 1) * D], 128),
              reads=[c["R_modd"][l]], writes=[R1])
        S.op("dve", lambda e, r=r: e.tensor_tensor(out=G[:, r, :], in0=G[:, r, :], in1=gb[:], op=ALU.mult),
             reads=[R1, R3], writes=[R1])
    return G, R1


def rms_rstd(S, e_reads, x_ap, n, junk, R_junk, ss, R_ss, rstd, R_rstd):
    S.op("act", lambda e: e.activation(out=junk, in_=x_ap, func=AF.Square, accum_out=ss),
         reads=e_reads, writes=[R_junk, R_ss])
    S.op("dve", lambda e: e.tensor_scalar(out=rstd, in0=ss, scalar1=1.0 / n, scalar2=EPS, op0=ALU.mult, op1=ALU.add),
         reads=[R_ss], writes=[R_rstd])
    S.op("act", lambda e: e.activation(out=rstd, in_=rstd, func=AF.Sqrt), reads=[R_rstd], writes=[R_rstd])
    S.op("dve", lambda e: e.reciprocal(out=rstd, in_=rstd), reads=[R_rstd], writes=[R_rstd])


def phase_modulate(c, l, which, keep=None):
    P, S, nc, sbt, pst = c["P"], c["S"], c["nc"], c["sbt"], c["pst"]
    cm, R_cm, hT, R_hT, R_xres = c["cm"], c["R_cm"], c["hT"], c["R_hT"], c["R_xres"]
    route = which == "f"
    rt = None
    if route:
        rt = dict()
        rt["gates"] = sbt(keep, "gatesall", [128, NT, 4], F32)
        rt["slots"] = sbt(keep, "slotsall", [128, NT, 4], U32)
        rt["R_gates"] = Res(); rt["R_slots"] = Res()
    with ExitStack() as st:
        if which == "a":
            gsc, sh, R_gsc, R_sh = load_mod_bcast(c, l, st, 1, 0, RV_G0, "a")
        else:
            gsc, sh, R_gsc, R_sh = load_mod_bcast(c, l, st, 4, 3, RV_G2, "f")
        NB = 2
        xt = [sbt(st, "xt%d" % i, [128, D], F32) for i in range(NB)]
        hx = [sbt(st, "hx%d" % i, [128, D], F32) for i in range(NB)]
        junk = sbt(st, "junk", [128, D], F32)
        ss = [sbt(st, "ss%d" % i, [128, 1], F32) for i in range(NB)]
        rstd = [sbt(st, "rstd%d" % i, [128, 1], F32) for i in range(NB)]
        ptr = [pst(st, "ptr%d" % i, [128, 8, 128], F32) for i in range(NB)]
        R_xt = [Res() for _ in range(NB)]; R_hx = [Res() for _ in range(NB)]; R_junk = Res()
        R_ss = [Res() for _ in range(NB)]; R_rstd = [Res() for _ in range(NB)]; R_ptr = [Res() for _ in range(NB)]
        if route:
            h32 = [sbt(st, "h32_%d" % i, [128, 8, 128], F32) for i in range(NB)]
            hb = [sbt(st, "hb%d" % i, [128, D], BF16) for i in range(NB)]
            R_h32 = [Res() for _ in range(NB)]; R_hb = [Res() for _ in range(NB)]
            wr = sbt(st, "wr", [128, 8, NEXP], F32)
            rb = sbt(st, "rb", [1, NEXP], F32)
            R_wr, R_rb = Res(), Res()
            S.dma("sp", wr[:], P.wr[l].rearrange("(c p) n -> p c n", p=128), writes=[R_wr])
            S.dma("sp", rb[:], P.rowv[l:l + 1, RV_RB:RV_RB + NEXP], writes=[R_rb])
            plg = [pst(st, "plg%d" % i, [128, NEXP], F32) for i in range(NB)]
            ppos = [pst(st, "ppos%d" % i, [128, NEXP], F32) for i in range(NB)]
            R_plg = [Res() for _ in range(NB)]; R_ppos = [Res() for _ in range(NB)]
            lg = sbt(st, "lg", [128, NEXP], F32); m8 = sbt(st, "m8", [128, 8], F32); i8 = sbt(st, "i8", [128, 8], U32)
            idxf = sbt(st, "idxf", [128, 4], F32); negm = sbt(st, "negm", [128, 1], F32)
            e4 = sbt(st, "e4", [128, 4], F32); se = sbt(st, "se", [128, 1], F32)
            oh = sbt(st, "oh", [128, 4, NEXP], F32); mask = sbt(st, "mask", [128, NEXP], F32)
            cum = sbt(st, "cum", [128, NEXP], F32); posk = sbt(st, "posk", [128, 4], F32)
            j32 = sbt(st, "j32", [128, NEXP], F32); slf = sbt(st, "slf", [128, 4], F32); valid = sbt(st, "valid", [128, 4], F32)
            R_r = Res("route_tmp"); R_cum = Res("cum"); R_mask = Res("mask")
            S.op("pool", lambda e: e.memset(cum[:], 0.0), writes=[R_cum])
            R_xg = c.setdefault("R_xg", Res("xg"))
            rt["R_scatter"] = []
        for i in range(NT):
            b = i % NB
            r = 1 if i < 2 else 0
            S.dma("sp" if i % 2 else "act", xt[b][:], P.xres[i * 128:(i + 1) * 128, :], reads=[R_xres[i]], writes=[R_xt[b]])
            rms_rstd(S, [R_xt[b]], xt[b][:], D, junk[:], R_junk, ss[b][:], R_ss[b], rstd[b][:], R_rstd[b])
            S.op("dve", lambda e, b=b, r=r: e.scalar_tensor_tensor(out=hx[b][:], in0=xt[b][:], scalar=rstd[b][:, 0:1],
                                                                   in1=gsc[:, r, :], op0=ALU.mult, op1=ALU.mult),
                 reads=[R_xt[b], R_rstd[b], R_gsc], writes=[R_hx[b]])
            S.op("pool", lambda e, b=b, r=r: e.tensor_tensor(out=hx[b][:], in0=hx[b][:], in1=sh[:, r, :], op=ALU.add),
                 reads=[R_hx[b], R_sh], writes=[R_hx[b]])
            for k in range(8):
                S.op("pe", lambda e, b=b, k=k: e.transpose(out=ptr[b][:, k, :], in_=hx[b][:, k * 128:(k + 1) * 128],
                                                          identity=cm[:, CM_ID:CM_ID + 128]),
                     reads=[R_hx[b], R_cm], writes=[R_ptr[b]])
            if not route:
                S.op("act", lambda e, b=b, i=i: e.activation(out=hT[:, :, i * 128:(i + 1) * 128], in_=ptr[b][:], func=AF.Copy),
                     reads=[R_ptr[b]], writes=[R_hT])
                continue
            S.op("act", lambda e, b=b: e.activation(out=h32[b][:], in_=ptr[b][:], func=AF.Copy),
                 reads=[R_ptr[b]], writes=[R_h32[b]])
            S.op("pool", lambda e, b=b: e.tensor_copy(out=hb[b][:], in_=hx[b][:]), reads=[R_hx[b]], writes=[R_hb[b]])
            for k in range(8):
                S.op("pe", lambda e, b=b, k=k: e.matmul(plg[b][:], lhsT=h32[b][:, k, :], rhs=wr[:, k, :], start=(k == 0), stop=False),
                     reads=[R_h32[b], R_wr], writes=[R_plg[b]])
            S.op("pe", lambda e, b=b: e.matmul(plg[b][:], lhsT=cm[0:1, CM_ONES:CM_ONES + 128], rhs=rb[:], start=False, stop=True),
                 reads=[R_cm, R_rb], writes=[R_plg[b]])
            gates_i = rt["gates"][:, i, :]
            S.op("dve", lambda e, b=b: e.tensor_copy(out=lg[:], in_=plg[b][:]), reads=[R_plg[b]], writes=[R_r])
            S.op("dve", lambda e: e.max(out=m8[:], in_=lg[:]), reads=[R_r], writes=[R_r])
            S.op("dve", lambda e: e.max_index(out=i8[:], in_max=m8[:], in_values=lg[:]), reads=[R_r], writes=[R_r])
            S.op("dve", lambda e: e.tensor_scalar(out=negm[:], in0=m8[:, 0:1], scalar1=-1.0, scalar2=None, op0=ALU.mult),
                 reads=[R_r], writes=[R_r])
            S.op("act", lambda e: e.activation(out=e4[:], in_=m8[:, 0:4], func=AF.Exp, bias=negm[:, 0:1], accum_out=se[:]),
                 reads=[R_r], writes=[R_r])
            S.op("dve", lambda e: e.reciprocal(out=se[:], in_=se[:]), reads=[R_r], writes=[R_r])
            S.op("dve", lambda e, g=gates_i: e.tensor_scalar(out=g, in0=e4[:], scalar1=se[:, 0:1], scalar2=None, op0=ALU.mult),
                 reads=[R_r], writes=[rt["R_gates"]])
            S.op("dve", lambda e: e.tensor_copy(out=idxf[:], in_=i8[:, 0:4]), reads=[R_r], writes=[R_r])
            for k in range(4):
                S.op("dve", lambda e, k=k: e.tensor_scalar(out=oh[:, k, :], in0=cm[:, CM_IOTA:CM_IOTA + NEXP], scalar1=idxf[:, k:k + 1],
                                                          scalar2=None, op0=ALU.is_equal),
                     reads=[R_r, R_cm], writes=[R_r])
            S.op("dve", lambda e: e.tensor_tensor(out=mask[:], in0=oh[:, 0, :], in1=oh[:, 1, :], op=ALU.add), reads=[R_r], writes=[R_mask])
            S.op("dve", lambda e: e.tensor_tensor(out=mask[:], in0=mask[:], in1=oh[:, 2, :], op=ALU.add), reads=[R_r, R_mask], writes=[R_mask])
            S.op("dve", lambda e: e.tensor_tensor(out=mask[:], in0=mask[:], in1=oh[:, 3, :], op=ALU.add), reads=[R_r, R_mask], writes=[R_mask])
            S.op("pe", lambda e, b=b: e.matmul(ppos[b][:], lhsT=cm[:, CM_LTRI:CM_LTRI + 128], rhs=mask[:], start=True, stop=False),
                 reads=[R_cm, R_mask], writes=[R_ppos[b]])
            S.op("pe", lambda e, b=b: e.matmul(ppos[b][:], lhsT=cm[:, CM_ONES:CM_ONES + 128], rhs=cum[:], start=False, stop=True),
                 reads=[R_cm, R_cum], writes=[R_ppos[b]])
            S.op("dve", lambda e: e.tensor_tensor(out=cum[:], in0=cum[:], in1=mask[:], op=ALU.add), reads=[R_mask, R_cum], writes=[R_cum])
            for k in range(4):
                S.op("dve", lambda e, b=b, k=k: e.tensor_tensor(out=j32[:], in0=oh[:, k, :], in1=ppos[b][:], op=ALU.mult),
                     reads=[R_r, R_ppos[b]], writes=[R_r])
                S.op("dve", lambda e, k=k: e.tensor_reduce(out=posk[:, k:k + 1], in_=j32[:], axis=AX.X, op=ALU.add),
                     reads=[R_r], writes=[R_r])
            S.op("dve", lambda e: e.scalar_tensor_tensor(out=slf[:], in0=idxf[:], scalar=float(CAP), in1=posk[:], op0=ALU.mult, op1=ALU.add),
                 reads=[R_r], writes=[R_r])
            S.op("dve", lambda e: e.tensor_scalar(out=valid[:], in0=posk[:], scalar1=float(CAP), scalar2=None, op0=ALU.is_lt), reads=[R_r], writes=[R_r])
            S.op("dve", lambda e: e.tensor_scalar(out=slf[:], in0=slf[:], scalar1=-float(NSLOT), scalar2=None, op0=ALU.add), reads=[R_r], writes=[R_r])
            S.op("dve", lambda e: e.tensor_tensor(out=slf[:], in0=slf[:], in1=valid[:], op=ALU.mult), reads=[R_r], writes=[R_r])
            S.op("dve", lambda e: e.tensor_scalar(out=slf[:], in0=slf[:], scalar1=float(NSLOT), scalar2=None, op0=ALU.add), reads=[R_r], writes=[R_r])
            S.op("dve", lambda e, g=gates_i: e.tensor_tensor(out=g, in0=g, in1=valid[:], op=ALU.mult), reads=[R_r, rt["R_gates"]], writes=[rt["R_gates"]])
            S.op("dve", lambda e, i=i: e.tensor_copy(out=rt["slots"][:, i, :], in_=slf[:]), reads=[R_r], writes=[rt["R_slots"]])
            for k in range(4):
                Rs = Res("sc")
                rt["R_scatter"].append(Rs)
                S.dma("pool", P.xg[:, :], hb[b][:], reads=[R_hb[b], rt["R_slots"]], writes=[Rs],
                      indirect=dict(out_offset=bass.IndirectOffsetOnAxis(ap=rt["slots"][:, i, k:k + 1], axis=0), in_offset=None))
        S.drain()
    return rt


def phase_proj(c, l):
    P, S, nc, sbt, pst = c["P"], c["S"], c["nc"], c["sbt"], c["pst"]
    cm, R_cm, hT, R_hT, cmb = c["cm"], c["R_cm"], c["hT"], c["R_hT"], c["cmb"]
    R_fm = c.setdefault("R_fm", Res("fm")); R_tokv = c.setdefault("R_tokv", Res("tokv"))
    with ExitStack() as st:
        sq = [sbt(st, "sq%d" % i, [128, 512], BF16) for i in range(2)]
        rs = [sbt(st, "rs%d" % i, [128, 512], F32) for i in range(2)]
        R_sq = [Res(), Res()]; R_rs = [Res(), Res()]
        cqn0 = sbt(st, "cqn0", [128, 512], BF16); cqn1 = sbt(st, "cqn1", [64, 512], BF16); ckvn = sbt(st, "ckvn", [128, 512], BF16)
        R_cqn, R_ckvn = Res(), Res()
        wx = sbt(st, "wx", [128, 8, NWEXT], BF16)
        wuqA = sbt(st, "wuqA", [128, 512], BF16); wuqB = sbt(st, "wuqB", [64, 512], BF16)
        wukv = sbt(st, "wukvx", [128, 512], BF16)
        colv = sbt(st, "colv", [128, NCV], F32)
        R_wx, R_w2, R_colv = Res("wx"), Res("w2nd"), Res("colv")
        src = P.wext[l].rearrange("(c p) n -> p c n", p=128)
        for k in range(8):
            for (a, b) in ((0, 2048), (2048, 4096), (4096, NWEXT)):
                S.dma("pool", wx[:, k, a:b], src[:, k, a:b], writes=[R_wx])
        S.dma("pool", wuqA[:], P.wuq[l, 0:128, :], writes=[R_w2])
        S.dma("pool", wuqB[:], P.wuq[l, 128:192, :], writes=[R_w2])
        S.dma("pool", wukv[:], P.wukv[l], writes=[R_w2])
        S.dma("sp", colv[:], P.colv[l], writes=[R_colv])
        tabs = [sbt(st, "tabs%d" % i, [128, 4, 512], F32) for i in range(2)]
        R_tabs = [Res(), Res()]
        NPS = 2
        pa = [pst(st, "pa%d" % i, [128, 512], F32) for i in range(NPS)]
        pb = [pst(st, "pb%d" % i, [128, 512], F32) for i in range(NPS)]
        pc = [pst(st, "pc%d" % i, [128, 512], F32) for i in range(NPS)]
        pd = [pst(st, "pd%d" % i, [128, 512], F32) for i in range(NPS)]
        R_pa = [Res() for _ in range(NPS)]; R_pb = [Res() for _ in range(NPS)]
        R_pc = [Res() for _ in range(NPS)]; R_pd = [Res() for _ in range(NPS)]
        NW = 3
        t1 = [sbt(st, "t1_%d" % i, [128, 512], F32) for i in range(NW)]
        t2 = [sbt(st, "t2_%d" % i, [128, 512], F32) for i in range(NW)]
        ob = [sbt(st, "ob%d" % i, [128, 512], BF16) for i in range(NW)]
        R_t1 = [Res() for _ in range(NW)]; R_t2 = [Res() for _ in range(NW)]; R_ob = [Res() for _ in range(NW)]
        tvs = [sbt(st, "tvs%d" % i, [128, 1152], BF16) for i in range(2)]
        R_tvs = [Res(), Res()]
        cnt = dict(w=0, a=0, b=0, c=0, d=0, ev=0, q=0)

        def nxt(key, n):
            v = cnt[key]; cnt[key] = (v + 1) % n
            return v

        def group(ps, R_ps, u, tok0, W, M=128, wtile=None, rhs=None, R_rhs=None):
            for k in range(8):
                S.op("pe", lambda e, k=k: e.matmul(ps[0:M, 0:W], lhsT=wx[:, k, u * 128:u * 128 + M], rhs=hT[:, k, tok0:tok0 + W],
                                                   start=(k == 0), stop=(k == 7)),
                     reads=[R_wx, R_hT], writes=[R_ps])

        def store_fm(name, src_tile, R_src, tok0, W, M=128):
            q = ("sp", "act")[nxt("q", 2)]
            S.dma(q, P.fm[FM[name], 0:M, tok0:tok0 + W], src_tile[0:M, 0:W], reads=[R_src], writes=[Res()])

        def rope_out(name, p1, R_p1, p2, R_p2, tb, tcos, tok0, W, g=None, gs=None, rstd=None, R_rstd=None):
            w = nxt("w", NW)
            cos = tabs[tb][:, tcos, 0:W]; sin = tabs[tb][:, tcos + 1, 0:W]
            if g is None:
                S.op("dve", lambda e: e.tensor_tensor(out=t1[w][:, 0:W], in0=p1[:, 0:W], in1=cos, op=ALU.mult),
                     reads=[R_p1, R_tabs[tb]], writes=[R_t1[w]])
                S.op("dve", lambda e: e.tensor_tensor(out=t2[w][:, 0:W], in0=p2[:, 0:W], in1=sin, op=ALU.mult),
                     reads=[R_p2, R_tabs[tb]], writes=[R_t2[w]])
                S.op("pool", lambda e: e.tensor_tensor(out=ob[w][:, 0:W], in0=t1[w][:, 0:W], in1=t2[w][:, 0:W], op=ALU.add),
                     reads=[R_t1[w], R_t2[w]], writes=[R_ob[w]])
            else:
                S.op("dve", lambda e: e.scalar_tensor_tensor(out=t1[w][:, 0:W], in0=p1[:, 0:W], scalar=g, in1=cos, op0=ALU.mult, op1=ALU.mult),
                     reads=[R_p1, R_tabs[tb], R_colv], writes=[R_t1[w]])
                S.op("dve", lambda e: e.scalar_tensor_tensor(out=t2[w][:, 0:W], in0=p2[:, 0:W], scalar=gs, in1=sin, op0=ALU.mult, op1=ALU.mult),
                     reads=[R_p2, R_tabs[tb], R_colv], writes=[R_t2[w]])
                S.op("pool", lambda e: e.tensor_tensor(out=t1[w][:, 0:W], in0=t1[w][:, 0:W], in1=t2[w][:, 0:W], op=ALU.add),
                     reads=[R_t1[w], R_t2[w]], writes=[R_t1[w]])
                S.op("pool", lambda e: e.tensor_tensor(out=ob[w][:, 0:W], in0=t1[w][:, 0:W], in1=rstd[:, 0:W], op=ALU.mult),
                     reads=[R_t1[w], R_rstd], writes=[R_ob[w]])
            store_fm(name, ob[w], R_ob[w], tok0, W)

        def rstd_from_ms(ps, R_ps, scale, si, W, M=128):
            S.op("dve", lambda e: e.tensor_scalar(out=rs[si][0:M, 0:W], in0=ps[0:M, 0:W], scalar1=scale, scalar2=EPS, op0=ALU.mult, op1=ALU.add),
                 reads=[R_ps], writes=[R_rs[si]])
            S.op("act", lambda e: e.activation(out=rs[si][0:M, 0:W], in_=rs[si][0:M, 0:W], func=AF.Sqrt), reads=[R_rs[si]], writes=[R_rs[si]])
            S.op("dve", lambda e: e.reciprocal(out=rs[si][0:M, 0:W], in_=rs[si][0:M, 0:W]), reads=[R_rs[si]], writes=[R_rs[si]])

        def do_block(bi, tok0, W):
            tb = bi % 2
            S.dma("sp", tabs[tb][:, :, 0:W], P.ropet[:, :, tok0:tok0 + W].rearrange("t p n -> p t n"), writes=[R_tabs[tb]])
            for name, u, tc in (("rq0", 0, 0), ("rq1", 1, 0), ("rk0", 2, 0), ("rk1", 3, 0),
                                ("dq0", 4, 2), ("dq1", 5, 2), ("dk0", 6, 2), ("dk1", 7, 2), ("mkr", 11, 2)):
                a = nxt("a", NPS); b = nxt("b", NPS)
                group(pa[a], R_pa[a], u, tok0, W)
                group(pb[b], R_pb[b], u + U_SW, tok0, W)
                rope_out(name, pa[a], R_pa[a], pb[b], R_pb[b], tb, tc, tok0, W)
            for name, u, cg in (("gq0", 8, CV_GQG), ("gq1", 9, CV_GQG), ("gk", 10, CV_GKG)):
                a = nxt("a", NPS); b = nxt("b", NPS); cc = nxt("c", NPS)
                group(pa[a], R_pa[a], u, tok0, W)
                group(pb[b], R_pb[b], u + U_SW, tok0, W)
                S.op("act", lambda e, a=a: e.activation(out=sq[0][:, 0:W], in_=pa[a][:, 0:W], func=AF.Square),
                     reads=[R_pa[a]], writes=[R_sq[0]])
                for h0 in range(0, W, 256):
                    S.op("pe", lambda e, cc=cc, h0=h0: e.matmul(pc[cc][:, h0:h0 + 256], lhsT=cmb[:, CM_BLK64:CM_BLK64 + 128], rhs=sq[0][:, h0:h0 + 256], start=True, stop=True),
                         reads=[R_cm, R_sq[0]], writes=[R_pc[cc]])
                if "dbgC" in P.debug and bi == 0 and name == "gq0":
                    dd = nc.dram_tensor("dbgC", [3, 128, 512], F32, kind="ExternalOutput").ap()
                    S.op("dve", lambda e, cc=cc: e.tensor_copy(out=t1[0][:], in_=pc[cc][:]), reads=[R_pc[cc]], writes=[R_t1[0]])
                    S.dma("sp", dd[1], t1[0][:], reads=[R_t1[0]], writes=[Res()])
                    S.op("dve", lambda e, a=a: e.tensor_copy(out=t2[0][:], in_=pa[a][:]), reads=[R_pa[a]], writes=[R_t2[0]])
                    S.dma("sp", dd[2], t2[0][:], reads=[R_t2[0]], writes=[Res()])
                rstd_from_ms(pc[cc], R_pc[cc], 1.0, 0, W)
                rope_out(name, pa[a], R_pa[a], pb[b], R_pb[b], tb, 0, tok0, W, g=colv[:, cg:cg + 1], gs=colv[:, cg + 1:cg + 2],
                         rstd=rs[0], R_rstd=R_rs[0])
            a = nxt("a", NPS); b = nxt("b", NPS); cc = nxt("c", NPS)
            group(pa[a], R_pa[a], U_CQ0, tok0, W)
            group(pb[b], R_pb[b], U_CQ1, tok0, W, M=64)
            S.op("act", lambda e, a=a: e.activation(out=sq[0][:, 0:W], in_=pa[a][:, 0:W], func=AF.Square), reads=[R_pa[a]], writes=[R_sq[0]])
            S.op("act", lambda e, b=b: e.activation(out=sq[1][0:64, 0:W], in_=pb[b][0:64, 0:W], func=AF.Square), reads=[R_pb[b]], writes=[R_sq[1]])
            for h0 in range(0, W, 256):
                S.op("pe", lambda e, cc=cc, h0=h0: e.matmul(pc[cc][:, h0:h0 + 256], lhsT=cmb[:, CM_ONES:CM_ONES + 128], rhs=sq[0][:, h0:h0 + 256], start=True, stop=False),
                     reads=[R_cm, R_sq[0]], writes=[R_pc[cc]])
                S.op("pe", lambda e, cc=cc, h0=h0: e.matmul(pc[cc][:, h0:h0 + 256], lhsT=cmb[0:64, CM_ONES:CM_ONES + 128], rhs=sq[1][0:64, h0:h0 + 256], start=False, stop=True),
                     reads=[R_cm, R_sq[1]], writes=[R_pc[cc]])
            rstd_from_ms(pc[cc], R_pc[cc], 1.0 / 192, 0, W)
            S.op("dve", lambda e, a=a: e.scalar_tensor_tensor(out=cqn0[:, 0:W], in0=pa[a][:, 0:W], scalar=colv[:, CV_QN0:CV_QN0 + 1], in1=rs[0][:, 0:W],
                                                             op0=ALU.mult, op1=ALU.mult), reads=[R_pa[a], R_rs[0], R_colv], writes=[R_cqn])
            S.op("dve", lambda e, b=b: e.scalar_tensor_tensor(out=cqn1[:, 0:W], in0=pb[b][0:64, 0:W], scalar=colv[0:64, CV_QN1:CV_QN1 + 1], in1=rs[0][0:64, 0:W],
                                                             op0=ALU.mult, op1=ALU.mult), reads=[R_pb[b], R_rs[0], R_colv], writes=[R_cqn])
            a = nxt("a", NPS); cc = nxt("c", NPS)
            group(pa[a], R_pa[a], U_CKV, tok0, W)
            S.op("act", lambda e, a=a: e.activation(out=sq[0][:, 0:W], in_=pa[a][:, 0:W], func=AF.Square), reads=[R_pa[a]], writes=[R_sq[0]])
            for h0 in range(0, W, 256):
                S.op("pe", lambda e, cc=cc, h0=h0: e.matmul(pc[cc][:, h0:h0 + 256], lhsT=cmb[:, CM_ONES:CM_ONES + 128], rhs=sq[0][:, h0:h0 + 256], start=True, stop=True),
                     reads=[R_cm, R_sq[0]], writes=[R_pc[cc]])
            rstd_from_ms(pc[cc], R_pc[cc], 1.0 / 128, 1, W)
            S.op("dve", lambda e, a=a: e.scalar_tensor_tensor(out=ckvn[:, 0:W], in0=pa[a][:, 0:W], scalar=colv[:, CV_KVG:CV_KVG + 1], in1=rs[1][:, 0:W],
                                                             op0=ALU.mult, op1=ALU.mult), reads=[R_pa[a], R_rs[1], R_colv], writes=[R_ckvn])

            def uq(ps, R_ps, c0):
                S.op("pe", lambda e: e.matmul(ps[:, 0:W], lhsT=wuqA[:, c0:c0 + 128], rhs=cqn0[:, 0:W], start=True, stop=False),
                     reads=[R_w2, R_cqn], writes=[R_ps])
                S.op("pe", lambda e: e.matmul(ps[:, 0:W], lhsT=wuqB[:, c0:c0 + 128], rhs=cqn1[:, 0:W], start=False, stop=True),
                     reads=[R_w2, R_cqn], writes=[R_ps])

            def plain_out(name, ps, R_ps):
                w = nxt("w", NW)
                if nxt("ev", 2):
                    S.op("act", lambda e: e.activation(out=ob[w][:, 0:W], in_=ps[:, 0:W], func=AF.Copy), reads=[R_ps], writes=[R_ob[w]])
                else:
                    S.op("dve", lambda e: e.tensor_copy(out=ob[w][:, 0:W], in_=ps[:, 0:W]), reads=[R_ps], writes=[R_ob[w]])
                store_fm(name, ob[w], R_ob[w], tok0, W)

            for name, c0 in (("mqn0", 0), ("mqn1", 128)):
                d = nxt("d", NPS)
                uq(pd[d], R_pd[d], c0)
                plain_out(name, pd[d], R_pd[d])
            a = nxt("a", NPS); b = nxt("b", NPS)
            uq(pa[a], R_pa[a], 256)
            uq(pb[b], R_pb[b], 384)
            rope_out("mqr", pa[a], R_pa[a], pb[b], R_pb[b], tb, 2, tok0, W)
            for name, c0 in (("mkn0", 0), ("mkn1", 128)):
                d = nxt("d", NPS)
                S.op("pe", lambda e, d=d, c0=c0: e.matmul(pd[d][:, 0:W], lhsT=wukv[:, c0:c0 + 128], rhs=ckvn[:, 0:W], start=True, stop=True),
                     reads=[R_w2, R_ckvn], writes=[R_pd[d]])
                plain_out(name, pd[d], R_pd[d])
            for j in range(W // 128):
                t0 = tok0 + j * 128
                v = (t0 // 128) % 2
                d = nxt("d", NPS); cc = nxt("c", NPS); a = nxt("a", NPS)
                for k in range(8):
                    S.op("pe", lambda e, k=k, d=d, t0=t0: e.matmul(pd[d][:, 0:512], lhsT=hT[:, k, t0:t0 + 128], rhs=wx[:, k, TOK_OFF:TOK_OFF + 512],
                                                                 start=(k == 0), stop=(k == 7)), reads=[R_wx, R_hT], writes=[R_pd[d]])
                for k in range(8):
                    S.op("pe", lambda e, k=k, cc=cc, t0=t0: e.matmul(pc[cc][:, 0:384], lhsT=hT[:, k, t0:t0 + 128], rhs=wx[:, k, TOK_OFF + 512:TOK_OFF + 896],
                                                                   start=(k == 0), stop=(k == 7)), reads=[R_wx, R_hT], writes=[R_pc[cc]])
                S.op("pe", lambda e, a=a, j=j: e.matmul(pa[a][:, 0:256], lhsT=ckvn[:, j * 128:(j + 1) * 128], rhs=wukv[:, 256:512], start=True, stop=True),
                     reads=[R_w2, R_ckvn], writes=[R_pa[a]])
                S.op("dve", lambda e, d=d, v=v: e.tensor_copy(out=tvs[v][:, TV_RV:TV_RV + 256], in_=pd[d][:, 0:256]), reads=[R_pd[d]], writes=[R_tvs[v]])
                S.op("act", lambda e, d=d, v=v: e.activation(out=tvs[v][:, TV_RG:TV_RG + 256], in_=pd[d][:, 256:512], func=AF.Silu), reads=[R_pd[d]], writes=[R_tvs[v]])
                S.op("dve", lambda e, cc=cc, v=v: e.tensor_copy(out=tvs[v][:, TV_DV:TV_DV + 384], in_=pc[cc][:, 0:384]), reads=[R_pc[cc]], writes=[R_tvs[v]])
                S.op("act", lambda e, a=a, v=v: e.activation(out=tvs[v][:, TV_MV:TV_MV + 256], in_=pa[a][:, 0:256], func=AF.Copy), reads=[R_pa[a]], writes=[R_tvs[v]])
                S.dma("sp", P.tokv[t0:t0 + 128, :], tvs[v][:], reads=[R_tvs[v]], writes=[Res()])

        for bi, (tok0, W) in enumerate(TB):
            do_block(bi, tok0, W)
        S.drain()


QBLOCKS = [(0, 256, 2)] + [(256 + 512 * j, 512, NT) for j in range(8)]


def phase_attention(c, l):
    P, S, nc, sbt, pst = c["P"], c["S"], c["nc"], c["sbt"], c["pst"]
    for grp in ATT_GROUPS:
        with ExitStack() as st:
            names = dict(diff=("dq0", "dq1", "dk0", "dk1"), gqa=("gq0", "gq1", "gk"),
                         mla=("mqn0", "mqn1", "mqr", "mkn0", "mkn1", "mkr"))[grp]
            fmt = {}
            R_in = Res("attn_in")
            if grp == "mla":
                Kc, Qc = [], []
                for h in range(4):
                    kc_ = sbt(st, "Kc%d" % h, [96, T], BF16); qc_ = sbt(st, "Qc%d" % h, [96, T], BF16)
                    r0 = 64 * (h % 2)
                    S.dma("sp", kc_[0:64, :], P.fm[FM["mkn%d" % (h // 2)], r0:r0 + 64, :], writes=[R_in])
                    S.dma("act", kc_[64:96, :], P.fm[FM["mkr"], 0:32, :], writes=[R_in])
                    S.dma("sp", qc_[0:64, :], P.fm[FM["mqn%d" % (h // 2)], r0:r0 + 64, :], writes=[R_in])
                    S.dma("act", qc_[64:96, :], P.fm[FM["mqr"], 32 * h:32 * h + 32, :], writes=[R_in])
                    Kc.append(kc_); Qc.append(qc_)
                names = ()
            for i, n in enumerate(names):
                fmt[n] = sbt(st, "fm_" + n, [128, T], BF16)
                S.dma(("sp", "act")[i % 2], fmt[n][:], P.fm[FM[n]], writes=[R_in])
            nh, voff, moff = dict(diff=(4, TV_DV, 256), gqa=(2, TV_GV, 512), mla=(4, TV_MV, 768))[grp]
            vraw = sbt(st, "vraw", [128, NT, nh * 64], BF16)
            vaug = sbt(st, "vaug", [128, NT, nh, 65], BF16)
            S.dma("sp", vraw[:], P.tokv[:, voff:voff + nh * 64].rearrange("(i p) c -> p i c", p=128), writes=[R_in])
            S.op("pool", lambda e: e.memset(vaug[:], 1.0), writes=[R_in])
            S.op("pool", lambda e: e.tensor_copy(out=vaug[:, :, :, 0:64], in_=vraw[:].rearrange("p i (h d) -> p i h d", h=nh)),
                 reads=[R_in], writes=[R_in])
            kz = {}
            for n in dict(diff=("dk0", "dk1"), gqa=(), mla=())[grp]:
                kz[n] = sbt(st, "kz_" + n, [128, T], BF16)
                S.op("pool", lambda e, n=n: e.tensor_copy(out=kz[n][64:128, :], in_=fmt[n][64:128, :]), reads=[R_in], writes=[R_in])
                S.op("pool", lambda e, n=n: e.memset(kz[n][64:96, :], 0.0), reads=[R_in], writes=[R_in])
            mixo = sbt(st, "mixo", [128, NT, 256], BF16)
            R_mixo = Res("mixo")
            stp = [pst(st, "stp%d" % i, [128, 512], F32) for i in range(2)]
            po = [pst(st, "po%d" % i, [128, 512], F32) for i in range(4)]
            R_stp = [Res(), Res()]; R_po = [Res() for _ in range(4)]
            NE = 4
            eb = [sbt(st, "eb%d" % i, [128, 512], BF16) for i in range(NE)]
            R_eb = [Res() for _ in range(NE)]
            rden = sbt(st, "rden", [128, 4], F32); R_rden = Res()
            cnt = dict(s=0, e=0)
            if grp == "diff":
                d1 = sbt(st, "d1", [128, NT, 64], F32); R_d1 = Res()
                lamr = sbt(st, "lamr", [128, 128], F32); lam2 = sbt(st, "lam2", [128, 2], F32)
                neglam = sbt(st, "neglam", [128, 1], F32); li = sbt(st, "li", [128, 1], F32)
                subb = sbt(st, "subb", [128, 64], F32)
                o2 = sbt(st, "o2", [128, 64], F32); cmbt = sbt(st, "cmbt", [128, 64], F32); jk = sbt(st, "jk", [128, 64], F32)
                ssd = sbt(st, "ssd", [128, 1], F32)
                R_lam = Res(); R_t = Res()
                S.dma("sp", lamr[:], bc(P.rowv[l:l + 1, RV_LAM:RV_LAM + 128], 128), writes=[R_lam])
                S.dma("sp", li[:], bc(P.rowv[l:l + 1, RV_LAMINIT:RV_LAMINIT + 1], 128), writes=[R_lam])
                S.dma("sp", subb[:], bc(P.rowv[l:l + 1, RV_SUBLN:RV_SUBLN + 64], 128), writes=[R_lam])
                S.op("dve", lambda e: e.tensor_tensor(out=lamr[:, 0:32], in0=lamr[:, 0:32], in1=lamr[:, 32:64], op=ALU.mult), reads=[R_lam], writes=[R_lam])
                S.op("dve", lambda e: e.tensor_tensor(out=lamr[:, 64:96], in0=lamr[:, 64:96], in1=lamr[:, 96:128], op=ALU.mult), reads=[R_lam], writes=[R_lam])
                S.op("dve", lambda e: e.tensor_reduce(out=lam2[:, 0:1], in_=lamr[:, 0:32], axis=AX.X, op=ALU.add), reads=[R_lam], writes=[R_lam])
                S.op("dve", lambda e: e.tensor_reduce(out=lam2[:, 1:2], in_=lamr[:, 64:96], axis=AX.X, op=ALU.add), reads=[R_lam], writes=[R_lam])
                S.op("act", lambda e: e.activation(out=lam2[:], in_=lam2[:], func=AF.Exp), reads=[R_lam], writes=[R_lam])
                S.op("dve", lambda e: e.tensor_tensor(out=neglam[:], in0=lam2[:, 1:2], in1=lam2[:, 0:1], op=ALU.subtract), reads=[R_lam], writes=[R_lam])
                S.op("dve", lambda e: e.tensor_tensor(out=neglam[:], in0=neglam[:], in1=li[:], op=ALU.subtract), reads=[R_lam], writes=[R_lam])
                S.op("dve", lambda e: e.tensor_scalar(out=li[:], in0=li[:], scalar1=-1.0, scalar2=1.0, op0=ALU.mult, op1=ALU.add), reads=[R_lam], writes=[R_lam])
                S.op("dve", lambda e: e.tensor_scalar(out=subb[:], in0=subb[:], scalar1=li[:, 0:1], scalar2=None, op0=ALU.mult), reads=[R_lam], writes=[R_lam])

            nmaps = [0]

            def run_map(parts, vh, scale, finish):
                nmaps[0] += 1
                if ATT_MAXMAPS is not None and nmaps[0] > ATT_MAXMAPS:
                    return
                dbg = "dbgAT" in P.debug and nmaps[0] == 1
                if dbg:
                    dd = nc.dram_tensor("dbgAT", [3, 128, 512], F32, kind="ExternalOutput").ap()
                    dv = nc.dram_tensor("dbgV", [128, NT * nh * 65], BF16, kind="ExternalOutput").ap()
                    dt1 = sbt(st, "dt1", [128, 512], F32); dt2 = sbt(st, "dt2", [128, 512], F32); dt3 = sbt(st, "dt3", [128, 512], F32)
                    S.dma("sp", dv, vaug[:].rearrange("p a b c -> p (a b c)"), reads=[R_in], writes=[Res()])
                for (q0, N, nkt) in (QBLOCKS[:2] if ATT_MAXMAPS is not None else QBLOCKS):
                    nqs = N // 128

                    def score(kt, q0=q0, N=N):
                        sb_ = cnt["s"]; cnt["s"] = (sb_ + 1) % 2
                        eb_ = cnt["e"]; cnt["e"] = (eb_ + 1) % NE
                        for pi, (Kt, Qt, r0, nr) in enumerate(parts):
                            S.op("pe", lambda e, Kt=Kt, Qt=Qt, r0=r0, nr=nr, kt=kt, sb_=sb_, pi=pi: e.matmul(
                                stp[sb_][:, 0:N], lhsT=Kt[r0:r0 + nr, kt * 128:(kt + 1) * 128], rhs=Qt[r0:r0 + nr, q0:q0 + N],
                                start=(pi == 0), stop=(pi == len(parts) - 1)), reads=[R_in], writes=[R_stp[sb_]])
                        S.op("act", lambda e, sb_=sb_, eb_=eb_: e.activation(out=eb[eb_][:, 0:N], in_=stp[sb_][:, 0:N], func=AF.Exp, scale=scale),
                             reads=[R_stp[sb_]], writes=[R_eb[eb_]])
                        return eb_

                    def pv(kt, eb_, nkt=nkt, nqs=nqs):
                        for qs in range(nqs):
                            S.op("pe", lambda e, qs=qs: e.matmul(
                                po[qs][:, 0:65], lhsT=eb[eb_][:, qs * 128:(qs + 1) * 128], rhs=vaug[:, kt, vh, :],
                                start=(kt == 0), stop=(kt == nkt - 1)), reads=[R_eb[eb_], R_in], writes=[R_po[qs]])

                    pend = score(0)
                    for kt in range(nkt):
                        nxt_e = score(kt + 1) if kt + 1 < nkt else None
                        pv(kt, pend)
                        pend = nxt_e
                    if dbg and q0 == 0:
                        Rd = Res()
                        S.op("dve", lambda e: e.tensor_copy(out=dt3[:], in_=po[0][:]), reads=[R_po[0]], writes=[Rd])
                        S.dma("sp", dd[0], dt1[:], reads=[Rd], writes=[Res()])
                        S.dma("sp", dd[1], dt2[:], reads=[Rd], writes=[Res()])
                        S.dma("sp", dd[2], dt3[:], reads=[Rd], writes=[Res()])
                    for qs in range(nqs):
                        ti = (q0 // 128) + qs
                        S.op("dve", lambda e, qs=qs: e.tensor_copy(out=rden[:, qs:qs + 1], in_=po[qs][:, 64:65]), reads=[R_po[qs]], writes=[R_rden])
                        S.op("dve", lambda e, qs=qs: e.reciprocal(out=rden[:, qs:qs + 1], in_=rden[:, qs:qs + 1]), reads=[R_rden], writes=[R_rden])
                        finish(qs, ti)

            def fin_plain(col):
                def f(qs, ti):
                    S.op("dve", lambda e: e.tensor_scalar(out=mixo[:, ti, col:col + 64], in0=po[qs][:, 0:64], scalar1=rden[:, qs:qs + 1],
                                                          scalar2=None, op0=ALU.mult), reads=[R_po[qs], R_rden], writes=[R_mixo])
                return f

            def fin_d1(qs, ti):
                S.op("dve", lambda e: e.tensor_scalar(out=d1[:, ti, :], in0=po[qs][:, 0:64], scalar1=rden[:, qs:qs + 1], scalar2=None, op0=ALU.mult),
                     reads=[R_po[qs], R_rden], writes=[R_d1])

            def fin_d2(col):
                def f(qs, ti):
                    S.op("dve", lambda e: e.tensor_scalar(out=o2[:], in0=po[qs][:, 0:64], scalar1=rden[:, qs:qs + 1], scalar2=None, op0=ALU.mult),
                         reads=[R_po[qs], R_rden], writes=[R_t])
                    S.op("dve", lambda e: e.scalar_tensor_tensor(out=cmbt[:], in0=o2[:], scalar=neglam[:, 0:1], in1=d1[:, ti, :], op0=ALU.mult, op1=ALU.add),
                         reads=[R_t, R_d1, R_lam], writes=[R_t])
                    S.op("dve", lambda e: e.tensor_tensor(out=jk[:], in0=cmbt[:], in1=cmbt[:], op=ALU.mult), reads=[R_t], writes=[R_t])
                    S.op("dve", lambda e: e.tensor_reduce(out=ssd[:], in_=jk[:], axis=AX.X, op=ALU.add), reads=[R_t], writes=[R_t])
                    S.op("dve", lambda e: e.tensor_scalar(out=ssd[:], in0=ssd[:], scalar1=1.0 / 64, scalar2=EPS, op0=ALU.mult, op1=ALU.add), reads=[R_t], writes=[R_t])
                    S.op("act", lambda e: e.activation(out=ssd[:], in_=ssd[:], func=AF.Sqrt), reads=[R_t], writes=[R_t])
                    S.op("dve", lambda e: e.reciprocal(out=ssd[:], in_=ssd[:]), reads=[R_t], writes=[R_t])
                    S.op("dve", lambda e: e.scalar_tensor_tensor(out=mixo[:, ti, col:col + 64], in0=cmbt[:], scalar=ssd[:, 0:1], in1=subb[:], op0=ALU.mult, op1=ALU.mult),
                         reads=[R_t, R_lam], writes=[R_mixo])
                return f

            if grp == "diff":
                for h in range(4):
                    Kt, Qt, base = fmt["dk%d" % (h // 2)], fmt["dq%d" % (h // 2)], 64 * (h % 2)
                    run_map([(Kt, Qt, base, 32)], h, 32 ** -0.5, fin_d1)
                    if base + 32 == 96:
                        run_map([(kz["dk%d" % (h // 2)], Qt, 64, 64)], h, 32 ** -0.5, fin_d2(h * 64))
                    else:
                        run_map([(Kt, Qt, base + 32, 32)], h, 32 ** -0.5, fin_d2(h * 64))
            elif grp == "gqa":
                for hq in range(4):
                    n, rep = hq // 2, hq % 2
                    run_map([(fmt["gk"], fmt["gq%d" % rep], 64 * n, 64)], n, 64 ** -0.5, fin_plain(hq * 64))
            else:
                for h in range(4):
                    run_map([(Kc[h], Qc[h], 0, 96)], h, 96 ** -0.5, fin_plain(h * 64))
            S.dma("sp", P.mixd[:, moff:moff + 256].rearrange("(i p) c -> p i c", p=128), mixo[:], reads=[R_mixo], writes=[Res()])
            S.drain()


def phase_outproj(c, l):
    P, S, nc, sbt, pst = c["P"], c["S"], c["nc"], c["sbt"], c["pst"]
    cmb, R_cmb, R_xres = c["cmb"], c["R_cmb"], c["R_xres"]
    with ExitStack() as st:
        G, R_G = load_gate_bcast(c, l, st, 2, RV_G1, "a")
        wo = sbt(st, "wo", [128, 8, D], BF16); R_wo = Res()
        src = P.wout[l].rearrange("(c p) n -> p c n", p=128)
        for k in range(8):
            S.dma("pool", wo[:, k, :], src[:, k, :], writes=[R_wo])
        NB = 2
        mx = [sbt(st, "mx%d" % i, [128, D], BF16) for i in range(NB)]
        mT = [sbt(st, "mT%d" % i, [128, 8, 128], BF16) for i in range(NB)]
        xt = [sbt(st, "xt%d" % i, [128, D], F32) for i in range(NB)]
        tt = [sbt(st, "tt%d" % i, [128, D], F32) for i in range(NB)]
        junk = sbt(st, "junk", [128, D], F32)
        ss = [sbt(st, "ss%d" % i, [128, 1], F32) for i in range(NB)]
        rstd = [sbt(st, "rstd%d" % i, [128, 1], F32) for i in range(NB)]
        ptm = [pst(st, "ptm%d" % i, [128, 8, 128], BF16) for i in range(NB)]
        py = [pst(st, "py%d" % i, [128, D], F32) for i in range(NB)]
        R = lambda: [Res() for _ in range(NB)]
        R_mx, R_mT, R_xt, R_tt, R_ss, R_rstd, R_ptm, R_py = R(), R(), R(), R(), R(), R(), R(), R()
        R_junk = Res()

        def tile(i):
            b = i % NB
            r = 1 if i < 2 else 0
            S.dma("sp", mx[b][:], P.mixd[i * 128:(i + 1) * 128, :], writes=[R_mx[b]])
            S.dma("act", xt[b][:], P.xres[i * 128:(i + 1) * 128, :], reads=[R_xres[i]], writes=[R_xt[b]])
            for k in range(8):
                S.op("pe", lambda e, k=k: e.transpose(out=ptm[b][:, k, :], in_=mx[b][:, k * 128:(k + 1) * 128], identity=cmb[:, CM_ID:CM_ID + 128]),
                     reads=[R_mx[b], R_cmb], writes=[R_ptm[b]])
            S.op("act", lambda e: e.activation(out=mT[b][:], in_=ptm[b][:], func=AF.Copy), reads=[R_ptm[b]], writes=[R_mT[b]])
            for half in range(2):
                for k in range(8):
                    S.op("pe", lambda e, k=k, half=half: e.matmul(py[b][:, half * 512:(half + 1) * 512], lhsT=mT[b][:, k, :],
                                                                 rhs=wo[:, k, half * 512:(half + 1) * 512], start=(k == 0), stop=(k == 7)),
                         reads=[R_mT[b], R_wo], writes=[R_py[b]])
            rms_rstd(S, [R_py[b]], py[b][:], D, junk[:], R_junk, ss[b][:], R_ss[b], rstd[b][:], R_rstd[b])
            S.op("dve", lambda e: e.scalar_tensor_tensor(out=tt[b][:], in0=py[b][:], scalar=rstd[b][:, 0:1], in1=G[:, r, :], op0=ALU.mult, op1=ALU.mult),
                 reads=[R_py[b], R_rstd[b], R_G], writes=[R_tt[b]])
            S.op("pool", lambda e: e.tensor_tensor(out=tt[b][:], in0=tt[b][:], in1=xt[b][:], op=ALU.add), reads=[R_tt[b], R_xt[b]], writes=[R_tt[b]])
            S.dma("sp", P.xres[i * 128:(i + 1) * 128, :], tt[b][:], reads=[R_tt[b]], writes=[R_xres[i]])

        for i in range(NT):
            tile(i)
        S.drain()


def phase_experts(c, l):
    P, S, nc, sbt, pst = c["P"], c["S"], c["nc"], c["sbt"], c["pst"]
    cmb, R_cmb = c["cmb"], c["R_cmb"]
    NS = CAP // 128
    with ExitStack() as st:
        w1b = [sbt(st, "w1b%d" % i, [128, 8, 2 * D], BF16) for i in range(2)]
        w2b = [sbt(st, "w2b%d" % i, [128, 8, D], BF16) for i in range(2)]
        b1 = [sbt(st, "b1_%d" % i, [128, 16], F32) for i in range(2)]
        b2b = [sbt(st, "b2b%d" % i, [128, D], F32) for i in range(2)]
        b1p = [sbt(st, "b1p%d" % i, [128, 8], F32) for i in range(2)]
        R_w = [Res(), Res()]
        xs = [sbt(st, "xs%d" % i, [128, D], BF16) for i in range(2)]
        xT = sbt(st, "xT", [128, 8, CAP], BF16); aT = sbt(st, "aT", [128, 8, CAP], BF16)
        R_xs = [Res(), Res()]; R_xT = Res(); R_aT = Res()
        NW = 2
        gs = [sbt(st, "gs%d" % i, [128, 512], F32) for i in range(NW)]
        sg = [sbt(st, "sg%d" % i, [128, 512], F32) for i in range(NW)]
        ls = [sbt(st, "ls%d" % i, [128, 512], F32) for i in range(NW)]
        R_gs, R_sg, R_ls = [Res() for _ in range(NW)], [Res() for _ in range(NW)], [Res() for _ in range(NW)]
        ys = [sbt(st, "ys%d" % i, [128, D], F32) for i in range(2)]; R_ys = [Res(), Res()]
        ptx = [pst(st, "ptx%d" % i, [128, 8, 128], BF16) for i in range(2)]
        pg = [pst(st, "pg%d" % i, [128, 512], F32) for i in range(2)]
        pl = [pst(st, "pl%d" % i, [128, 512], F32) for i in range(2)]
        pyy = [pst(st, "pyy%d" % i, [128, 512], F32) for i in range(2)]
        R_ptx, R_pg, R_pl, R_pyy = [Res(), Res()], [Res(), Res()], [Res(), Res()], [Res(), Res()]
        cnt = dict(x=0, w=0, g=0, y=0, ys=0)

        def nxt(key, n):
            v = cnt[key]; cnt[key] = (v + 1) % n
            return v

        def load_w(e):
            b = e % 2
            s1 = P.w1[l, e].rearrange("(c p) n -> p c n", p=128)
            s2 = P.w2[l, e].rearrange("(c p) n -> p c n", p=128)
            for k in range(8):
                S.dma("pool", w1b[b][:, k, :], s1[:, k, :], writes=[R_w[b]])
            for k in range(0, 8, 2):
                S.dma("pool", w2b[b][:, k:k + 2, :], s2[:, k:k + 2, :], writes=[R_w[b]])
            S.dma("sp", b1[b][:], P.b1c[l, e], writes=[R_w[b]])
            S.op("dve", lambda e_, b=b: e_.tensor_scalar(out=b1p[b][:], in0=b1[b][:, 1::2], scalar1=1.0, scalar2=None, op0=ALU.add), reads=[R_w[b]], writes=[R_w[b]])
            S.dma("sp", b2b[b][:], bc(P.b2[l, e:e + 1, :], 128), writes=[R_w[b]])

        def expert(e):
            b = e % 2
            for s in range(NS):
                x = nxt("x", 2)
                S.dma("sp" if s % 2 else "act", xs[x][:], P.xg[e * CAP + s * 128:e * CAP + (s + 1) * 128, :], writes=[R_xs[x]])
                for k in range(8):
                    S.op("pe", lambda e_, k=k, x=x: e_.transpose(out=ptx[x][:, k, :], in_=xs[x][:, k * 128:(k + 1) * 128], identity=cmb[:, CM_ID:CM_ID + 128]),
                         reads=[R_xs[x], R_cmb], writes=[R_ptx[x]])
                S.op("act" if s % 2 else "dve", (lambda e_, x=x, s=s: e_.activation(out=xT[:, :, s * 128:(s + 1) * 128], in_=ptx[x][:], func=AF.Copy)) if s % 2 else
                     (lambda e_, x=x, s=s: e_.tensor_copy(out=xT[:, :, s * 128:(s + 1) * 128], in_=ptx[x][:])), reads=[R_ptx[x]], writes=[R_xT])
            for fc in range(8):
                for (c0, W) in ((0, 512), (512, 512), (1024, CAP - 1024)):
                    g = nxt("g", 2); w = nxt("w", NW)
                    for k in range(8):
                        S.op("pe", lambda e_, k=k, g=g, fc=fc, c0=c0, W=W: e_.matmul(pg[g][:, 0:W], lhsT=w1b[b][:, k, fc * 256:(fc + 1) * 256:2],
                                                                                     rhs=xT[:, k, c0:c0 + W], start=(k == 0), stop=(k == 7)),
                             reads=[R_w[b], R_xT], writes=[R_pg[g]])
                    for k in range(8):
                        S.op("pe", lambda e_, k=k, g=g, fc=fc, c0=c0, W=W: e_.matmul(pl[g][:, 0:W], lhsT=w1b[b][:, k, fc * 256 + 1:(fc + 1) * 256:2],
                                                                                     rhs=xT[:, k, c0:c0 + W], start=(k == 0), stop=(k == 7)),
                             reads=[R_w[b], R_xT], writes=[R_pl[g]])
                    S.op("dve", lambda e_, g=g, w=w, fc=fc, W=W: e_.tensor_scalar(out=gs[w][:, 0:W], in0=pg[g][:, 0:W], scalar1=b1[b][:, 2 * fc:2 * fc + 1], scalar2=7.0,
                                                                               op0=ALU.add, op1=ALU.min), reads=[R_pg[g], R_w[b]], writes=[R_gs[w]])
                    S.op("act", lambda e_, w=w, W=W: e_.activation(out=sg[w][:, 0:W], in_=gs[w][:, 0:W], func=AF.Sigmoid, scale=1.702), reads=[R_gs[w]], writes=[R_sg[w]])
                    S.op("dve", lambda e_, g=g, w=w, fc=fc, W=W: e_.tensor_scalar(out=ls[w][:, 0:W], in0=pl[g][:, 0:W], scalar1=b1p[b][:, fc:fc + 1], scalar2=8.0,
                                                                               op0=ALU.add, op1=ALU.min), reads=[R_pl[g], R_w[b]], writes=[R_ls[w]])
                    S.op("dve", lambda e_, w=w, W=W: e_.tensor_tensor(out=sg[w][:, 0:W], in0=gs[w][:, 0:W], in1=sg[w][:, 0:W], op=ALU.mult),
                         reads=[R_gs[w], R_sg[w]], writes=[R_sg[w]])
                    S.op("dve", lambda e_, w=w, W=W, fc=fc, c0=c0: e_.scalar_tensor_tensor(out=aT[:, fc, c0:c0 + W], in0=ls[w][:, 0:W], scalar=-6.0, in1=sg[w][:, 0:W],
                                                                                         op0=ALU.max, op1=ALU.mult),
                         reads=[R_ls[w], R_sg[w]], writes=[R_aT])
            for s in range(NS):
                yb = nxt("ys", 2)
                for half in range(2):
                    y = nxt("y", 2)
                    for fc in range(8):
                        S.op("pe", lambda e_, fc=fc, y=y, s=s, half=half: e_.matmul(pyy[y][:], lhsT=aT[:, fc, s * 128:(s + 1) * 128],
                                                                                   rhs=w2b[b][:, fc, half * 512:(half + 1) * 512], start=(fc == 0), stop=(fc == 7)),
                             reads=[R_aT, R_w[b]], writes=[R_pyy[y]])
                    S.op("dve", lambda e_, y=y, yb=yb, half=half: e_.tensor_tensor(out=ys[yb][:, half * 512:(half + 1) * 512], in0=pyy[y][:],
                                                                                  in1=b2b[b][:, half * 512:(half + 1) * 512], op=ALU.add),
                         reads=[R_pyy[y], R_w[b]], writes=[R_ys[yb]])
                S.dma("sp", P.yg[e * CAP + s * 128:e * CAP + (s + 1) * 128, :], ys[yb][:], reads=[R_ys[yb]], writes=[Res()])

        load_w(0)
        for e in range(NEXP):
            if e + 1 < NEXP and F_MODE != "noload":
                load_w(e + 1)
            if F_MODE != "nocompute":
                expert(e)
        S.drain()


def phase_combine(c, l, rt):
    P, S, nc, sbt, pst = c["P"], c["S"], c["nc"], c["sbt"], c["pst"]
    R_xres = c["R_xres"]
    with ExitStack() as st:
        G, R_G = load_gate_bcast(c, l, st, 5, RV_G3, "f")
        NB = 2
        yk = [[sbt(st, "yk%d_%d" % (i, k), [128, D], F32) for k in range(4)] for i in range(NB)]
        R_yk = [[Res() for k in range(4)] for i in range(NB)]
        acc = [sbt(st, "acc%d" % i, [128, D], F32) for i in range(NB)]
        xt = [sbt(st, "xt%d" % i, [128, D], F32) for i in range(NB)]
        junk = sbt(st, "junk", [128, D], F32); R_junk = Res()
        ss = [sbt(st, "ss%d" % i, [128, 1], F32) for i in range(NB)]
        rstd = [sbt(st, "rstd%d" % i, [128, 1], F32) for i in range(NB)]
        R = lambda: [Res() for _ in range(NB)]
        R_acc, R_xt, R_ss, R_rstd = R(), R(), R(), R()
        gates, slots = rt["gates"], rt["slots"]

        def tile(i):
            b = i % NB
            r = 1 if i < 2 else 0
            S.dma("sp", xt[b][:], P.xres[i * 128:(i + 1) * 128, :], reads=[R_xres[i]], writes=[R_xt[b]])
            for k in range(4):
                S.dma("pool", yk[b][k][:], P.yg[:, :], reads=[rt["R_slots"]], writes=[R_yk[b][k]],
                      indirect=dict(out_offset=None, in_offset=bass.IndirectOffsetOnAxis(ap=slots[:, i, k:k + 1], axis=0)))
            S.op("dve", lambda e: e.tensor_scalar(out=acc[b][:], in0=yk[b][0][:], scalar1=gates[:, i, 0:1], scalar2=None, op0=ALU.mult),
                 reads=[R_yk[b][0], rt["R_gates"]], writes=[R_acc[b]])
            for k in range(1, 4):
                S.op("dve", lambda e, k=k: e.scalar_tensor_tensor(out=acc[b][:], in0=yk[b][k][:], scalar=gates[:, i, k:k + 1], in1=acc[b][:],
                                                                                      op0=ALU.mult, op1=ALU.add),
                     reads=[R_yk[b][k], rt["R_gates"], R_acc[b]], writes=[R_acc[b]])
            rms_rstd(S, [R_acc[b]], acc[b][:], D, junk[:], R_junk, ss[b][:], R_ss[b], rstd[b][:], R_rstd[b])
            S.op("dve", lambda e: e.scalar_tensor_tensor(out=acc[b][:], in0=acc[b][:], scalar=rstd[b][:, 0:1], in1=G[:, r, :], op0=ALU.mult, op1=ALU.mult),
                 reads=[R_acc[b], R_rstd[b], R_G], writes=[R_acc[b]])
            S.op("pool", lambda e: e.tensor_tensor(out=acc[b][:], in0=acc[b][:], in1=xt[b][:], op=ALU.add), reads=[R_acc[b], R_xt[b]], writes=[R_acc[b]])
            S.dma("sp", P.xres[i * 128:(i + 1) * 128, :], acc[b][:], reads=[R_acc[b]], writes=[R_xres[i]])

        for i in range(NT):
            tile(i)
        S.drain()


def phase_retention(c, l):
    P, S, nc, sbt, pst = c["P"], c["S"], c["nc"], c["sbt"], c["pst"]
    cm, R_cm, cmb, R_cmb = c["cm"], c["R_cm"], c["cmb"], c["R_cmb"]
    with ExitStack() as st:
        QT = [sbt(st, "rQT%d" % p, [128, T], BF16) for p in range(2)]
        KT = [sbt(st, "rKT%d" % p, [128, T], BF16) for p in range(2)]
        QfT = [sbt(st, "rQfT%d" % p, [128, T], BF16) for p in range(2)]
        QbT = [sbt(st, "rQbT%d" % p, [128, T], BF16) for p in range(2)]
        V = sbt(st, "rV", [128, NT, 256], BF16); Gt = sbt(st, "rG", [128, NT, 256], BF16)
        Kzf = sbt(st, "Kzf", [128, NT, 256], BF16); Kzb = sbt(st, "Kzb", [128, NT, 256], BF16)
        SfAll = sbt(st, "SfAll", [128, NT, 2, 64], BF16); SbAll = sbt(st, "SbAll", [128, NT, 2, 64], BF16)
        mixo = sbt(st, "rmixo", [128, NT, 256], BF16)
        R_in, R_q, R_kz, R_sall, R_mixo = Res(), Res(), Res(), Res(), Res()
        for p in range(2):
            S.dma("sp", QT[p][:], P.fm[FM["rq%d" % p]], writes=[R_in])
            S.dma("act", KT[p][:], P.fm[FM["rk%d" % p]], writes=[R_in])
        S.dma("sp", V[:], P.tokv[:, TV_RV:TV_RV + 256].rearrange("(i p) c -> p i c", p=128), writes=[R_in])
        S.dma("act", Gt[:], P.tokv[:, TV_RG:TV_RG + 256].rearrange("(i p) c -> p i c", p=128), writes=[R_in])
        lg = sbt(st, "lg", [128, 8], F32); lgc = sbt(st, "lgc", [128, 4], F32); gC = sbt(st, "gC", [128, 4], F32)
        DT = sbt(st, "DT", [128, 4, 128], F32); e1 = sbt(st, "e1", [128, 128], F32); e2 = sbt(st, "e2", [128, 128], F32)
        Xi = sbt(st, "Xi", [128, 4, 128], BF16)
        Zt = sbt(st, "Zt", [128, 8], F32)
        gnw = sbt(st, "gnw", [128, 256], F32); gnb = sbt(st, "gnb", [128, 256], F32)
        R_t = Res()
        S.dma("sp", lg[:], bc(P.rowv[l:l + 1, RV_DECAY:RV_DECAY + 8], 128), writes=[R_t])
        S.dma("sp", gnw[:], bc(P.rowv[l:l + 1, RV_GNW:RV_GNW + 256], 128), writes=[R_t])
        S.dma("sp", gnb[:], bc(P.rowv[l:l + 1, RV_GNB:RV_GNB + 256], 128), writes=[R_t])
        S.op("act", lambda e: e.activation(out=lg[:], in_=lg[:], func=AF.Exp), reads=[R_t], writes=[R_t])
        S.op("dve", lambda e: e.tensor_scalar(out=lg[:], in0=lg[:], scalar1=-1.0, scalar2=None, op0=ALU.mult), reads=[R_t], writes=[R_t])
        for h in range(4):
            S.op("act", lambda e, h=h: e.activation(out=e1[:], in_=cm[:, CM_DPOS:CM_DPOS + 128], func=AF.Exp, scale=lg[:, h:h + 1]), reads=[R_t, R_cm], writes=[R_t])
            S.op("dve", lambda e: e.tensor_tensor(out=e1[:], in0=e1[:], in1=cm[:, CM_MF:CM_MF + 128], op=ALU.mult), reads=[R_t, R_cm], writes=[R_t])
            S.op("act", lambda e, h=h: e.activation(out=e2[:], in_=cm[:, CM_DNEG:CM_DNEG + 128], func=AF.Exp, scale=lg[:, 4 + h:5 + h]), reads=[R_t, R_cm], writes=[R_t])
            S.op("dve", lambda e: e.tensor_tensor(out=e2[:], in0=e2[:], in1=cm[:, CM_MB:CM_MB + 128], op=ALU.mult), reads=[R_t, R_cm], writes=[R_t])
            S.op("dve", lambda e, h=h: e.tensor_tensor(out=DT[:, h, :], in0=e1[:], in1=e2[:], op=ALU.add), reads=[R_t], writes=[R_t])
        for d in range(2):
            for p in range(2):
                S.op("dve", lambda e, d=d, p=p: e.tensor_copy(out=lgc[0:64, 2 * d + p:2 * d + p + 1], in_=lg[0:64, 4 * d + 2 * p:4 * d + 2 * p + 1]), reads=[R_t], writes=[R_t])
                S.op("dve", lambda e, d=d, p=p: e.tensor_copy(out=lgc[64:128, 2 * d + p:2 * d + p + 1], in_=lg[64:128, 4 * d + 2 * p + 1:4 * d + 2 * p + 2]), reads=[R_t], writes=[R_t])
        for p in range(2):
            S.op("act", lambda e, p=p: e.activation(out=Xi[:, p, :], in_=cm[:, CM_NP1:CM_NP1 + 128], func=AF.Exp, scale=lgc[:, p:p + 1]), reads=[R_t, R_cm], writes=[R_t])
            S.op("act", lambda e, p=p: e.activation(out=Xi[:, 2 + p, :], in_=cm[:, CM_NREV:CM_NREV + 128], func=AF.Exp, scale=lgc[:, 2 + p:3 + p]), reads=[R_t, R_cm], writes=[R_t])
        S.op("act", lambda e: e.activation(out=Zt[:, 0:4], in_=lg[:, 0:4], func=AF.Exp, scale=cm[:, CM_PREV:CM_PREV + 1]), reads=[R_t, R_cm], writes=[R_t])
        S.op("act", lambda e: e.activation(out=Zt[:, 4:8], in_=lg[:, 4:8], func=AF.Exp, scale=cm[:, CM_PCOL:CM_PCOL + 1]), reads=[R_t, R_cm], writes=[R_t])
        S.op("act", lambda e: e.activation(out=gC[:], in_=lgc[:], func=AF.Exp, scale=128.0), reads=[R_t], writes=[R_t])
        if RET_STOP == 1:
            S.drain()
            return
        for p in range(2):
            S.op("pool", lambda e, p=p: e.tensor_tensor(out=QfT[p][:].rearrange("f (c n) -> f c n", n=128), in0=QT[p][:].rearrange("f (c n) -> f c n", n=128),
                                                      in1=Xi[:, p:p + 1, :].to_broadcast([128, NT, 128]), op=ALU.mult), reads=[R_in, R_t], writes=[R_q])
            S.op("pool", lambda e, p=p: e.tensor_tensor(out=QbT[p][:].rearrange("f (c n) -> f c n", n=128), in0=QT[p][:].rearrange("f (c n) -> f c n", n=128),
                                                      in1=Xi[:, 2 + p:3 + p, :].to_broadcast([128, NT, 128]), op=ALU.mult), reads=[R_in, R_t], writes=[R_q])
        if RET_STOP == 2:
            S.drain()
            return
        ptk = [pst(st, "ptk%d" % i, [128, 1024], BF16) for i in range(2)]; R_ptk = [Res(), Res()]
        for cidx in range(NT):
            b = cidx % 2
            for p in range(2):
                S.op("pe", lambda e, p=p, b=b, cidx=cidx: e.transpose(out=ptk[b][:, p * 128:(p + 1) * 128], in_=KT[p][:, cidx * 128:(cidx + 1) * 128], identity=cmb[:, CM_ID:CM_ID + 128]),
                     reads=[R_in, R_cmb], writes=[R_ptk[b]])
            S.op("dve", lambda e, b=b, cidx=cidx: e.tensor_tensor(out=Kzf[:, cidx, :].rearrange("p (h d) -> p h d", h=4), in0=ptk[b][:, 0:256].rearrange("p (h d) -> p h d", h=4),
                                                                in1=Zt[:, 0:4, None].to_broadcast([128, 4, 64]), op=ALU.mult), reads=[R_ptk[b], R_t], writes=[R_kz])
            S.op("dve", lambda e, b=b, cidx=cidx: e.tensor_tensor(out=Kzb[:, cidx, :].rearrange("p (h d) -> p h d", h=4), in0=ptk[b][:, 0:256].rearrange("p (h d) -> p h d", h=4),
                                                                in1=Zt[:, 4:8, None].to_broadcast([128, 4, 64]), op=ALU.mult), reads=[R_ptk[b], R_t], writes=[R_kz])
        if RET_STOP == 3:
            S.drain()
            return
        pu = [pst(st, "pu%d" % i, [128, 512], F32) for i in range(2)]; R_pu = [Res(), Res()]
        Sst = sbt(st, "Sst", [128, 2, 64], F32); R_S = Res()
        ucnt = [0]
        for d, Kz, SAll, order in ((0, Kzf, SfAll, list(range(NT))), (1, Kzb, SbAll, [1, 0] + list(range(NT - 1, 1, -1)))):
            S.op("pool", lambda e: e.memset(Sst[:], 0.0), reads=[R_S], writes=[R_S])
            for cidx in order:
                S.op("act", lambda e, SAll=SAll, cidx=cidx: e.activation(out=SAll[:, cidx, :, :], in_=Sst[:], func=AF.Copy), reads=[R_S], writes=[R_sall])
                for p in range(2):
                    u = ucnt[0]; ucnt[0] = (u + 1) % 2
                    S.op("pe", lambda e, u=u, Kz=Kz, cidx=cidx, p=p: e.matmul(pu[u][:, 0:128], lhsT=Kz[:, cidx, p * 128:(p + 1) * 128], rhs=V[:, cidx, p * 128:(p + 1) * 128], start=True, stop=True),
                         reads=[R_kz, R_in], writes=[R_pu[u]])
                    for half in range(2):
                        rows = slice(64 * half, 64 * half + 64)
                        S.op("dve", lambda e, u=u, p=p, d=d, rows=rows: e.scalar_tensor_tensor(out=Sst[rows, p, :], in0=Sst[rows, p, :], scalar=gC[rows, 2 * d + p:2 * d + p + 1],
                                                                                            in1=pu[u][rows, rows], op0=ALU.mult, op1=ALU.add),
                             reads=[R_S, R_pu[u], R_t], writes=[R_S])
        if RET_STOP == 4:
            S.drain()
            return
        paE = pst(st, "rpaE", [128, 512], F32); paO = pst(st, "rpaO", [128, 512], F32)
        poE = pst(st, "rpoE", [128, 512], F32); poO = pst(st, "rpoO", [128, 512], F32)
        pa_ = (paE, paO); po_ = (poE, poO)
        R_pa = [Res(), Res()]; R_po = [Res(), Res()]
        Wt = [sbt(st, "Wt%d" % i, [128, 4, 128], BF16) for i in range(2)]; R_W = [Res(), Res()]
        ob = [sbt(st, "rob%d" % i, [128, 4, 64], F32) for i in range(2)]; xc = [sbt(st, "rxc%d" % i, [128, 4, 64], F32) for i in range(2)]
        sqv = [sbt(st, "rsq%d" % i, [128, 4, 64], F32) for i in range(2)]
        mu = [sbt(st, "rmu%d" % i, [128, 4], F32) for i in range(2)]; var = [sbt(st, "rvar%d" % i, [128, 4], F32) for i in range(2)]
        R_o = [Res(), Res()]

        def chunk(cidx):
            b = cidx % 2
            cs = slice(cidx * 128, (cidx + 1) * 128)
            for h in (0, 2, 1, 3):
                p, par, rows = h // 2, h % 2, slice(64 * (h % 2), 64 * (h % 2) + 64)
                S.op("pe", lambda e, h=h, p=p, par=par, rows=rows: e.matmul(pa_[par][:, p * 128:(p + 1) * 128], lhsT=KT[p][rows, cs], rhs=QT[p][rows, cs], start=True, stop=True),
                     reads=[R_in], writes=[R_pa[par]])
            for h in range(4):
                p, par = h // 2, h % 2
                S.op("dve", lambda e, h=h, p=p, par=par: e.tensor_tensor(out=Wt[b][:, h, :], in0=pa_[par][:, p * 128:(p + 1) * 128], in1=DT[:, h, :], op=ALU.mult),
                     reads=[R_pa[par], R_t], writes=[R_W[b]])
            for h in (0, 2, 1, 3):
                p, par, rows = h // 2, h % 2, slice(64 * (h % 2), 64 * (h % 2) + 64)
                dst = po_[par][:, p * 64:(p + 1) * 64]
                S.op("pe", lambda e, h=h, dst=dst: e.matmul(dst, lhsT=Wt[b][:, h, :], rhs=V[:, cidx, h * 64:(h + 1) * 64], start=True, stop=False),
                     reads=[R_W[b], R_in], writes=[R_po[par]])
                S.op("pe", lambda e, h=h, p=p, rows=rows, dst=dst: e.matmul(dst, lhsT=QfT[p][rows, cs], rhs=SfAll[rows, cidx, p, :], start=False, stop=False),
                     reads=[R_q, R_sall], writes=[R_po[par]])
                S.op("pe", lambda e, h=h, p=p, rows=rows, dst=dst: e.matmul(dst, lhsT=QbT[p][rows, cs], rhs=SbAll[rows, cidx, p, :], start=False, stop=True),
                     reads=[R_q, R_sall], writes=[R_po[par]])
            Ro = R_o[b]
            for h in range(4):
                p, par = h // 2, h % 2
                S.op("act", lambda e, h=h, p=p, par=par: e.activation(out=ob[b][:, h, :], in_=po_[par][:, p * 64:(p + 1) * 64], func=AF.Copy, scale=0.125),
                     reads=[R_po[par]], writes=[Ro])
            S.op("dve", lambda e: e.tensor_reduce(out=mu[b][:], in_=ob[b][:], axis=AX.X, op=ALU.add), reads=[Ro], writes=[Ro])
            S.op("dve", lambda e: e.tensor_scalar(out=mu[b][:], in0=mu[b][:], scalar1=1.0 / 64, scalar2=None, op0=ALU.mult), reads=[Ro], writes=[Ro])
            S.op("pool", lambda e: e.tensor_tensor(out=xc[b][:], in0=ob[b][:], in1=mu[b][:, :, None].to_broadcast([128, 4, 64]), op=ALU.subtract), reads=[Ro], writes=[Ro])
            S.op("pool", lambda e: e.tensor_tensor(out=sqv[b][:], in0=xc[b][:], in1=xc[b][:], op=ALU.mult), reads=[Ro], writes=[Ro])
            S.op("dve", lambda e: e.tensor_reduce(out=var[b][:], in_=sqv[b][:], axis=AX.X, op=ALU.add), reads=[Ro], writes=[Ro])
            S.op("dve", lambda e: e.tensor_scalar(out=var[b][:], in0=var[b][:], scalar1=1.0 / 64, scalar2=EPS, op0=ALU.mult, op1=ALU.add), reads=[Ro], writes=[Ro])
            S.op("act", lambda e: e.activation(out=var[b][:], in_=var[b][:], func=AF.Sqrt), reads=[Ro], writes=[Ro])
            S.op("dve", lambda e: e.reciprocal(out=var[b][:], in_=var[b][:]), reads=[Ro], writes=[Ro])
            S.op("pool", lambda e: e.tensor_tensor(out=xc[b][:], in0=xc[b][:], in1=var[b][:, :, None].to_broadcast([128, 4, 64]), op=ALU.mult), reads=[Ro], writes=[Ro])
            xcf = xc[b][:].rearrange("p h d -> p (h d)")
            S.op("pool", lambda e: e.tensor_tensor(out=xcf, in0=xcf, in1=gnw[:], op=ALU.mult), reads=[Ro, R_t], writes=[Ro])
            S.op("pool", lambda e: e.tensor_tensor(out=xcf, in0=xcf, in1=gnb[:], op=ALU.add), reads=[Ro, R_t], writes=[Ro])
            S.op("pool", lambda e: e.tensor_tensor(out=mixo[:, cidx, :], in0=xcf, in1=Gt[:, cidx, :], op=ALU.mult), reads=[Ro, R_in], writes=[R_mixo])

        for cidx in range(NT):
            chunk(cidx)
        S.dma("sp", P.mixd[:, 0:256].rearrange("(i p) c -> p i c", p=128), mixo[:], reads=[R_mixo], writes=[Res()])
        S.drain()


_PROG_CACHE = {}


def kernel(**inputs):
    n = 8
    if DEPTH not in _PROG_CACHE:
        _PROG_CACHE[DEPTH] = build_program(DEPTH)
    P = _PROG_CACHE[DEPTH]
    cmn, tabs = build_consts()
    x = np.asarray(inputs["x"], np.float32)
    ctxv = np.asarray(inputs["ctx"], np.float32)
    la = prep_layer_arrays(inputs, list(range(DEPTH)))
    in_maps = []
    for b in range(n):
        xin = np.ascontiguousarray(np.concatenate([ctxv[b], x[b]], 0))
        cvec = np.ascontiguousarray(np.concatenate([_col(inputs["c"][b]), _col(inputs["c_ctx"])], 1))
        in_maps.append(dict(xin=xin, cvec=cvec, cmat=cmn, ropet=tabs, **la))
    res = run_bass_kernel_spmd(P.nc, in_maps, core_ids=list(range(n)))
    return np.stack([np.asarray(r["yout"], np.float32)[NCTX:] for r in res.results], 0)
```
